# Optimizing a Trainium2 kernel written in Bass

```python
import math
import jax, jax.numpy as jnp
from jax import lax
import numpy as np

D_MODEL = 1024
BATCH = 4
SEQ = 4096
DEPTH = 2

F32 = jnp.float32
EPS = 1e-6
NEG_INF = -1e30
Q_BLOCK = 128
N_MEM = 256
POS_OFFSET_MAX = 1024
S5_WIDTH = 256
S5_GROUP = 16
S5_GROUPS = S5_WIDTH // S5_GROUP
S5_STATE = 64
S5_DT_MIN = 1e-3
S5_DT_MAX = 1e-1
MLA_HEADS = 4
MLA_Q_RANK = 192
MLA_KV_RANK = 128
MLA_NOPE = 64
MLA_ROPE = 32
MLA_V = 64
MLA_WIDTH = MLA_HEADS * MLA_V
ROPE_THETA = 10000.0
LRU_WIDTH = 256
LRU_BLOCKS = 4
LRU_BLOCK = LRU_WIDTH // LRU_BLOCKS
LRU_CONV = 4
LRU_C = 8.0
DIFF_HEADS = 4
DIFF_HEAD_DIM = 32
DIFF_V = 2 * DIFF_HEAD_DIM
DIFF_WIDTH = DIFF_HEADS * DIFF_V
REL_BUCKETS = 32
REL_MAX_DIST = 128
CROSS_HEADS = 4
CROSS_HEAD_DIM = 64
CROSS_WIDTH = CROSS_HEADS * CROSS_HEAD_DIM
DENSE_FF = 2816
N_EXPERTS = 8
TOP_K = 2
EXPERT_FF = 3584
MOE_BLOCK = 256
N_DENSE = (DEPTH + 1) // 2
N_MOE = DEPTH // 2
N_BRANCH = 4
BRANCH_WIDTH = 256
IN_SIZES = (S5_WIDTH, MLA_Q_RANK, MLA_KV_RANK, MLA_ROPE, LRU_WIDTH, LRU_WIDTH,
            DIFF_HEADS * 2 * DIFF_HEAD_DIM, DIFF_HEADS * 2 * DIFF_HEAD_DIM, DIFF_WIDTH,
            N_BRANCH * D_MODEL)
D_IN = sum(IN_SIZES)

kernel_name = 'hybrid_gated_decoder_layers'


def rms_norm(x, g):
    xf = x.astype(F32)
    y = xf * lax.rsqrt(jnp.mean(xf * xf, axis=-1, keepdims=True) + EPS)
    return (y * g.astype(F32)).astype(x.dtype)


def rope(x, pos):
    half = x.shape[-1] // 2
    inv = ROPE_THETA ** (-jnp.arange(half, dtype=F32) / half)
    ang = pos.astype(F32)[:, :, None, None] * inv
    cos, sin = jnp.cos(ang), jnp.sin(ang)
    x1 = x[..., :half].astype(F32)
    x2 = x[..., half:].astype(F32)
    return jnp.concatenate([x1 * cos - x2 * sin, x1 * sin + x2 * cos], axis=-1).astype(x.dtype)


def rel_bucket(dist):
    n = jnp.maximum(dist, 0)
    exact = REL_BUCKETS // 2
    log_ratio = jnp.log(jnp.maximum(n, exact).astype(F32) / exact) / math.log(REL_MAX_DIST / exact)
    large = jnp.minimum(exact + (log_ratio * (REL_BUCKETS - exact)).astype(jnp.int32), REL_BUCKETS - 1)
    return jnp.where(n < exact, n, large)


def to_blocks(a):
    b, s = a.shape[:2]
    a = a.reshape((b, s // Q_BLOCK, Q_BLOCK) + a.shape[2:])
    return jnp.moveaxis(a, 1, 0)


def from_blocks(a):
    a = jnp.moveaxis(a, 0, 1)
    return a.reshape((a.shape[0], a.shape[1] * a.shape[2]) + a.shape[3:])


def _affine_combine(e1, e2):
    a1, b1 = e1
    a2, b2 = e2
    return (a1 * a2, a2 * b1 + b2)


def _complex_affine_combine(e1, e2):
    a1r, a1i, b1r, b1i = e1
    a2r, a2i, b2r, b2i = e2
    return (a2r * a1r - a2i * a1i, a2r * a1i + a2i * a1r,
            a2r * b1r - a2i * b1i + b2r, a2r * b1i + a2i * b1r + b2i)


def s5_branch(u, lam_re, lam_im, log_step, b_re, b_im, c_re, c_im, d_skip, w_glu, b_glu):
    b, s, _ = u.shape
    uf = u.astype(F32)
    ug = uf.reshape(b, s, S5_GROUPS, S5_GROUP)
    dt = jnp.exp(log_step.astype(F32))[:, None]
    lr = lam_re.astype(F32)
    li = lam_im.astype(F32)
    mag = jnp.exp(lr * dt)
    ar = mag * jnp.cos(li * dt)
    ai = mag * jnp.sin(li * dt)
    den = lr * lr + li * li
    zr = ((ar - 1.0) * lr + ai * li) / den
    zi = (ai * lr - (ar - 1.0) * li) / den
    br = b_re.astype(F32)
    bi = b_im.astype(F32)
    bbr = zr[..., None] * br - zi[..., None] * bi
    bbi = zr[..., None] * bi + zi[..., None] * br
    bur = jnp.einsum('bsgc,gpc->bsgp', ug, bbr)
    bui = jnp.einsum('bsgc,gpc->bsgp', ug, bbi)
    shp = bur.shape
    _, _, hr, hi = lax.associative_scan(
        _complex_affine_combine,
        (jnp.broadcast_to(ar, shp), jnp.broadcast_to(ai, shp), bur, bui), axis=1)
    y = (jnp.einsum('bsgp,gcp->bsgc', hr, c_re.astype(F32))
         - jnp.einsum('bsgp,gcp->bsgc', hi, c_im.astype(F32)))
    y = y.reshape(b, s, S5_WIDTH) + d_skip.astype(F32) * uf
    y = jax.nn.gelu(y)
    y = y * jax.nn.sigmoid(jnp.dot(y, w_glu.astype(F32)) + b_glu.astype(F32))
    return y.astype(u.dtype)


def mla_branch(cq, ckv, kpe, pos, g_cq, g_ckv, w_uq, w_ukv, g_qn, g_kn):
    b, s, _ = cq.shape
    q = jnp.dot(rms_norm(cq, g_cq), w_uq).reshape(b, s, MLA_HEADS, MLA_NOPE + MLA_ROPE)
    kv = jnp.dot(rms_norm(ckv, g_ckv), w_ukv).reshape(b, s, MLA_HEADS, MLA_NOPE + MLA_V)
    k_pe = jnp.broadcast_to(kpe[:, :, None, :], (b, s, MLA_HEADS, MLA_ROPE))
    k = jnp.concatenate([kv[..., :MLA_NOPE], k_pe], axis=-1)
    v = kv[..., MLA_NOPE:]
    q = rms_norm(q, g_qn)
    k = rms_norm(k, g_kn)
    q = jnp.concatenate([q[..., :MLA_NOPE], rope(q[..., MLA_NOPE:], pos)], axis=-1) * (MLA_NOPE + MLA_ROPE) ** -0.5
    k = jnp.concatenate([k[..., :MLA_NOPE], rope(k[..., MLA_NOPE:], pos)], axis=-1)

    def one_block(args):
        qb, pb = args
        sc = jnp.einsum('bqhd,bkhd->bhqk', qb, k).astype(F32)
        mask = pb[:, None, :, None] >= pos[:, None, None, :]
        p = jax.nn.softmax(jnp.where(mask, sc, NEG_INF), axis=-1).astype(v.dtype)
        return jnp.einsum('bhqk,bkhd->bqhd', p, v)

    out = from_blocks(lax.map(one_block, (to_blocks(q), to_blocks(pos))))
    return out.reshape(b, s, MLA_WIDTH)


def rglru_branch(xb, gb, conv_w, conv_b, w_r, b_r, w_i, b_i, lam):
    b, s, w = xb.shape
    xp = jnp.pad(xb, ((0, 0), (LRU_CONV - 1, 0), (0, 0)))
    xc = (conv_b + sum(xp[:, k:k + s] * conv_w[k] for k in range(LRU_CONV))).astype(F32)
    xblk = xc.reshape(b, s, LRU_BLOCKS, LRU_BLOCK)
    r = jax.nn.sigmoid(jnp.einsum('bsnc,ncd->bsnd', xblk, w_r.astype(F32)).reshape(b, s, w) + b_r.astype(F32))
    i = jax.nn.sigmoid(jnp.einsum('bsnc,ncd->bsnd', xblk, w_i.astype(F32)).reshape(b, s, w) + b_i.astype(F32))
    log_a = -LRU_C * r * jax.nn.softplus(-lam.astype(F32))
    a = jnp.exp(log_a)
    inp = jnp.sqrt(-jnp.expm1(2.0 * log_a)) * (i * xc)
    _, hseq = lax.associative_scan(_affine_combine, (a, inp), axis=1)
    return (hseq * jax.nn.gelu(gb.astype(F32))).astype(xb.dtype)


def diff_branch(q, k, v, pos, rel_table, g_qn, g_kn, lq1, lk1, lq2, lk2, g_sub, lam_init):
    b, s, _ = q.shape
    q = rms_norm(q.reshape(b, s, DIFF_HEADS, 2, DIFF_HEAD_DIM), g_qn) * DIFF_HEAD_DIM ** -0.5
    k = rms_norm(k.reshape(b, s, DIFF_HEADS, 2, DIFF_HEAD_DIM), g_kn)
    v = v.reshape(b, s, DIFF_HEADS, DIFF_V)
    k1 = k[:, :, :, 0]
    k2 = k[:, :, :, 1]
    lam = (jnp.exp(jnp.sum(lq1.astype(F32) * lk1.astype(F32)))
           - jnp.exp(jnp.sum(lq2.astype(F32) * lk2.astype(F32))) + lam_init)

    def one_block(args):
        qb, pb = args
        dist = pb[:, :, None] - pos[:, None, :]
        bias = jnp.moveaxis(rel_table[rel_bucket(dist)], -1, 1).astype(F32)
        mask = (dist >= 0)[:, None]

        def attn_probs(qq, kk):
            sc = jnp.einsum('bqhd,bkhd->bhqk', qq, kk).astype(F32) + bias
            return jax.nn.softmax(jnp.where(mask, sc, NEG_INF), axis=-1)

        p = attn_probs(qb[:, :, :, 0], k1) - lam * attn_probs(qb[:, :, :, 1], k2)
        return jnp.einsum('bhqk,bkhd->bqhd', p.astype(v.dtype), v)

    out = from_blocks(lax.map(one_block, (to_blocks(q), to_blocks(pos))))
    out = rms_norm(out, g_sub) * (1.0 - lam_init)
    return out.reshape(b, s, DIFF_WIDTH)


def cross_attention(h, m, w_q, w_k, w_v, w_o, g_qn, g_kn):
    b, s, _ = h.shape
    n = m.shape[1]
    q = rms_norm(jnp.dot(h, w_q).reshape(b, s, CROSS_HEADS, CROSS_HEAD_DIM), g_qn) * CROSS_HEAD_DIM ** -0.5
    k = rms_norm(jnp.dot(m, w_k).reshape(b, n, CROSS_HEADS, CROSS_HEAD_DIM), g_kn)
    v = jnp.dot(m, w_v).reshape(b, n, CROSS_HEADS, CROSS_HEAD_DIM)
    p = jax.nn.softmax(jnp.einsum('bqhd,bkhd->bhqk', q, k).astype(F32), axis=-1).astype(v.dtype)
    o = jnp.einsum('bhqk,bkhd->bqhd', p, v).reshape(b, s, CROSS_WIDTH)
    return jnp.dot(o, w_o)


def swiglu(h, w_gate, w_up, w_down):
    return jnp.dot(jax.nn.silu(jnp.dot(h, w_gate)) * jnp.dot(h, w_up), w_down)


def moe_swiglu(h, w_router, w_gate, w_up, w_down):
    b, s, d = h.shape
    n_tok = b * s
    n_assign = n_tok * TOP_K
    n_blk = -(-(n_assign + N_EXPERTS * (MOE_BLOCK - 1)) // MOE_BLOCK)
    n_rows = n_blk * MOE_BLOCK
    ht = h.reshape(n_tok, d)
    logits = jnp.dot(ht, w_router).astype(F32)
    top_v, top_e = lax.top_k(logits, TOP_K)
    top_w = jax.nn.softmax(top_v, axis=-1)
    flat_e = top_e.reshape(-1)
    flat_t = jnp.repeat(jnp.arange(n_tok, dtype=jnp.int32), TOP_K)
    flat_w = top_w.reshape(-1)
    order = jnp.argsort(flat_e)
    se, st, sw = flat_e[order], flat_t[order], flat_w[order]
    counts = jnp.bincount(flat_e, length=N_EXPERTS)
    padded = (counts + MOE_BLOCK - 1) // MOE_BLOCK * MOE_BLOCK
    grp_start = jnp.cumsum(counts) - counts
    pad_end = jnp.cumsum(padded)
    pad_start = pad_end - padded
    dest = pad_start[se] + jnp.arange(n_assign, dtype=jnp.int32) - grp_start[se]
    row_tok = jnp.zeros((n_rows,), jnp.int32).at[dest].set(st)
    row_w = jnp.zeros((n_rows,), F32).at[dest].set(sw)
    blk_e = jnp.minimum(jnp.searchsorted(pad_end, jnp.arange(n_blk) * MOE_BLOCK, side='right'), N_EXPERTS - 1)
    xs = ht[row_tok].reshape(n_blk, MOE_BLOCK, d)

    def one_block(args):
        xb, e = args
        return swiglu(xb, w_gate[e], w_up[e], w_down[e])

    ys = lax.map(one_block, (xs, blk_e)).reshape(n_rows, d)
    out = jnp.zeros((n_tok, d), h.dtype).at[row_tok].add(ys * row_w[:, None].astype(ys.dtype))
    return out.reshape(b, s, d)


def _in_split_points():
    pts, acc = [], 0
    for w in IN_SIZES[:-1]:
        acc += w
        pts.append(acc)
    return pts


def setup_inputs(seed: int = 0) -> dict:
    key = jax.random.key(seed)
    keys = iter(jax.random.split(key, 80))

    def nrm(shape, scale):
        return scale * jax.random.normal(next(keys), shape, F32)

    def gain(shape):
        return 1.0 + 0.01 * jax.random.normal(next(keys), shape, F32)

    L = DEPTH
    x = nrm((BATCH, SEQ, D_MODEL), 1.0)
    mem = nrm((BATCH, N_MEM, D_MODEL), 1.0)
    offsets = jax.random.randint(next(keys), (BATCH, 1), 0, POS_OFFSET_MAX, jnp.int32)
    positions = offsets + jnp.arange(SEQ, dtype=jnp.int32)[None, :]
    rel_table = nrm((REL_BUCKETS, DIFF_HEADS), 0.5)
    g_mix = gain((L, D_MODEL))
    g_cross = gain((L, D_MODEL))
    g_mem = gain((L, D_MODEL))
    g_ffn = gain((L, D_MODEL))
    w_in = nrm((L, D_MODEL, D_IN), D_MODEL ** -0.5)
    b_gate = nrm((L, N_BRANCH, D_MODEL), 0.01)
    s5_lam_re = -0.5 + nrm((L, S5_GROUPS, S5_STATE), 0.01)
    s5_lam_im = math.pi * jnp.arange(S5_STATE, dtype=F32) + nrm((L, S5_GROUPS, S5_STATE), 0.01)
    s5_log_step = jax.random.uniform(next(keys), (L, S5_GROUPS), F32, math.log(S5_DT_MIN), math.log(S5_DT_MAX))
    s5_b_re = nrm((L, S5_GROUPS, S5_STATE, S5_GROUP), S5_GROUP ** -0.5)
    s5_b_im = nrm((L, S5_GROUPS, S5_STATE, S5_GROUP), S5_GROUP ** -0.5)
    s5_c_re = nrm((L, S5_GROUPS, S5_GROUP, S5_STATE), S5_STATE ** -0.5)
    s5_c_im = nrm((L, S5_GROUPS, S5_GROUP, S5_STATE), S5_STATE ** -0.5)
    s5_d = nrm((L, S5_WIDTH), 1.0)
    s5_w_glu = nrm((L, S5_WIDTH, S5_WIDTH), S5_WIDTH ** -0.5)
    s5_b_glu = nrm((L, S5_WIDTH), 0.01)
    mla_g_cq = gain((L, MLA_Q_RANK))
    mla_g_ckv = gain((L, MLA_KV_RANK))
    mla_w_uq = nrm((L, MLA_Q_RANK, MLA_HEADS * (MLA_NOPE + MLA_ROPE)), MLA_Q_RANK ** -0.5)
    mla_w_ukv = nrm((L, MLA_KV_RANK, MLA_HEADS * (MLA_NOPE + MLA_V)), MLA_KV_RANK ** -0.5)
    mla_g_qn = gain((L, MLA_NOPE + MLA_ROPE))
    mla_g_kn = gain((L, MLA_NOPE + MLA_ROPE))
    lru_conv_w = nrm((L, LRU_CONV, LRU_WIDTH), LRU_CONV ** -0.5)
    lru_conv_b = nrm((L, LRU_WIDTH), 0.01)
    lru_w_r = nrm((L, LRU_BLOCKS, LRU_BLOCK, LRU_BLOCK), LRU_BLOCK ** -0.5)
    lru_b_r = nrm((L, LRU_WIDTH), 0.01)
    lru_w_i = nrm((L, LRU_BLOCKS, LRU_BLOCK, LRU_BLOCK), LRU_BLOCK ** -0.5)
    lru_b_i = nrm((L, LRU_WIDTH), 0.01)
    a_c = jax.random.uniform(next(keys), (L, LRU_WIDTH), F32, 0.9, 0.999)
    a_base = a_c ** (1.0 / LRU_C)
    lru_lam = jnp.log(a_base) - jnp.log1p(-a_base)
    diff_g_qn = gain((L, DIFF_HEAD_DIM))
    diff_g_kn = gain((L, DIFF_HEAD_DIM))
    diff_lq1 = nrm((L, DIFF_HEAD_DIM), 0.1)
    diff_lk1 = nrm((L, DIFF_HEAD_DIM), 0.1)
    diff_lq2 = nrm((L, DIFF_HEAD_DIM), 0.1)
    diff_lk2 = nrm((L, DIFF_HEAD_DIM), 0.1)
    diff_g_sub = gain((L, DIFF_V))
    w_branch = nrm((L, N_BRANCH, BRANCH_WIDTH, D_MODEL), BRANCH_WIDTH ** -0.5)
    w_out = nrm((L, D_MODEL, D_MODEL), D_MODEL ** -0.5)
    x_wq = nrm((L, D_MODEL, CROSS_WIDTH), D_MODEL ** -0.5)
    x_wk = nrm((L, D_MODEL, CROSS_WIDTH), D_MODEL ** -0.5)
    x_wv = nrm((L, D_MODEL, CROSS_WIDTH), D_MODEL ** -0.5)
    x_wo = nrm((L, CROSS_WIDTH, D_MODEL), CROSS_WIDTH ** -0.5)
    x_g_qn = gain((L, CROSS_HEAD_DIM))
    x_g_kn = gain((L, CROSS_HEAD_DIM))
    ffn_w_gate = nrm((N_DENSE, D_MODEL, DENSE_FF), D_MODEL ** -0.5)
    ffn_w_up = nrm((N_DENSE, D_MODEL, DENSE_FF), D_MODEL ** -0.5)
    ffn_w_down = nrm((N_DENSE, DENSE_FF, D_MODEL), DENSE_FF ** -0.5)
    moe_w_router = nrm((N_MOE, D_MODEL, N_EXPERTS), D_MODEL ** -0.5)
    moe_w_gate = nrm((N_MOE, N_EXPERTS, D_MODEL, EXPERT_FF), D_MODEL ** -0.5)
    moe_w_up = nrm((N_MOE, N_EXPERTS, D_MODEL, EXPERT_FF), D_MODEL ** -0.5)
    moe_w_down = nrm((N_MOE, N_EXPERTS, EXPERT_FF, D_MODEL), EXPERT_FF ** -0.5)
    return {'x': x, 'mem': mem, 'positions': positions, 'rel_table': rel_table,
            'g_mix': g_mix, 'g_cross': g_cross, 'g_mem': g_mem, 'g_ffn': g_ffn,
            'w_in': w_in, 'b_gate': b_gate,
            's5_lam_re': s5_lam_re, 's5_lam_im': s5_lam_im, 's5_log_step': s5_log_step,
            's5_b_re': s5_b_re, 's5_b_im': s5_b_im, 's5_c_re': s5_c_re, 's5_c_im': s5_c_im,
            's5_d': s5_d, 's5_w_glu': s5_w_glu, 's5_b_glu': s5_b_glu,
            'mla_g_cq': mla_g_cq, 'mla_g_ckv': mla_g_ckv, 'mla_w_uq': mla_w_uq, 'mla_w_ukv': mla_w_ukv,
            'mla_g_qn': mla_g_qn, 'mla_g_kn': mla_g_kn,
            'lru_conv_w': lru_conv_w, 'lru_conv_b': lru_conv_b, 'lru_w_r': lru_w_r, 'lru_b_r': lru_b_r,
            'lru_w_i': lru_w_i, 'lru_b_i': lru_b_i, 'lru_lam': lru_lam,
            'diff_g_qn': diff_g_qn, 'diff_g_kn': diff_g_kn, 'diff_lq1': diff_lq1, 'diff_lk1': diff_lk1,
            'diff_lq2': diff_lq2, 'diff_lk2': diff_lk2, 'diff_g_sub': diff_g_sub,
            'w_branch': w_branch, 'w_out': w_out,
            'x_wq': x_wq, 'x_wk': x_wk, 'x_wv': x_wv, 'x_wo': x_wo, 'x_g_qn': x_g_qn, 'x_g_kn': x_g_kn,
            'ffn_w_gate': ffn_w_gate, 'ffn_w_up': ffn_w_up, 'ffn_w_down': ffn_w_down,
            'moe_w_router': moe_w_router, 'moe_w_gate': moe_w_gate, 'moe_w_up': moe_w_up,
            'moe_w_down': moe_w_down}


def reference(x, mem, positions, rel_table, g_mix, g_cross, g_mem, g_ffn, w_in, b_gate,
              s5_lam_re, s5_lam_im, s5_log_step, s5_b_re, s5_b_im, s5_c_re, s5_c_im,
              s5_d, s5_w_glu, s5_b_glu,
              mla_g_cq, mla_g_ckv, mla_w_uq, mla_w_ukv, mla_g_qn, mla_g_kn,
              lru_conv_w, lru_conv_b, lru_w_r, lru_b_r, lru_w_i, lru_b_i, lru_lam,
              diff_g_qn, diff_g_kn, diff_lq1, diff_lk1, diff_lq2, diff_lk2, diff_g_sub,
              w_branch, w_out,
              x_wq, x_wk, x_wv, x_wo, x_g_qn, x_g_kn,
              ffn_w_gate, ffn_w_up, ffn_w_down,
              moe_w_router, moe_w_gate, moe_w_up, moe_w_down):
    b, s, _ = x.shape
    split_points = _in_split_points()
    for l in range(DEPTH):
        h = rms_norm(x, g_mix[l])
        z = jnp.dot(h, w_in[l])
        (u_s5, c_q, c_kv, k_pe, x_lru, gate_lru, q_d, k_d, v_d, z_gate) = jnp.split(z, split_points, axis=-1)
        lam_init = 0.8 - 0.6 * math.exp(-0.3 * l)
        branches = (
            s5_branch(u_s5, s5_lam_re[l], s5_lam_im[l], s5_log_step[l], s5_b_re[l], s5_b_im[l],
                      s5_c_re[l], s5_c_im[l], s5_d[l], s5_w_glu[l], s5_b_glu[l]),
            mla_branch(c_q, c_kv, k_pe, positions, mla_g_cq[l], mla_g_ckv[l], mla_w_uq[l], mla_w_ukv[l],
                       mla_g_qn[l], mla_g_kn[l]),
            rglru_branch(x_lru, gate_lru, lru_conv_w[l], lru_conv_b[l], lru_w_r[l], lru_b_r[l],
                         lru_w_i[l], lru_b_i[l], lru_lam[l]),
            diff_branch(q_d, k_d, v_d, positions, rel_table, diff_g_qn[l], diff_g_kn[l], diff_lq1[l],
                        diff_lk1[l], diff_lq2[l], diff_lk2[l], diff_g_sub[l], lam_init),
        )
        z_gate = z_gate.reshape(b, s, N_BRANCH, D_MODEL)
        merged = sum(jax.nn.sigmoid(z_gate[:, :, n] + b_gate[l, n]) * jnp.dot(y, w_branch[l, n])
                     for n, y in enumerate(branches))
        x = x + jnp.dot(merged, w_out[l])
        x = x + cross_attention(rms_norm(x, g_cross[l]), rms_norm(mem, g_mem[l]),
                                x_wq[l], x_wk[l], x_wv[l], x_wo[l], x_g_qn[l], x_g_kn[l])
        hf = rms_norm(x, g_ffn[l])
        if l % 2 == 0:
            x = x + swiglu(hf, ffn_w_gate[l // 2], ffn_w_up[l // 2], ffn_w_down[l // 2])
        else:
            x = x + moe_swiglu(hf, moe_w_router[l // 2], moe_w_gate[l // 2], moe_w_up[l // 2], moe_w_down[l // 2])
    return x
```

```python
import contextlib
import numpy as np
import concourse.bass as bass
import concourse.mybir as mybir
from concourse.bass_utils import run_bass_kernel_spmd

F32 = mybir.dt.float32
BF16 = mybir.dt.bfloat16
I32 = mybir.dt.int32
AF = mybir.ActivationFunctionType
ALU = mybir.AluOpType
AX = mybir.AxisListType


def _box(ap):
    t = ap.tensor
    shp = list(t.shape)
    row = 1
    for s in shp[1:]:
        row *= s
    off = ap.offset
    p0 = off // row
    f0 = off % row
    dims = list(ap.ap)
    pstride, pcount = dims[0]
    if pstride == 0:
        pcount = 1
    ext = 0
    for st, cn in dims[1:]:
        ext += abs(st) * (cn - 1)
    return (p0, p0 + pcount, f0, f0 + ext + 1)


def _ov(a, b):
    return a[0] < b[1] and b[0] < a[1] and a[2] < b[3] and b[2] < a[3]


def _covers(a, b):
    return a[0] <= b[0] and a[1] >= b[1] and a[2] <= b[2] and a[3] >= b[3]


class KB:
    NDMA = 32

    def __init__(self):
        self.nc = bass.Bass("TRN2", target_bir_lowering=False)
        nc = self.nc
        self.es = contextlib.ExitStack()
        self.eng = {"pe": nc.tensor, "act": nc.scalar, "dve": nc.vector, "pool": nc.gpsimd, "sp": nc.sync}
        self.sem = {}
        self.cnt = {}
        for e in ("pe", "act", "dve", "pool"):
            self.sem[e] = self.es.enter_context(nc.semaphore("sem_" + e))
            self.cnt[e] = 0
        self.dsem = [self.es.enter_context(nc.semaphore(f"sem_dma{i}")) for i in range(self.NDMA)]
        for i, s in enumerate(self.dsem):
            self.sem[f"d{i}"] = s
            self.cnt[f"d{i}"] = 0
        self.sem["cc"] = self.es.enter_context(nc.semaphore("sem_cc"))
        self.cnt["cc"] = 0
        self.dnext = 0
        self.dnext_sw = 0
        self.waited = {e: {} for e in self.eng}
        self.rec = {}
        self.nins = {e: 0 for e in self.eng}
        self.same_engine_sync = {"dve": True, "pool": True, "act": True, "pe": False, "sp": True}

    sfx = ""

    def sb(self, name, shape, dtype, es=None):
        return (es or self.es).enter_context(self.nc.sbuf_tensor(name + self.sfx, list(shape), dtype))

    def ps(self, name, shape, dtype=F32, es=None):
        return (es or self.es).enter_context(self.nc.psum_tensor(name, list(shape), dtype))

    def dram(self, name, shape, dtype, kind):
        return self.nc.dram_tensor(name, list(shape), dtype, kind=kind).ap()

    def _tracked(self, ap):
        nm = type(ap.tensor).__name__
        return nm.startswith("SB") or nm.startswith("PSum")

    def _collect(self, reads, writes, extra_r=(), extra_w=()):
        deps = {}

        def add(d):
            for k, v in d.items():
                if deps.get(k, 0) < v:
                    deps[k] = v
        for ap in reads:
            if ap is None or not self._tracked(ap):
                continue
            b = _box(ap)
            for r in self.rec.get(ap.tensor.name, ()):
                if _ov(r[0], b):
                    add(r[1])
        for ap in writes:
            if ap is None or not self._tracked(ap):
                continue
            b = _box(ap)
            for r in self.rec.get(ap.tensor.name, ()):
                if _ov(r[0], b):
                    add(r[1])
                    add(r[2])
        for key in extra_r:
            for r in self.rec.get(key, ()):
                add(r[1])
        for key in extra_w:
            for r in self.rec.get(key, ()):
                add(r[1]); add(r[2])
        return deps

    def _emit_waits(self, e, deps, force=False):
        eng = self.eng[e]
        w = self.waited[e]
        for k, v in deps.items():
            if k == e and not force and not self.same_engine_sync.get(e, True):
                continue
            if w.get(k, 0) >= v:
                continue
            eng.wait_ge(self.sem[k], v)
            self.nins[e] += 1
            w[k] = v

    def _update(self, ev, reads, writes, extra_r=(), extra_w=()):
        k, v = ev
        for ap in reads:
            if ap is None or not self._tracked(ap):
                continue
            b = _box(ap)
            lst = self.rec.setdefault(ap.tensor.name, [])
            for r in lst:
                if _ov(r[0], b):
                    if r[2].get(k, 0) < v:
                        r[2][k] = v
        for ap in writes:
            if ap is None or not self._tracked(ap):
                continue
            b = _box(ap)
            lst = self.rec.setdefault(ap.tensor.name, [])
            new = [r for r in lst if not _covers(b, r[0])]
            new.append([b, {k: v}, {}])
            self.rec[ap.tensor.name] = new
        for key in extra_r:
            for r in self.rec.setdefault(key, []):
                if r[2].get(k, 0) < v:
                    r[2][k] = v
        for key in extra_w:
            self.rec[key] = [[(0, 1, 0, 1), {k: v}, {}]]

    def op(self, e, fn, reads, writes, extra_r=(), extra_w=()):
        deps = self._collect(reads, writes, extra_r, extra_w)
        self._emit_waits(e, deps)
        ins = fn()
        self.cnt[e] += 1
        ins.then_inc(self.sem[e], 1)
        self.nins[e] += 1
        ev = (e, self.cnt[e])
        self._update(ev, reads, writes, extra_r, extra_w)
        return ev

    def dma(self, q, out, in_, extra_r=(), extra_w=(), **kw):
        half = self.NDMA // 2
        if q == "pool":
            i = half + self.dnext_sw
            self.dnext_sw = (self.dnext_sw + 1) % (self.NDMA - half)
        else:
            i = self.dnext
            self.dnext = (self.dnext + 1) % half
        dk = f"d{i}"
        deps = self._collect([in_], [out], extra_r, extra_w)
        if self.cnt[dk] > 0:
            deps[dk] = max(deps.get(dk, 0), self.cnt[dk])
        self._emit_waits(q, deps)
        ins = self.eng[q].dma_start(out=out, in_=in_, **kw)
        self.cnt[dk] += 16
        ins.then_inc(self.sem[dk], 16)
        self.nins[q] += 1
        ev = (dk, self.cnt[dk])
        self._update(ev, [in_], [out], extra_r, extra_w)
        return ev

    def collective(self, kind, op, rg, src, dst, rkey, wkey):
        deps = self._collect([], [], extra_r=(rkey,), extra_w=(wkey,))
        self._emit_waits("pool", deps)
        ins = self.nc.gpsimd.collective_compute(kind, op, replica_groups=rg, ins=[src.ap().opt()], outs=[dst.ap().opt()])
        self.cnt["cc"] += 1
        ins.then_inc(self.sem["cc"], 1)
        self.nins["pool"] += 1
        self._update(("cc", self.cnt["cc"]), [], [], extra_r=(rkey,), extra_w=(wkey,))

    def barrier(self):
        deps = {k: v for k, v in self.cnt.items() if v > 0}
        for e in self.eng:
            self._emit_waits(e, dict(deps), force=True)
        self.rec = {}

    def wait_all(self, e="sp"):
        deps = {k: v for k, v in self.cnt.items() if v > 0}
        self._emit_waits(e, deps)

    def mm(self, out, lhsT, rhs, start=True, stop=True, **kw):
        return self.op("pe", lambda: self.nc.tensor.matmul(out, lhsT=lhsT, rhs=rhs, start=start, stop=stop, **kw),
                       [lhsT, rhs] + ([] if start else [out]), [out])

    def tr(self, out, in_, ident):
        return self.op("pe", lambda: self.nc.tensor.transpose(out, in_, ident), [in_, ident], [out])

    def act(self, out, in_, func, bias=0.0, scale=1.0, accum_out=None, e="act"):
        rd = [in_]
        kw = {}
        if not isinstance(bias, (int, float)):
            rd.append(bias)
        if not isinstance(scale, (int, float)):
            rd.append(scale)
        wr = [out]
        if accum_out is not None:
            wr.append(accum_out)
            kw["accum_out"] = accum_out
        return self.op("act", lambda: self.nc.scalar.activation(out=out, in_=in_, func=func, bias=bias, scale=scale, **kw), rd, wr)

    def tt(self, out, in0, in1, op, e="dve"):
        return self.op(e, lambda: self.eng[e].tensor_tensor(out=out, in0=in0, in1=in1, op=op), [in0, in1], [out])

    def ts(self, out, in0, s1, s2, op0, op1=None, e="dve", accum_out=None):
        rd = [in0]
        if not isinstance(s1, (int, float)):
            rd.append(s1)
        if s2 is not None and not isinstance(s2, (int, float)):
            rd.append(s2)
        kw = {}
        if op1 is not None:
            kw["op1"] = op1
        wr = [out]
        if accum_out is not None:
            kw["accum_out"] = accum_out
            wr.append(accum_out)
        return self.op(e, lambda: self.eng[e].tensor_scalar(out=out, in0=in0, scalar1=s1, scalar2=s2, op0=op0, **kw), rd, wr)

    def stt(self, out, in0, scalar, in1, op0, op1, e="dve"):
        rd = [in0, in1]
        if not isinstance(scalar, (int, float)):
            rd.append(scalar)
        return self.op(e, lambda: self.eng[e].scalar_tensor_tensor(out=out, in0=in0, scalar=scalar, in1=in1, op0=op0, op1=op1), rd, [out])

    def cp(self, out, in_, e="dve"):
        return self.op(e, lambda: self.eng[e].tensor_copy(out=out, in_=in_), [in_], [out])

    def memset(self, ap, val, e="dve"):
        return self.op(e, lambda: self.eng[e].memset(ap, val), [], [ap])

    def recip(self, out, in_):
        return self.op("dve", lambda: self.nc.vector.reciprocal(out=out, in_=in_), [in_], [out])

    def scan(self, out, d0, d1, initial, op0=ALU.mult, op1=ALU.add):
        rd = [d0, d1]
        if not isinstance(initial, (int, float)):
            rd.append(initial)
        return self.op("dve", lambda: self.nc.vector.tensor_tensor_scan(out=out, data0=d0, data1=d1, initial=initial, op0=op0, op1=op1), rd, [out])

    def run(self, in_maps, n=8, trace=False):
        res = run_bass_kernel_spmd(self.nc, in_maps, core_ids=list(range(n)), trace=trace)
        return res

import math

T = 4096
CH = 512
NCH = T // CH
PI = math.pi
EPS = 1e-6
NJ = 1151

PC_LAYOUT = [("gmix", 8), ("gcq", 2), ("gckv", 1), ("mgq", 1), ("mgk", 1), ("inv", 1), ("dgq", 1), ("dgk", 1),
             ("gsub", 1), ("tb31", 2), ("s5lr", 4), ("s5li", 4), ("s5ls", 4), ("s5d", 1),
             ("cw", 4), ("cb", 1), ("br", 1), ("bi", 1), ("llam", 1),
             ("lq1", 32), ("lk1", 32), ("lq2", 32), ("lk2", 32)]
PC = {}
_o = 0
for _n, _w in PC_LAYOUT:
    PC[_n] = (_o, _w)
    _o += _w
NPC = _o


def build_M(lam_init, debug=False, kb=None, sfx="", PS=None, fin=None, fout=None):
    standalone = kb is None
    if standalone:
        kb = KB()
    nc = kb.nc
    old_es = kb.es
    kb.es = contextlib.ExitStack()
    old_sfx = kb.sfx
    kb.sfx = sfx
    _dram = kb.dram
    class _K:
        pass
    def dram(name, shape, dt, kind):
        return _dram(name + sfx, shape, dt, kind)
    kb_dram = dram
    D = {}
    if fin is None:
        D["xT"] = kb_dram("xT", [1024, T], F32, "ExternalInput")
    D["wmix"] = kb_dram("wmix", [1024, 1120], F32, "ExternalInput")
    D["pcols"] = kb_dram("pcols", [128, NPC], F32, "ExternalInput")
    D["pos"] = kb_dram("pos", [1, T], I32, "ExternalInput")
    D["wuq"] = kb_dram("wuq", [192, 192], F32, "ExternalInput")
    D["wkn"] = kb_dram("wkn", [128, 128], F32, "ExternalInput")
    D["wvm"] = kb_dram("wvm", [128, 128], F32, "ExternalInput")
    D["rotT"] = kb_dram("rotT", [96, 96], F32, "ExternalInput")
    D["wr"] = kb_dram("wr", [128, 128], F32, "ExternalInput")
    D["wi"] = kb_dram("wi", [128, 128], F32, "ExternalInput")
    D["s5rep"] = kb_dram("s5rep", [32, 3, 512], F32, "ExternalInput")
    D["s5bT"] = kb_dram("s5bT", [32, 2, 512], F32, "ExternalInput")
    D["s5cT"] = kb_dram("s5cT", [128, 2, 4, 32], F32, "ExternalInput")
    D["oh"] = kb_dram("oh", [32, NJ], F32, "ExternalInput")
    D["maskrows"] = kb_dram("maskrows", [3, NJ], F32, "ExternalInput")
    D["relmy"] = kb_dram("relmy", [32, 2], F32, "ExternalInput")
    Y = kb_dram("Y", [4, 128, T], BF16, "ExternalOutput") if fout is None else None
    DBG = kb_dram("DBG", [4, 128, T], F32, "ExternalOutput") if debug else None
    RL = NJ + 128
    scratch = nc.dram_tensor("ebscratch" + sfx, [3, 128, RL], F32, kind="Internal")

    es = kb.es
    pc = kb.sb("pc", [128, NPC], F32)
    def col(name, i=0, rows=slice(0, 128)):
        o, w = PC[name]
        return pc[rows, o + i:o + i + 1]
    ones = kb.sb("ones", [128, 128], BF16)
    blk32 = kb.sb("blk32", [128, 128], BF16)
    uT = kb.sb("uT", [128, T], BF16)
    QT = kb.sb("QT", [96, 2, T], BF16)
    KT = kb.sb("KT", [96, 2, T], BF16)
    QdT = kb.sb("QdT", [128, T], BF16)
    KdT = kb.sb("KdT", [128, T], BF16)
    VA = kb.sb("VA", [128, 32, 2, 128], BF16)
    VD = kb.sb("VD", [128, 32, 2, 128], BF16)
    neglam = kb.sb("neglam", [128, 1], F32)
    if PS is None:
        PS = [kb.ps(f"ps{i}", [128, 512], F32, es=old_es) for i in range(8)]
    pctr = [0]
    pbanks = [list(range(8))]

    def pnext():
        b = pbanks[0]
        p = PS[b[pctr[0] % len(b)]]
        pctr[0] += 1
        return p

    kb.dma("sp", pc[:], D["pcols"][:, :])
    kb.memset(ones[:], 1.0)
    wtile = kb.sb("wtile", [128, 512], BF16)
    kb.memset(wtile[:], 1.0)

    def warm_pe(banks, n=24):
        for i in range(n):
            kb.mm(PS[banks[i % len(banks)]][:, :], ones[:, :], wtile[:, :])

    kb.memset(blk32[:], 0.0, e="pool")
    for g in range(4):
        kb.memset(blk32[32 * g:32 * g + 32, 32 * g:32 * g + 32], 1.0, e="pool")
    kb.memset(VA[:, :, :, 64:128], 1.0, e="pool")
    kb.memset(VD[:, :, :, 64:128], 1.0, e="pool")

    ystage = [kb.sb(f"yst{i}", [128, CH], BF16) for i in range(3)]
    ystage_a = [kb.sb(f"ysta{i}", [128, CH], BF16) for i in range(2)]
    yai = [0]

    def ynext_a():
        y = ystage_a[yai[0] % 2]
        yai[0] += 1
        return y
    ymk = [kb.sb(f"ymk{i}", [128, CH], BF16) for i in range(4)] if fout is not None else None
    BL = kb.sb("BL", [128, 2, 128], BF16)
    cTb = kb.sb("cTb", [128, 2, 4, 32], BF16)
    magp = kb.sb("magp", [128, 4], F32)
    thp = kb.sb("thp", [128, 4], F32)
    tmpi = [0]
    sqs = [kb.sb(f"sq{i}", [128, 512], BF16) for i in range(4)]
    lnvs = [kb.sb(f"lnv{i}", [128, 512], F32) for i in range(2)]
    rstds = [kb.sb(f"rstd{i}", [128, 512], F32) for i in range(2)]
    es_lru = contextlib.ExitStack()
    xlT = kb.sb("xlT", [128, T], F32, es=es_lru)
    ggT = kb.sb("ggT", [128, T], BF16, es=es_lru)

    def rms_T(srcs, gains, outs, onesT, nfeat, npo, lnbias=0.0, W=512):
        i0 = tmpi[0]
        tmpi[0] += 1
        ssp = pnext()
        n = len(srcs)
        for i, s in enumerate(srcs):
            p0, p1 = _box(s)[0], _box(s)[1]
            sq = sqs[(i0 * 2 + i) % 4]
            kb.act(sq[p0:p1, 0:W], s, AF.Square)
            kb.mm(ssp[0:npo, 0:W], onesT[i], sq[p0:p1, 0:W], start=(i == 0), stop=(i == n - 1))
        lnv = lnvs[i0 % 2]
        rstd = rstds[i0 % 2]
        kb.act(lnv[0:npo, 0:W], ssp[0:npo, 0:W], AF.Ln, bias=EPS, scale=1.0 / nfeat)
        kb.act(rstd[0:npo, 0:W], lnv[0:npo, 0:W], AF.Exp, scale=-0.5, bias=lnbias)
        for i, s in enumerate(srcs):
            p0, p1 = _box(s)[0], _box(s)[1]
            kb.stt(outs[i], s, gains[i], rstd[p0:p1, 0:W], ALU.mult, ALU.mult)

    def sincos(x, s_out, c_out, ki, r, h):
        kb.ts(ki, x, 1.0 / (2 * PI), None, ALU.mult)
        kb.stt(r, ki, -2 * PI, x, ALU.mult, ALU.add)
        kb.ts(r, r, PI, -PI, ALU.min, ALU.max)
        kb.act(s_out, r, AF.Sin)
        kb.act(h, r, AF.Sin, scale=0.5)
        kb.act(h, h, AF.Square)
        kb.act(c_out, h, AF.Identity, scale=-2.0, bias=1.0)

    with contextlib.ExitStack() as s0:
        ohs = kb.sb("ohs", [32, NJ], F32, es=s0)
        rel = kb.sb("rel", [32, 2], F32, es=s0)
        relb = kb.sb("relb", [32, 128], F32, es=s0)
        ebf = [kb.sb(f"ebf{i}", [128, NJ], F32, es=s0) for i in range(3)]
        msk = kb.sb("msk", [128, NJ], F32, es=s0)
        kb.dma("sp", ohs[:], D["oh"][:, :])
        kb.dma("sp", rel[:], D["relmy"][:, :])
        kb.dma("sp", msk[:], D["maskrows"][0:1, :].partition_broadcast(128))
        for r in range(3):
            if r < 2:
                kb.cp(relb[:, :], rel[:, r:r + 1].to_broadcast([32, 128]))
                for j0 in range(0, NJ, 512):
                    n = min(512, NJ - j0)
                    bp = pnext()
                    kb.mm(bp[:, 0:n], relb[:, :], ohs[:, j0:j0 + n])
                    kb.act(ebf[r][:, j0:j0 + n], bp[:, 0:n], AF.Exp)
                kb.tt(ebf[r][:, :], ebf[r][:, :], msk[:, :], ALU.mult)
                srcsb = ebf[r]
            else:
                srcsb = msk
            dst = bass.AP(tensor=scratch, offset=r * 128 * RL, ap=[[RL + 1, 128], [1, NJ]])
            kb.dma("sp", dst, srcsb[:, :])
        lt = kb.sb("lt", [128, 32], F32, es=s0)
        ssum = kb.sb("ssum", [128, 2], F32, es=s0)
        ee = kb.sb("ee", [128, 2], F32, es=s0)
        for i, (a, b) in enumerate((("lq1", "lk1"), ("lq2", "lk2"))):
            oa, ob = PC[a][0], PC[b][0]
            kb.tt(lt[:], pc[:, oa:oa + 32], pc[:, ob:ob + 32], ALU.mult)
            kb.op("dve", lambda i=i: nc.vector.reduce_sum(out=ssum[:, i:i + 1], in_=lt[:], axis=AX.X), [lt[:]], [ssum[:, i:i + 1]])
        kb.act(ee[:], ssum[:], AF.Exp)
        kb.tt(neglam[:], ee[:, 1:2], ee[:, 0:1], ALU.subtract)
        kb.ts(neglam[:], neglam[:], -lam_init, None, ALU.add)
        kb.barrier()

    with contextlib.ExitStack() as s1:
        wmixb = kb.sb("wmixb", [128, 8, 1120], BF16, es=s1)
        wuqb = kb.sb("wuqb", [128, 2, 192], BF16, es=s1)
        wknb = kb.sb("wknb", [128, 128], BF16, es=s1)
        wvmb = kb.sb("wvmb", [128, 128], BF16, es=s1)
        rotb = kb.sb("rotb", [96, 96], BF16, es=s1)
        xbuf = [kb.sb(f"xbuf{i}", [128, 8, CH], F32, es=s1) for i in range(1)]
        hT = kb.sb("hT", [128, 8, CH], BF16, es=s1)
        cqa = kb.sb("cqa", [128, CH], BF16, es=s1)
        cqb = kb.sb("cqb", [64, CH], BF16, es=s1)
        ckvn = kb.sb("ckvn", [128, CH], BF16, es=s1)
        qn = [kb.sb(f"qn{i}", [96, CH], BF16, es=s1) for i in range(2)]
        t1 = [kb.sb(f"rt1_{i}", [96, CH], F32, es=s1) for i in range(1)] * 2
        t2 = [kb.sb(f"rt2_{i}", [96, CH], F32, es=s1) for i in range(1)] * 2
        COSs = [kb.sb(f"COS{i}", [96, CH], BF16, es=s1) for i in range(2)]
        SINs = [kb.sb(f"SIN{i}", [96, CH], BF16, es=s1) for i in range(2)]
        posf = kb.sb("posf", [96, CH], F32, es=s1)
        tmpa = kb.sb("tmpa", [96, CH], F32, es=s1)
        tmpk = kb.sb("tmpk", [96, CH], I32, es=s1)
        tmpr = kb.sb("tmpr", [96, CH], F32, es=s1)
        tmph = tmpa
        posi = tmpk
        for i in range(2):
            kb.memset(COSs[i][0:64, :], 1.0, e="pool")
            kb.memset(SINs[i][0:64, :], 0.0, e="pool")
        wv = D["wmix"].rearrange("(kt p) n -> p kt n", p=128)
        for kt in range(8):
            kb.dma("pool", wmixb[:, kt, :], wv[:, kt, :])
        kb.dma("pool", wuqb[:, 0, :], D["wuq"][0:128, :])
        kb.dma("pool", wuqb[0:64, 1, :], D["wuq"][128:192, :])
        kb.dma("pool", wknb[:], D["wkn"][:, :])
        kb.dma("pool", wvmb[:], D["wvm"][:, :])
        kb.dma("pool", rotb[:], D["rotT"][:, :])
        xv = D["xT"].rearrange("(kt p) t -> p kt t", p=128) if fin is None else None
        hv = [fin[hh].rearrange("(kt p) t -> p kt t", p=128) for hh in range(2)] if fin is not None else None
        ri = [0]

        def proj(c0, n, outp):
            for kt in range(8):
                kb.mm(outp, wmixb[:, kt, c0:c0 + n], hT[:, kt, :], start=(kt == 0), stop=(kt == 7))

        def rope(src, dst, c):
            i = ri[0] % 2
            ri[0] += 1
            rp = pnext()
            kb.mm(rp[0:96, :], rotb[:, :], src)
            kb.tt(t1[i][:], src, COSs[c % 2][:, :], ALU.mult)
            kb.tt(t2[i][:], rp[0:96, :], SINs[c % 2][:, :], ALU.mult)
            kb.tt(dst, t1[i][:], t2[i][:], ALU.add, e="pool")

        warm_pe([0, 1, 2, 3, 4, 5, 6, 7])
        for c in range(NCH):
            cs = slice(c * CH, (c + 1) * CH)
            xc = xbuf[0]
            if fin is None:
                kb.dma("sp", xc[:], xv[:, :, cs])
            else:
                kb.dma("sp", hT[:], hv[c // 4][:, :, (c % 4) * CH:(c % 4 + 1) * CH], extra_r=("hdst",))
            kb.dma("sp", posi[64:96, :], D["pos"][0:1, cs].partition_broadcast(32))
            kb.cp(posf[64:96, :], posi[64:96, :])
            kb.ts(tmpa[64:96, :], posf[64:96, :], col("inv", 0, slice(64, 96)), None, ALU.mult)
            sincos(tmpa[64:96, :], SINs[c % 2][64:96, :], COSs[c % 2][64:96, :], tmpk[64:96, :], tmpr[64:96, :], tmph[64:96, :])
            if fin is None:
                rms_T([xc[:, kt, :] for kt in range(8)], [col("gmix", kt) for kt in range(8)],
                      [hT[:, kt, :] for kt in range(8)], [ones[:, :]] * 8, 1024, 128)
            p = pnext(); proj(0, 128, p[:, :]); kb.act(uT[:, cs], p[:, :], AF.Copy)
            pa = pnext(); proj(128, 128, pa[:, :])
            pb = pnext(); proj(256, 64, pb[0:64, :])
            rms_T([pa[:, :], pb[0:64, :]], [col("gcq", 0), col("gcq", 1, slice(0, 64))], [cqa[:], cqb[:]],
                  [ones[:, :], ones[0:64, :]], 192, 128)
            for h in range(2):
                qp = pnext()
                kb.mm(qp[0:96, :], wuqb[:, 0, 96 * h:96 * h + 96], cqa[:], start=True, stop=False)
                kb.mm(qp[0:96, :], wuqb[0:64, 1, 96 * h:96 * h + 96], cqb[:], start=False, stop=True)
                rms_T([qp[0:96, :]], [col("mgq", 0, slice(0, 96))], [qn[h][:]], [ones[0:96, 0:96]], 96, 96,
                      lnbias=math.log(96 ** -0.5))
                rope(qn[h][:], QT[:, h, cs], c)
            p = pnext(); proj(320, 128, p[:, :])
            rms_T([p[:, :]], [col("gckv", 0)], [ckvn[:]], [ones[:, :]], 128, 128)
            for h in range(2):
                kp = pnext()
                kb.mm(kp[0:64, :], wknb[:, 64 * h:64 * h + 64], ckvn[:])
                proj(448, 32, kp[64:96, :])
                rms_T([kp[0:96, :]], [col("mgk", 0, slice(0, 96))], [qn[h][:]], [ones[0:96, 0:96]], 96, 96)
                rope(qn[h][:], KT[:, h, cs], c)
            vp = pnext()
            for j in range(4):
                kb.mm(vp[:, j * 128:(j + 1) * 128], ckvn[:, j * 128:(j + 1) * 128], wvmb[:, :])
            kb.act(VA[:, 4 * c:4 * c + 4, :, 0:64], vp[:, :].rearrange("p (j h d) -> p j h d", j=4, h=2), AF.Copy)
            p = pnext(); proj(480, 128, p[:, :]); kb.act(xlT[:, cs], p[:, :], AF.Copy)
            p = pnext(); proj(608, 128, p[:, :]); kb.act(ggT[:, cs], p[:, :], AF.Gelu_apprx_tanh)
            p = pnext(); proj(736, 128, p[:, :])
            rms_T([p[:, :]], [col("dgq", 0)], [QdT[:, cs]], [blk32[:, :]], 32, 128, lnbias=math.log(32 ** -0.5))
            p = pnext(); proj(864, 128, p[:, :])
            rms_T([p[:, :]], [col("dgk", 0)], [KdT[:, cs]], [blk32[:, :]], 32, 128)
            vp = pnext()
            for j in range(4):
                for kt in range(8):
                    kb.mm(vp[:, j * 128:(j + 1) * 128], hT[:, kt, j * 128:(j + 1) * 128], wmixb[:, kt, 992:1120],
                          start=(kt == 0), stop=(kt == 7))
            kb.act(VD[:, 4 * c:4 * c + 4, :, 0:64], vp[:, :].rearrange("p (j h d) -> p j h d", j=4, h=2), AF.Copy)
        kb.barrier()

    yi = [0]

    ymi = [0]

    def yout(br, rs, cs, yo_ap):
        if fout is None:
            kb.dma("sp", Y[br, rs, cs], yo_ap)
            return
        q = cs.start // 2048
        tsl = slice(cs.start - 2048 * q, cs.stop - 2048 * q)
        for s in range(2):
            t = ymk[ymi[0] % 4]
            ymi[0] += 1
            kb.ts(t[rs, :], yo_ap, fout["meq"][rs, s:s + 1], None, ALU.mult)
            kb.dma("sp", fout["ysrc"].ap()[q, br, s, rs, tsl], t[rs, :], extra_w=("ysrc",))

    def ynext():
        y = ystage[yi[0] % 3]
        yi[0] += 1
        return y

    with contextlib.ExitStack() as s3:
        wrb = kb.sb("wrb", [128, 128], BF16, es=s3)
        wib = kb.sb("wib", [128, 128], BF16, es=s3)
        kb.dma("pool", wrb[:], D["wr"][:, :])
        kb.dma("pool", wib[:], D["wi"][:, :])
        xc = kb.sb("lxc", [128, T], F32, es=s3)
        xcb = kb.sb("lxcb", [128, T], BF16, es=s3)
        av = kb.sb("lav", [128, T], F32, es=s3)
        inp = kb.sb("linp", [128, T], F32, es=s3)
        lt = [kb.sb(f"lt{i}", [128, CH], F32, es=s3) for i in range(4)]
        spc = kb.sb("spc", [128, 1], F32, es=s3)
        kb.act(spc[:], col("llam", 0), AF.Exp, scale=-1.0)
        kb.act(spc[:], spc[:], AF.Ln, bias=1.0)
        kb.ts(spc[:], spc[:], -8.0, None, ALU.mult)
        kb.ts(xc[:], xlT[:], col("cw", 3), col("cb", 0), ALU.mult, ALU.add)
        for k in range(3):
            sh = 3 - k
            kb.stt(xc[:, sh:T], xlT[:, 0:T - sh], col("cw", k), xc[:, sh:T], ALU.mult, ALU.add)
        kb.act(xcb[:], xc[:], AF.Copy)
        for c in range(NCH):
            cs = slice(c * CH, (c + 1) * CH)
            rp = pnext(); ip = pnext()
            kb.mm(rp[:, :], wrb[:, :], xcb[:, cs])
            kb.mm(ip[:, :], wib[:, :], xcb[:, cs])
            kb.act(lt[0][:], rp[:, :], AF.Sigmoid, bias=col("br", 0))
            kb.act(lt[1][:], ip[:, :], AF.Sigmoid, bias=col("bi", 0))
            kb.act(av[:, cs], lt[0][:], AF.Exp, scale=spc[:, 0:1])
            kb.tt(lt[2][:], av[:, cs], av[:, cs], ALU.mult)
            kb.ts(lt[2][:], lt[2][:], -1.0, 1.0, ALU.mult, ALU.add)
            kb.act(lt[3][:], lt[2][:], AF.Sqrt)
            kb.tt(lt[1][:], lt[1][:], xc[:, cs], ALU.mult, e="pool")
            kb.tt(inp[:, cs], lt[3][:], lt[1][:], ALU.mult)
        for c in range(NCH):
            cs = slice(c * CH, (c + 1) * CH)
            init = 0.0 if c == 0 else inp[:, c * CH - 1:c * CH]
            kb.scan(inp[:, cs], av[:, cs], inp[:, cs], init)
            yo = ynext()
            kb.tt(yo[:], inp[:, cs], ggT[:, cs], ALU.mult, e="pool")
            yout(2, slice(0, 128), cs, yo[:])
        if debug:
            kb.dma("sp", DBG[0], xc[:]); kb.dma("sp", DBG[1], av[:]); kb.dma("sp", DBG[2], inp[:]); kb.dma("sp", DBG[3], xlT[:])
        kb.barrier()

    es_lru.close()

    with contextlib.ExitStack() as s2:
        rep = kb.sb("rep", [32, 3, 512], F32, es=s2)
        bT = kb.sb("bT", [32, 2, 512], F32, es=s2)
        kb.dma("sp", rep[:], D["s5rep"][:, :, :])
        kb.dma("sp", bT[:], D["s5bT"][:, :, :])
        kb.dma("pool", cTb[:], D["s5cT"][:, :, :, :])

        def derived(lr, li, ls, shape, nm, est, keep):
            names = ["dt", "mag", "ang", "sn", "cs", "tmp", "tmp2", "ar1", "ai", "den", "zr", "zi", "th"]
            tt_ = {}
            for n in names:
                if n in keep:
                    tt_[n] = kb.sb(f"{nm}_{n}", shape, F32, es=s2)
            for n in names:
                if n not in keep:
                    tt_[n] = kb.sb(f"{nm}_{n}", shape, F32, es=est)
            dt = tt_["dt"]; mag = tt_["mag"]; ang = tt_["ang"]; sn = tt_["sn"]; cs_ = tt_["cs"]; tmp = tt_["tmp"]; tmp2 = tt_["tmp2"]
            ar1 = tt_["ar1"]; ai = tt_["ai"]; den = tt_["den"]; zr = tt_["zr"]; zi = tt_["zi"]; th = tt_["th"]
            kb.act(dt[:], ls, AF.Exp)
            kb.tt(tmp[:], lr, dt[:], ALU.mult)
            kb.act(mag[:], tmp[:], AF.Exp)
            kb.tt(ang[:], li, dt[:], ALU.mult)
            kii = kb.sb(f"{nm}_kii", shape, I32, es=est)
            sincos(ang[:], sn[:], cs_[:], kii[:], th[:], tmp[:])
            kb.tt(ai[:], mag[:], sn[:], ALU.mult)
            kb.tt(ar1[:], mag[:], cs_[:], ALU.mult)
            kb.ts(ar1[:], ar1[:], -1.0, None, ALU.add)
            kb.tt(den[:], lr, lr, ALU.mult)
            kb.tt(tmp[:], li, li, ALU.mult)
            kb.tt(den[:], den[:], tmp[:], ALU.add)
            kb.recip(den[:], den[:])
            kb.tt(tmp[:], ar1[:], lr, ALU.mult)
            kb.tt(tmp2[:], ai[:], li, ALU.mult)
            kb.tt(tmp[:], tmp[:], tmp2[:], ALU.add)
            kb.tt(zr[:], tmp[:], den[:], ALU.mult)
            kb.tt(tmp[:], ai[:], lr, ALU.mult)
            kb.tt(tmp2[:], ar1[:], li, ALU.mult)
            kb.tt(tmp[:], tmp[:], tmp2[:], ALU.subtract)
            kb.tt(zi[:], tmp[:], den[:], ALU.mult)
            return dict(mag=mag, th=th, zr=zr, zi=zi)

        o_lr, o_li, o_ls = PC["s5lr"][0], PC["s5li"][0], PC["s5ls"][0]
        dc = derived(pc[:, o_lr:o_lr + 4], pc[:, o_li:o_li + 4], pc[:, o_ls:o_ls + 4], [128, 4], "c4", s2,
                     ("mag", "th", "zr", "zi"))
        bbr = kb.sb("bbr", [32, 512], F32, es=s2)
        bbi = kb.sb("bbi", [32, 512], F32, es=s2)
        s2t = contextlib.ExitStack()
        dr = derived(rep[:, 0, :], rep[:, 1, :], rep[:, 2, :], [32, 512], "r32", s2t, ())
        tb = kb.sb("tb", [32, 512], F32, es=s2t)
        kb.tt(bbr[:], dr["zr"][:], bT[:, 0, :], ALU.mult)
        kb.tt(tb[:], dr["zi"][:], bT[:, 1, :], ALU.mult)
        kb.tt(bbr[:], bbr[:], tb[:], ALU.subtract)
        kb.tt(bbi[:], dr["zr"][:], bT[:, 1, :], ALU.mult)
        kb.tt(tb[:], dr["zi"][:], bT[:, 0, :], ALU.mult)
        kb.tt(bbi[:], bbi[:], tb[:], ALU.add)
        kb.barrier()
        s2t.close()
        for st in range(4):
            kb.act(BL[32 * st:32 * st + 32, 0, :], bbr[:, st * 128:(st + 1) * 128], AF.Copy)
            kb.act(BL[32 * st:32 * st + 32, 1, :], bbi[:, st * 128:(st + 1) * 128], AF.Copy)
        kb.cp(magp[:], dc["mag"][:])
        kb.cp(thp[:], dc["th"][:])
        kb.barrier()

    EB = kb.sb("EB", [128, 3, 5, 512], BF16)
    for r in range(3):
        src = bass.AP(tensor=scratch, offset=r * 128 * RL + 127, ap=[[RL, 128], [128, 5], [1, 512]])
        kb.dma("pool", EB[:, r, :, :], src)
    pbanks[0] = [0, 1, 6]
    Qz = [kb.sb(f"Qz{i}", [128, CH], BF16) for i in range(8)]
    for _q in Qz:
        kb.memset(_q[:], 0.0, e="pool")
    Pt = [kb.sb(f"Pt{i}", [128, CH], BF16) for i in range(4)]
    pi_ = [0]
    fin = [kb.sb(f"fin{i}", [128, CH], F32) for i in range(6)]
    SB_ = [PS[0], PS[1], PS[6]]
    sctr = [0]

    def snext():
        p = SB_[sctr[0] % 3]
        sctr[0] += 1
        return p

    def attn_gen():
        LOOK = 2
        for qc in range(NCH):
            qs = slice(qc * CH, (qc + 1) * CH)
            nk = 4 * qc + 4
            OA = [PS[4], PS[5]]
            steps = [(kt, h) for kt in range(nk) for h in range(2)]
            spt = {}

            def qk_mla(i):
                kt, h = steps[i]
                sp_ = snext()
                kb.mm(sp_[:, :], KT[:, h, kt * 128:(kt + 1) * 128], QT[:, h, qs])
                spt[i] = sp_
            for i in range(min(LOOK, len(steps))):
                qk_mla(i)
            for i, (kt, h) in enumerate(steps):
                sp_ = spt.pop(i)
                P = Pt[pi_[0] % 4]; pi_[0] += 1
                kb.act(P[:], sp_[:, :], AF.Exp)
                v = kt - 4 * qc + 1
                if v >= 1:
                    kb.tt(P[:], P[:], EB[:, 2, 4 - v, :], ALU.mult)
                if i + LOOK < len(steps):
                    qk_mla(i + LOOK)
                kb.mm(OA[h][:, :], VA[:, kt, h, :], P[:], start=(kt == 0), stop=(kt == nk - 1))
                yield
            yo = ynext_a()
            for h in range(2):
                rd = fin[h]
                kb.act(rd[0:64, :], OA[h][64:128, :], AF.Ln)
                kb.act(rd[0:64, :], rd[0:64, :], AF.Exp, scale=-1.0)
                kb.tt(yo[64 * h:64 * h + 64, :], OA[h][0:64, :], rd[0:64, :], ALU.mult)
            yout(1, slice(0, 128), qs, yo[:])
            qz = Qz[4 * (qc % 2):4 * (qc % 2) + 4]
            for _i in range(4):
                kb.act(qz[_i][32 * _i:32 * _i + 32, :], QdT[32 * _i:32 * _i + 32, qs], AF.Copy)
            yo = ynext_a()
            for h in range(2):
                OD = [PS[4], PS[5]]
                steps = [(kt, s) for kt in range(nk) for s in range(2)]
                spt = {}

                def qk_d(i):
                    kt, s = steps[i]
                    r0 = 64 * h + 32 * s
                    sp_ = snext()
                    kb.mm(sp_[:, :], KdT[:, kt * 128:(kt + 1) * 128], qz[2 * h + s][:, :])
                    spt[i] = sp_
                for i in range(min(LOOK, len(steps))):
                    qk_d(i)
                for i, (kt, s) in enumerate(steps):
                    v = kt - 4 * qc + 1
                    sp_ = spt.pop(i)
                    P = Pt[pi_[0] % 4]; pi_[0] += 1
                    if v >= 0:
                        kb.act(P[:], sp_[:, :], AF.Exp)
                        kb.tt(P[:], P[:], EB[:, h, 4 - v, :], ALU.mult)
                    else:
                        kb.act(P[:], sp_[:, :], AF.Exp, bias=col("tb31", h))
                    if i + LOOK < len(steps):
                        qk_d(i + LOOK)
                    kb.mm(OD[s][:, :], VD[:, kt, h, :], P[:], start=(kt == 0), stop=(kt == nk - 1))
                    yield
                r1, r2, o1, o2, dd = fin[0], fin[1], fin[2], fin[3], fin[4]
                kb.act(r1[0:64, :], OD[0][64:128, :], AF.Ln)
                kb.act(r1[0:64, :], r1[0:64, :], AF.Exp, scale=-1.0)
                kb.act(r2[0:64, :], OD[1][64:128, :], AF.Ln)
                kb.act(r2[0:64, :], r2[0:64, :], AF.Exp, scale=-1.0)
                kb.tt(o1[0:64, :], OD[0][0:64, :], r1[0:64, :], ALU.mult)
                kb.tt(o2[0:64, :], OD[1][0:64, :], r2[0:64, :], ALU.mult)
                kb.stt(dd[0:64, :], o2[0:64, :], neglam[0:64, 0:1], o1[0:64, :], ALU.mult, ALU.add)
                rms_T([dd[0:64, :]], [col("gsub", 0, slice(0, 64))], [yo[64 * h:64 * h + 64, :]], [ones[0:64, 0:64]], 64, 64,
                      lnbias=math.log(1.0 - lam_init))
            yout(3, slice(0, 128), qs, yo[:])

    p5c = [0]

    def p5next():
        p = PS[2 + (p5c[0] % 2)]
        p5c[0] += 1
        return p

    s2 = contextlib.ExitStack()
    if True:
        iot = kb.sb("iot", [128, CH], F32, es=s2)
        kb.op("pool", lambda: nc.gpsimd.iota(iot[:], pattern=[[1, CH]], base=0, channel_multiplier=0,
                                               allow_small_or_imprecise_dtypes=True), [], [iot[:]])
        basec = kb.sb("basec", [128, 32], F32, es=s2)
        ph0 = [kb.sb(f"ph0_{i}", [128, CH], F32, es=s2) for i in range(2)]
        NB = 2
        cosT = [kb.sb(f"cosT{i}", [128, CH], F32, es=s2) for i in range(NB)]
        sinT = [kb.sb(f"sinT{i}", [128, CH], F32, es=s2) for i in range(NB)]
        pr = [kb.sb(f"pr{i}", [128, CH], F32, es=s2) for i in range(NB)]
        pim = [kb.sb(f"pim{i}", [128, CH], F32, es=s2) for i in range(NB)]
        m = [kb.sb(f"s5m{i}", [128, CH], F32, es=s2) for i in range(4)]
        hrb = [kb.sb(f"hrb{i}", [128, CH], BF16, es=s2) for i in range(2)]
        hib = [kb.sb(f"hib{i}", [128, CH], BF16, es=s2) for i in range(2)]
        ytm = [kb.sb(f"ytm{i}", [128, CH], F32, es=s2) for i in range(2)]
        cmul = kb.sb("cmul", [128, 8], F32, es=s2)
        bx = kb.sb("bx", [128, 32], F32, es=s2)
        bki = kb.sb("bki", [128, 32], I32, es=s2)
        kb.op("pool", lambda: nc.gpsimd.iota(cmul[:], pattern=[[CH, 8]], base=0, channel_multiplier=0,
                                               allow_small_or_imprecise_dtypes=True), [], [cmul[:]])
        for st in range(4):
            kb.ts(bx[:, st * 8:st * 8 + 8], cmul[:], thp[:, st:st + 1], None, ALU.mult)
        kb.ts(bki[:], bx[:], 1.0 / (2 * PI), None, ALU.mult)
        kb.stt(basec[:], bki[:], -2 * PI, bx[:], ALU.mult, ALU.add)
        ski = [kb.sb(f"ski{i}", [128, CH], I32, es=s2) for i in range(2)]
        shh = [kb.sb(f"shh{i}", [128, CH], F32, es=s2) for i in range(2)]
        def s5_gen():
            its = [(st, c) for st in range(4) for c in range(NCH)]
            PRE, PIE, YP = PS[2], PS[3], PS[7]

            def tables(i):
                st, c = its[i]
                b = i % NB
                kb.ts(ph0[b][:], iot[:, :], thp[:, st:st + 1], basec[:, st * 8 + c:st * 8 + c + 1], ALU.mult, ALU.add)
                sincos(ph0[b][:], sinT[b][:], cosT[b][:], ski[b][:], ph0[b][:], shh[b][:])

            def bmm(i):
                st, c = its[i]
                cs = slice(c * CH, (c + 1) * CH)
                kb.mm(PRE[:, :], BL[32 * st:32 * st + 32, 0, :], uT[32 * st:32 * st + 32, cs], tile_position=(32 * st, 0))
                kb.mm(PIE[:, :], BL[32 * st:32 * st + 32, 1, :], uT[32 * st:32 * st + 32, cs], tile_position=(32 * st, 0))
            tables(0)
            bmm(0)
            for i, (st, c) in enumerate(its):
                cs = slice(c * CH, (c + 1) * CH)
                b = i % NB
                pb = (i - 1) % NB
                if i + 1 < len(its):
                    tables(i + 1)
                yield
                kb.tt(m[0][:], PRE[:, :], cosT[b][:], ALU.mult)
                kb.tt(m[1][:], PIE[:, :], sinT[b][:], ALU.mult)
                kb.tt(pr[b][:], m[0][:], m[1][:], ALU.add)
                yield
                kb.tt(m[2][:], PIE[:, :], cosT[b][:], ALU.mult)
                kb.tt(m[3][:], PRE[:, :], sinT[b][:], ALU.mult)
                kb.tt(pim[b][:], m[2][:], m[3][:], ALU.subtract)
                if i + 1 < len(its):
                    bmm(i + 1)
                yield
                for buf in (pr, pim):
                    init = 0.0 if c == 0 else buf[pb][:, CH - 1:CH]
                    kb.scan(buf[b][:], magp[:, st:st + 1].to_broadcast([128, CH]), buf[b][:], init)
                yield
                kb.tt(m[0][:], pr[b][:], cosT[b][:], ALU.mult)
                kb.tt(m[1][:], pim[b][:], sinT[b][:], ALU.mult)
                kb.tt(hrb[c % 2][:], m[0][:], m[1][:], ALU.subtract)
                yield
                kb.tt(m[2][:], pr[b][:], sinT[b][:], ALU.mult)
                kb.tt(m[3][:], pim[b][:], cosT[b][:], ALU.mult)
                kb.stt(hib[c % 2][:], m[2][:], -1.0, m[3][:], ALU.mult, ALU.subtract)
                kb.mm(YP[0:32, :], cTb[:, 0, st, :], hrb[c % 2][:], start=True, stop=False)
                kb.mm(YP[0:32, :], cTb[:, 1, st, :], hib[c % 2][:], start=False, stop=True)
                yield
                rs = slice(32 * st, 32 * st + 32)
                yt = ytm[c % 2]
                kb.act(yt[rs, :], YP[0:32, :], AF.Copy)
                kb.stt(yt[rs, :], uT[rs, cs], col("s5d", 0, rs), yt[rs, :], ALU.mult, ALU.add)
                yo = ynext()
                kb.act(yo[rs, :], yt[rs, :], AF.Gelu_apprx_tanh)
                yout(0, rs, cs, yo[rs, :])
                yield

    warm_pe([4, 5])
    g5 = s5_gen()
    ga = attn_gen()
    n_att = sum((4 * qc + 4) * 6 for qc in range(NCH))
    per = max(1, n_att // (32 * 7))
    done5 = False
    donea = False
    while not (done5 and donea):
        if not done5:
            try:
                next(g5)
            except StopIteration:
                done5 = True
        for _ in range(per if not done5 else 10 ** 9):
            try:
                next(ga)
            except StopIteration:
                donea = True
                break
    kb.barrier()
    s2.close()
    kb.barrier()
    kb.es.close()
    kb.es = old_es
    kb.sfx = old_sfx
    if standalone:
        kb.wait_all("sp")
    return kb

import math

TO = 2048
CH = 512
NC4 = TO // CH
EPS = 1e-6
NMEM = 256

P2_LAYOUT = [("gmix", 8), ("gcross", 8), ("gmem", 8), ("gffn", 8), ("bgate", 32), ("bglu", 2), ("xgq", 1), ("xgk", 1)]
P2 = {}
_o = 0
for _n, _w in P2_LAYOUT:
    P2[_n] = (_o, _w)
    _o += _w
NP2 = _o


def build_R(moe, debug=False, kb=None, sfx="", PS=None, xin=None, yin=None, xout=None, hout=None):
    standalone = kb is None
    if standalone:
        kb = KB()
    nc = kb.nc
    old_es = kb.es
    kb.es = contextlib.ExitStack()
    old_sfx = kb.sfx
    kb.sfx = sfx
    _dram = kb.dram
    def kb_dram(name, shape, dt, kind):
        return _dram(name + sfx, shape, dt, kind)
    D = {}
    D["xT"] = kb_dram("xT", [1024, TO], F32, "ExternalInput") if xin is None else xin
    D["yall"] = kb_dram("yall", [4, 256, TO], BF16, "ExternalInput") if yin is None else yin
    D["wg8"] = kb_dram("wg8", [8, 1024, 512], F32, "ExternalInput")
    D["wbr"] = kb_dram("wbr", [4, 256, 1024], F32, "ExternalInput")
    D["wout"] = kb_dram("wout", [1024, 1024], F32, "ExternalInput")
    D["pc2"] = kb_dram("pc2", [128, NP2], F32, "ExternalInput")
    D["wglu"] = kb_dram("wglu", [256, 256], F32, "ExternalInput")
    D["xwq"] = kb_dram("xwq", [1024, 256], F32, "ExternalInput")
    D["xwk"] = kb_dram("xwk", [1024, 256], F32, "ExternalInput")
    D["xwv"] = kb_dram("xwv", [1024, 256], F32, "ExternalInput")
    D["xwo"] = kb_dram("xwo", [256, 1024], F32, "ExternalInput")
    D["memT"] = kb_dram("memT", [1024, NMEM], F32, "ExternalInput")
    if moe:
        NE, FF = 8, 3584
        D["wr"] = kb_dram("wrt", [1024, 8], F32, "ExternalInput")
    else:
        NE, FF = 1, 2816
    D["fg"] = kb_dram("fg", [NE, 1024, FF], F32, "ExternalInput")
    D["fu"] = kb_dram("fu", [NE, 1024, FF], F32, "ExternalInput")
    D["fd"] = kb_dram("fd", [NE, FF, 1024], F32, "ExternalInput")
    XO = kb_dram("xo", [1024, TO], F32, "ExternalOutput") if xout is None else xout
    NFT = FF // 128

    pc = kb.sb("pc", [128, NP2], F32)

    def col(name, i=0, rows=slice(0, 128)):
        o, w = P2[name]
        return pc[rows, o + i:o + i + 1]
    ones = kb.sb("ones", [128, 128], BF16)
    blk64 = kb.sb("blk64", [128, 128], BF16)
    xT = kb.sb("xTs", [128, 8, TO], F32)
    if PS is None:
        PS = [kb.ps(f"ps{i}", [128, 512], F32, es=old_es) for i in range(8)]
    pctr = [0]
    pbanks = [list(range(8))]

    def pnext():
        b = pbanks[0]
        p = PS[b[pctr[0] % len(b)]]
        pctr[0] += 1
        return p
    kb.dma("sp", pc[:], D["pc2"][:, :])
    kb.memset(ones[:], 1.0)
    kb.memset(blk64[:], 0.0, e="pool")
    for g in range(2):
        kb.memset(blk64[64 * g:64 * g + 64, 64 * g:64 * g + 64], 1.0, e="pool")
    xv = D["xT"].rearrange("(kt p) t -> p kt t", p=128)
    for c in range(NC4):
        kb.dma("sp", xT[:, :, c * CH:(c + 1) * CH], xv[:, :, c * CH:(c + 1) * CH], extra_r=("xsp",))

    tmpi = [0]
    sqs = [kb.sb(f"sq{i}", [128, 512], BF16) for i in range(4)]
    lnvs = [kb.sb(f"lnv{i}", [128, 512], F32) for i in range(2)]
    rstds = [kb.sb(f"rstd{i}", [128, 512], F32) for i in range(2)]

    def rms_T(srcs, gains, outs, onesT, nfeat, npo, lnbias=0.0, W=512, rstd_out=None):
        i0 = tmpi[0]
        tmpi[0] += 1
        ssp = pnext()
        n = len(srcs)
        for i, s in enumerate(srcs):
            p0, p1 = _box(s)[0], _box(s)[1]
            sq = sqs[(i0 * 2 + i) % 4]
            kb.act(sq[p0:p1, 0:W], s, AF.Square)
            kb.mm(ssp[0:npo, 0:W], onesT[i], sq[p0:p1, 0:W], start=(i == 0), stop=(i == n - 1))
        lnv = lnvs[i0 % 2]
        rstd = rstds[i0 % 2]
        kb.act(lnv[0:npo, 0:W], ssp[0:npo, 0:W], AF.Ln, bias=EPS, scale=1.0 / nfeat)
        kb.act(rstd[0:npo, 0:W], lnv[0:npo, 0:W], AF.Exp, scale=-0.5, bias=lnbias)
        if rstd_out is not None:
            kb.act(rstd_out, rstd[0:1, 0:W], AF.Copy)
        for i, s in enumerate(srcs):
            p0, p1 = _box(s)[0], _box(s)[1]
            kb.stt(outs[i], s, gains[i], rstd[p0:p1, 0:W], ALU.mult, ALU.mult)

    def norm_x(hT, gname, rstd_row=None):
        for c in range(NC4):
            cs = slice(c * CH, (c + 1) * CH)
            rms_T([xT[:, kt, cs] for kt in range(8)], [col(gname, kt) for kt in range(8)],
                  [hT[:, kt, cs] for kt in range(8)], [ones[:, :]] * 8, 1024, 128,
                  rstd_out=(None if rstd_row is None else rstd_row[32 * c:32 * c + 1, :]))

    def resid_add(ot, cs, ps_ap):
        kb.tt(xT[:, ot, cs], xT[:, ot, cs], ps_ap, ALU.add)

    with contextlib.ExitStack() as e1:
        merged = kb.sb("merged", [128, 8, TO], BF16, es=e1)
        with contextlib.ExitStack() as e2:
            hT = kb.sb("hT", [128, 8, TO], BF16, es=e2)
            yb = kb.sb("yb", [128, 4, 2, TO], BF16, es=e2)
            wbrb = kb.sb("wbrb", [128, 4, 2, 1024], BF16, es=e2)
            wglub = kb.sb("wglub", [128, 2, 256], BF16, es=e2)
            wgb = [kb.sb(f"wgb{i}", [128, 8, 512], BF16, es=e2) for i in range(1)] * 2
            sg = [kb.sb(f"sg{i}", [128, CH], F32, es=e2) for i in range(2)] + [None]
            sg[2] = sg[0]
            tmpm = [kb.sb(f"tmpm{i}", [128, CH], F32, es=e2) for i in range(1)] * 2
            accm = [kb.sb(f"accm{i}", [128, CH], F32, es=e2) for i in range(2)]
            for b in range(4):
                for k2 in range(2):
                    kb.dma("sp", yb[:, b, k2, :], D["yall"][b, 128 * k2:128 * k2 + 128, :], extra_r=("ydst",))
                    kb.dma("pool", wbrb[:, b, k2, :], D["wbr"][b, 128 * k2:128 * k2 + 128, :])
            for k2 in range(2):
                kb.dma("pool", wglub[:, k2, :], D["wglu"][128 * k2:128 * k2 + 128, :])
            norm_x(hT, "gmix")
            for c in range(NC4):
                cs = slice(c * CH, (c + 1) * CH)
                gl = []
                for j in range(2):
                    gp = pnext()
                    for k2 in range(2):
                        kb.mm(gp[:, :], wglub[:, k2, 128 * j:128 * j + 128], yb[:, 0, k2, cs], start=(k2 == 0), stop=(k2 == 1))
                    kb.act(sg[j][:], gp[:, :], AF.Sigmoid, bias=col("bglu", j))
                for j in range(2):
                    kb.tt(yb[:, 0, j, cs], yb[:, 0, j, cs], sg[j][:], ALU.mult)
            gv = D["wg8"].rearrange("d (kt p) n -> d p kt n", p=128)
            si = 0
            for dt in range(8):
                wg = wgb[dt % 2]
                kb.dma("pool", wg[:], gv[dt])
                for c in range(NC4):
                    cs = slice(c * CH, (c + 1) * CH)
                    acc = accm[(dt * NC4 + c) % 2]
                    for b in range(4):
                        up = pnext()
                        for k2 in range(2):
                            kb.mm(up[:, :], wbrb[:, b, k2, 128 * dt:128 * dt + 128], yb[:, b, k2, cs], start=(k2 == 0), stop=(k2 == 1))
                        gp = pnext()
                        for kt in range(8):
                            kb.mm(gp[:, :], wg[:, kt, 128 * b:128 * b + 128], hT[:, kt, cs], start=(kt == 0), stop=(kt == 7))
                        s = sg[si % 2]
                        si += 1
                        kb.act(s[:], gp[:, :], AF.Sigmoid, bias=col("bgate", b * 8 + dt))
                        if b == 0:
                            kb.tt(acc[:], s[:], up[:, :], ALU.mult)
                        elif b < 3:
                            t = tmpm[b % 2]
                            kb.tt(t[:], s[:], up[:, :], ALU.mult)
                            kb.tt(acc[:], acc[:], t[:], ALU.add, e="pool")
                        else:
                            t = tmpm[b % 2]
                            kb.tt(t[:], s[:], up[:, :], ALU.mult)
                            kb.tt(merged[:, dt, cs], acc[:], t[:], ALU.add, e="pool")
            kb.barrier()
        woutb = kb.sb("woutb", [128, 8, 1024], BF16, es=e1)
        kb.dma("pool", woutb[:], D["wout"].rearrange("(kt p) n -> p kt n", p=128))
        for ot in range(8):
            for c in range(NC4):
                cs = slice(c * CH, (c + 1) * CH)
                op_ = pnext()
                for kt in range(8):
                    kb.mm(op_[:, :], woutb[:, kt, 128 * ot:128 * ot + 128], merged[:, kt, cs], start=(kt == 0), stop=(kt == 7))
                resid_add(ot, cs, op_[:, :])
        kb.barrier()
    if debug:
        DBG = kb_dram("dbgxa", [1024, TO], F32, "ExternalOutput")
        kb.dma("sp", DBG.rearrange("(kt p) t -> p kt t", p=128), xT[:])

    with contextlib.ExitStack() as e1:
        hT = kb.sb("hTc", [128, 8, TO], BF16, es=e1)
        wq = kb.sb("wq", [128, 8, 256], BF16, es=e1)
        wk = kb.sb("wk", [128, 8, 256], BF16, es=e1)
        wvv = kb.sb("wvv", [128, 8, 256], BF16, es=e1)
        wo = kb.sb("wo", [128, 2, 1024], BF16, es=e1)
        mT = kb.sb("mT", [128, 8, NMEM], F32, es=e1)
        mh = kb.sb("mh", [128, 8, NMEM], BF16, es=e1)
        QcT = kb.sb("QcT", [128, 2, TO], BF16, es=e1)
        KcT = kb.sb("KcT", [128, 2, NMEM], BF16, es=e1)
        VC = kb.sb("VC", [128, 2, 4, 128], BF16, es=e1)
        OcT = kb.sb("OcT", [128, 2, TO], BF16, es=e1)
        Pt = [kb.sb(f"Pt{i}", [128, CH], BF16, es=e1) for i in range(4)]
        rdt = [kb.sb(f"rdt{i}", [128, CH], F32, es=e1) for i in range(2)]
        for nm, t in (("xwq", wq), ("xwk", wk), ("xwv", wvv)):
            kb.dma("pool", t[:], D[nm].rearrange("(kt p) n -> p kt n", p=128))
        kb.dma("pool", wo[:], D["xwo"].rearrange("(kt p) n -> p kt n", p=128))
        kb.dma("sp", mT[:], D["memT"].rearrange("(kt p) t -> p kt t", p=128))
        kb.memset(VC[:, :, :, 64:128], 1.0, e="pool")
        rms_T([mT[:, kt, :] for kt in range(8)], [col("gmem", kt) for kt in range(8)], [mh[:, kt, :] for kt in range(8)],
              [ones[:, :]] * 8, 1024, 128, W=NMEM)
        for j in range(2):
            kp = pnext()
            for kt in range(8):
                kb.mm(kp[:, 0:NMEM], wk[:, kt, 128 * j:128 * j + 128], mh[:, kt, :], start=(kt == 0), stop=(kt == 7))
            rms_T([kp[:, 0:NMEM]], [col("xgk", 0)], [KcT[:, j, :]], [blk64[:, :]], 64, 128, W=NMEM)
        for mt in range(2):
            vp = pnext()
            for kt in range(8):
                kb.mm(vp[:, 0:256], mh[:, kt, 128 * mt:128 * mt + 128], wvv[:, kt, :], start=(kt == 0), stop=(kt == 7))
            kb.act(VC[:, mt, :, 0:64], vp[:, 0:256].rearrange("p (h d) -> p h d", h=4), AF.Copy)
        norm_x(hT, "gcross")
        for c in range(NC4):
            cs = slice(c * CH, (c + 1) * CH)
            for j in range(2):
                qp = pnext()
                for kt in range(8):
                    kb.mm(qp[:, :], wq[:, kt, 128 * j:128 * j + 128], hT[:, kt, cs], start=(kt == 0), stop=(kt == 7))
                rms_T([qp[:, :]], [col("xgq", 0)], [QcT[:, j, cs]], [blk64[:, :]], 64, 128, lnbias=math.log(64 ** -0.5))
        pbanks[0] = [0, 1, 2, 3]
        pi_ = 0
        for c in range(NC4):
            cs = slice(c * CH, (c + 1) * CH)
            for h in range(4):
                r0 = 64 * (h % 2)
                j = h // 2
                O = PS[4 + h]
                for mt in range(2):
                    sp_ = pnext()
                    kb.mm(sp_[:, :], KcT[r0:r0 + 64, j, 128 * mt:128 * mt + 128], QcT[r0:r0 + 64, j, cs])
                    P = Pt[pi_ % 4]
                    pi_ += 1
                    kb.act(P[:], sp_[:, :], AF.Exp)
                    kb.mm(O[:, :], VC[:, mt, h, :], P[:], start=(mt == 0), stop=(mt == 1))
                rd = rdt[h % 2]
                kb.recip(rd[0:64, :], O[64:128, :])
                kb.tt(OcT[r0:r0 + 64, j, cs], O[0:64, :], rd[0:64, :], ALU.mult)
        pbanks[0] = list(range(8))
        for ot in range(8):
            for c in range(NC4):
                cs = slice(c * CH, (c + 1) * CH)
                op_ = pnext()
                for k2 in range(2):
                    kb.mm(op_[:, :], wo[:, k2, 128 * ot:128 * ot + 128], OcT[:, k2, cs], start=(k2 == 0), stop=(k2 == 1))
                resid_add(ot, cs, op_[:, :])
        kb.barrier()
    if debug:
        DBG2 = kb_dram("dbgxb", [1024, TO], F32, "ExternalOutput")
        kb.dma("sp", DBG2.rearrange("(kt p) t -> p kt t", p=128), xT[:])

    TB = TO
    NTB = 1
    JB = 256 if moe else 128
    WDW = 256
    NH = NFT // 2
    with contextlib.ExitStack() as e1:
        hT = kb.sb("hTf", [128, 8, TO], BF16, es=e1)
        hmid = kb.sb("hmid", [128, NH, TB], BF16, es=e1)
        wgB = [kb.sb(f"wgB{i}", [128, 8, JB], BF16, es=e1) for i in range(2)]
        wuB = [kb.sb(f"wuB{i}", [128, 8, JB], BF16, es=e1) for i in range(2)]
        wdB = [kb.sb(f"wdB{i}", [128, NH, WDW], BF16, es=e1) for i in range(2)]
        av = [kb.sb(f"av{i}", [128, CH], F32, es=e1) for i in range(2 if not moe else 1)] * 2
        tv = [kb.sb(f"tv{i}", [128, CH], F32, es=e1) for i in range(1)] * 2
        rrow = None
        if moe:
            rrow = kb.sb("rrow", [128, CH], F32, es=e1)
            identf = kb.sb("identf", [128, 128], F32, es=e1)
            wrf = kb.sb("wrf", [128, 8, 8], F32, es=e1)
            Wtok = kb.sb("Wtok", [128, TO // 128, 8], F32, es=e1)
            lg = kb.sb("lg", [128, 8], F32, es=e1)
            mx = kb.sb("mx", [128, 8], F32, es=e1)
            ex = kb.sb("ex", [128, 8], F32, es=e1)
            mk = kb.sb("mk", [128, 8], F32, es=e1)
            sm = kb.sb("sm", [128, 4], F32, es=e1)
            rc = kb.sb("rc", [128, 1], F32, es=e1)
            onef = kb.sb("onef", [128, 1], F32, es=e1)
            wbc = kb.sb("wbc", [128, TB], BF16, es=e1)
            wb128 = kb.sb("wb128", [128, 128], F32, es=e1)
            kb.memset(onef[:], 1.0)
            iop = kb.sb("iop", [128, 128], F32, es=e1)
            kb.op("pool", lambda: nc.gpsimd.iota(iop[:], pattern=[[1, 128]], base=0, channel_multiplier=-1,
                                                   allow_small_or_imprecise_dtypes=True), [], [iop[:]])
            kb.ts(identf[:], iop[:], 0.0, None, ALU.is_equal)
            kb.dma("sp", wrf[:], D["wr"].rearrange("(kt p) n -> p kt n", p=128))
            for kt in range(8):
                kb.ts(wrf[:, kt, :], wrf[:, kt, :], col("gffn", kt), None, ALU.mult)
        norm_x(hT, "gffn", rstd_row=rrow)
        if moe:
            for tt_ in range(TO // 128):
                ts_ = slice(128 * tt_, 128 * tt_ + 128)
                lp = pnext()
                for kt in range(8):
                    kb.mm(lp[:, 0:8], xT[:, kt, ts_], wrf[:, kt, :], start=(kt == 0), stop=(kt == 7))
                rp = pnext()
                cq = tt_ // 4
                kb.mm(rp[:, 0:1], rrow[32 * cq:32 * cq + 1, 128 * (tt_ % 4):128 * (tt_ % 4) + 128], onef[32 * cq:32 * cq + 1, 0:1], tile_position=(32 * cq, 0))
                kb.act(rc[:], rp[:, 0:1], AF.Copy)
                kb.ts(lg[:], lp[:, 0:8], rc[:, 0:1], None, ALU.mult)
                kb.op("dve", lambda: nc.vector.max(out=mx[:], in_=lg[:]), [lg[:]], [mx[:]])
                kb.ts(mk[:], lg[:], mx[:, 1:2], None, ALU.is_ge)
                kb.ts(sm[:, 0:1], mx[:, 0:1], -1.0, None, ALU.mult)
                kb.act(ex[:], lg[:], AF.Exp, bias=sm[:, 0:1])
                kb.tt(ex[:], ex[:], mk[:], ALU.mult)
                kb.op("dve", lambda: nc.vector.reduce_sum(out=sm[:, 1:2], in_=ex[:], axis=AX.X), [ex[:]], [sm[:, 1:2]])
                kb.recip(sm[:, 2:3], sm[:, 1:2])
                kb.ts(Wtok[:, tt_, :], ex[:], sm[:, 2:3], None, ALU.mult)
        fgv = D["fg"].rearrange("e (kt p) n -> e p kt n", p=128)
        fuv = D["fu"].rearrange("e (kt p) n -> e p kt n", p=128)
        fdv = D["fd"].rearrange("e (kt p) n -> e p kt n", p=128)
        bi = 0
        di = 0
        ai = 0
        for e in range(NE):
            if moe:
                for q in range(TB // 128):
                    kb.cp(wb128[:, :], Wtok[:, q, e:e + 1].to_broadcast([128, 128]))
                    wp = pnext()
                    kb.mm(wp[:, 0:128], wb128[:, :], identf[:, :])
                    kb.act(wbc[:, 128 * q:128 * q + 128], wp[:, 0:128], AF.Copy)
            for jh in range(2):
                nblk = (NH * 128) // JB
                for jb in range(nblk):
                    wgt = wgB[bi % 2]
                    wut = wuB[bi % 2]
                    bi += 1
                    c0 = jh * NH * 128 + JB * jb
                    kb.dma("pool", wgt[:], fgv[e][:, :, c0:c0 + JB])
                    kb.dma("pool", wut[:], fuv[e][:, :, c0:c0 + JB])
                    for jj in range(JB // 128):
                        jl = jb * (JB // 128) + jj
                        for c2 in range(TB // CH):
                            cs = slice(c2 * CH, (c2 + 1) * CH)
                            gp = pnext()
                            for kt in range(8):
                                kb.mm(gp[:, :], wgt[:, kt, 128 * jj:128 * jj + 128], hT[:, kt, cs], start=(kt == 0), stop=(kt == 7))
                            up = pnext()
                            for kt in range(8):
                                kb.mm(up[:, :], wut[:, kt, 128 * jj:128 * jj + 128], hT[:, kt, cs], start=(kt == 0), stop=(kt == 7))
                            a = av[ai % 2]
                            t = tv[ai % 2]
                            ai += 1
                            kb.act(a[:], gp[:, :], AF.Silu)
                            if moe:
                                kb.tt(t[:], a[:], up[:, :], ALU.mult)
                                kb.tt(hmid[:, jl, cs], t[:], wbc[:, cs], ALU.mult, e="pool")
                            else:
                                kb.tt(hmid[:, jl, cs], a[:], up[:, :], ALU.mult)
                for op2 in range(1024 // WDW):
                    wdt = wdB[di % 2]
                    di += 1
                    kb.dma("pool", wdt[:], fdv[e][:, jh * NH:(jh + 1) * NH, WDW * op2:WDW * (op2 + 1)])
                    for o2 in range(WDW // 128):
                        ot = (WDW // 128) * op2 + o2
                        for c2 in range(TB // CH):
                            cs = slice(c2 * CH, (c2 + 1) * CH)
                            dp = pnext()
                            for jl in range(NH):
                                kb.mm(dp[:, :], wdt[:, jl, 128 * o2:128 * o2 + 128], hmid[:, jl, cs],
                                      start=(jl == 0), stop=(jl == NH - 1))
                            resid_add(ot, cs, dp[:, :])
        kb.barrier()
    xov = XO.rearrange("(kt p) t -> p kt t", p=128)
    for c in range(NC4):
        kb.dma("sp", xov[:, :, c * CH:(c + 1) * CH], xT[:, :, c * CH:(c + 1) * CH], extra_w=("xsp",))
    if hout is not None:
        with contextlib.ExitStack() as eh:
            hTn = kb.sb("hTn", [128, 8, TO], BF16, es=eh)
            hmk = [kb.sb(f"hmk{i}", [128, 8, CH], BF16, es=eh) for i in range(2)]
            for c in range(NC4):
                cs = slice(c * CH, (c + 1) * CH)
                rms_T([xT[:, kt, cs] for kt in range(8)], [hout["g"][:, kt:kt + 1] for kt in range(8)],
                      [hTn[:, kt, cs] for kt in range(8)], [ones[:, :]] * 8, 1024, 128)
                for s in range(2):
                    t = hmk[s]
                    kb.act(t[:], hTn[:, :, cs], AF.Copy, scale=hout["meq"][:, s:s + 1])
                    for q in range(2):
                        kb.dma("sp", hout["hsrc"].ap()[q, s].rearrange("(kt p) t -> p kt t", p=128)[:, :, cs], t[:], extra_w=("hsrc",))
            kb.barrier()
    kb.barrier()
    kb.es.close()
    kb.es = old_es
    kb.sfx = old_sfx
    if standalone:
        kb.wait_all("sp")
    return kb

import math

RG = [[0, 1], [2, 3], [4, 5], [6, 7]]

def build_fused():
    kb = KB()
    nc = kb.nc
    PS = [kb.ps(f"ps{i}", [128, 512], F32) for i in range(8)]
    meq = kb.sb("meq_sb", [128, 2], F32)
    g1 = kb.sb("g1c_sb", [128, 8], F32)
    meq_d = kb.dram("meq", [128, 2], F32, "ExternalInput")
    g1_d = kb.dram("g1c", [128, 8], F32, "ExternalInput")
    kb.dma("sp", meq[:], meq_d[:, :])
    kb.dma("sp", g1[:], g1_d[:, :])
    ysrc = nc.dram_tensor("ysrc", [2, 4, 2, 128, 2048], BF16)
    ydst = nc.dram_tensor("ydst", [4, 256, 2048], BF16)
    hsrc = nc.dram_tensor("hsrc", [2, 2, 1024, 2048], BF16)
    hdst = nc.dram_tensor("hdst", [2, 1024, 2048], BF16)
    xsp = nc.dram_tensor("xsp", [1024, 2048], F32)
    fo = dict(ysrc=ysrc, meq=meq)
    build_M(0.8 - 0.6 * math.exp(-0.3 * 0), kb=kb, sfx="_m0", PS=PS, fin=None, fout=fo)
    kb.collective("ReduceScatter", ALU.add, RG, ysrc, ydst, "ysrc", "ydst")
    build_R(False, kb=kb, sfx="_r0", PS=PS, xin=None, yin=ydst.ap(), xout=xsp.ap(), hout=dict(hsrc=hsrc, meq=meq, g=g1))
    kb.collective("ReduceScatter", ALU.add, RG, hsrc, hdst, "hsrc", "hdst")
    build_M(0.8 - 0.6 * math.exp(-0.3 * 1), kb=kb, sfx="_m1", PS=PS, fin=hdst.ap(), fout=fo)
    kb.collective("ReduceScatter", ALU.add, RG, ysrc, ydst, "ysrc", "ydst")
    build_R(True, kb=kb, sfx="_r1", PS=PS, xin=xsp.ap(), yin=ydst.ap(), xout=None, hout=None)
    kb.wait_all("sp")
    return kb

import math
import numpy as np

SP = [256, 448, 576, 608, 864, 1120, 1376, 1632, 1888]
f32 = np.float32


def bucket_consts():
    dist = np.arange(NJ, dtype=np.int64) - 511
    n = np.maximum(dist, 0)
    exact = 16
    lr = np.log(np.maximum(n, exact).astype(f32) / f32(exact)) / f32(math.log(128 / exact))
    large = np.minimum(exact + (lr * f32(32 - exact)).astype(np.int32), 31)
    bk = np.where(n < exact, n, large)
    oh = np.zeros((32, NJ), f32)
    oh[bk, np.arange(NJ)] = 1.0
    mask = (dist >= 0).astype(f32)
    return oh, np.tile(mask[None], (3, 1))


def rot_consts():
    rotT = np.zeros((96, 96), f32)
    for i in range(16):
        rotT[80 + i, 64 + i] = -1.0
        rotT[64 + i, 80 + i] = 1.0
    inv = (f32(10000.0) ** (-np.arange(16, dtype=f32) / f32(16))).astype(f32)
    return rotT, inv


def colpad(v, rows=128):
    out = np.zeros((rows,), f32)
    out[:len(v)] = v
    return out


def prep_M(I, l, b, p, xT):
    w_in = I["w_in"][l]
    cols = np.concatenate([
        np.arange(128 * p, 128 * p + 128),
        np.arange(256, 448), np.arange(448, 576), np.arange(576, 608),
        np.arange(608 + 128 * p, 608 + 128 * p + 128),
        np.arange(864 + 128 * p, 864 + 128 * p + 128),
        np.arange(1120 + 128 * p, 1120 + 128 * p + 128),
        np.arange(1376 + 128 * p, 1376 + 128 * p + 128),
        np.arange(1632 + 128 * p, 1632 + 128 * p + 128)])
    m = {}
    m["xT"] = np.ascontiguousarray(xT)
    m["wmix"] = np.ascontiguousarray(w_in[:, cols])
    m["pos"] = np.ascontiguousarray(I["positions"][b:b + 1].astype(np.int32))
    hs = [2 * p, 2 * p + 1]
    wuq = I["mla_w_uq"][l]
    m["wuq"] = np.ascontiguousarray(np.concatenate([wuq[:, 96 * h:96 * h + 96] for h in hs], axis=1))
    wukv = I["mla_w_ukv"][l]
    m["wkn"] = np.ascontiguousarray(np.concatenate([wukv[:, 128 * h:128 * h + 64] for h in hs], axis=1))
    m["wvm"] = np.ascontiguousarray(np.concatenate([wukv[:, 128 * h + 64:128 * h + 128] for h in hs], axis=1))
    rotT, inv = rot_consts()
    m["rotT"] = rotT
    for nm, key in (("wr", "lru_w_r"), ("wi", "lru_w_i")):
        w = np.zeros((128, 128), f32)
        for j in range(2):
            w[64 * j:64 * j + 64, 64 * j:64 * j + 64] = I[key][l][2 * p + j]
        m[nm] = w
    gs = slice(8 * p, 8 * p + 8)
    lr = I["s5_lam_re"][l][gs].reshape(512)
    li = I["s5_lam_im"][l][gs].reshape(512)
    ls = np.repeat(I["s5_log_step"][l][gs], 64)
    m["s5rep"] = np.ascontiguousarray(np.tile(np.stack([lr, li, ls])[None], (32, 1, 1)).astype(f32))
    bT = np.zeros((32, 2, 512), f32)
    cT = np.zeros((128, 2, 4, 32), f32)
    for gi in range(8):
        g = 8 * p + gi
        st, half = gi // 2, gi % 2
        for ri, key in enumerate(("s5_b_re", "s5_b_im")):
            bT[16 * half:16 * half + 16, ri, st * 128 + 64 * half: st * 128 + 64 * half + 64] = I[key][l][g].T
        for ri, key in enumerate(("s5_c_re", "s5_c_im")):
            cT[64 * half:64 * half + 64, ri, st, 16 * half:16 * half + 16] = I[key][l][g].T
    m["s5bT"] = bT
    m["s5cT"] = cT
    oh, maskrows = bucket_consts()
    m["oh"] = oh
    m["maskrows"] = maskrows
    m["relmy"] = np.ascontiguousarray(I["rel_table"][:, hs])
    pc = np.zeros((128, NPC), f32)

    def put(name, i, v):
        o, w = PC[name]
        pc[:len(v), o + i] = v
    for kt in range(8):
        put("gmix", kt, I["g_mix"][l][128 * kt:128 * kt + 128])
    put("gcq", 0, I["mla_g_cq"][l][0:128]); put("gcq", 1, I["mla_g_cq"][l][128:192])
    put("gckv", 0, I["mla_g_ckv"][l])
    put("mgq", 0, I["mla_g_qn"][l]); put("mgk", 0, I["mla_g_kn"][l])
    iv = np.zeros(128, f32); iv[64:96] = np.tile(inv, 2); put("inv", 0, iv)
    put("dgq", 0, np.tile(I["diff_g_qn"][l], 4)); put("dgk", 0, np.tile(I["diff_g_kn"][l], 4))
    put("gsub", 0, np.tile(I["diff_g_sub"][l], 2))
    for j, h in enumerate(hs):
        put("tb31", j, np.full(128, I["rel_table"][31, h], f32))
    for st in range(4):
        put("s5lr", st, lr[128 * st:128 * st + 128]); put("s5li", st, li[128 * st:128 * st + 128])
        put("s5ls", st, ls[128 * st:128 * st + 128])
    cs = slice(128 * p, 128 * p + 128)
    put("s5d", 0, I["s5_d"][l][cs])
    for k in range(4):
        put("cw", k, I["lru_conv_w"][l][k][cs])
    put("cb", 0, I["lru_conv_b"][l][cs]); put("br", 0, I["lru_b_r"][l][cs]); put("bi", 0, I["lru_b_i"][l][cs])
    put("llam", 0, I["lru_lam"][l][cs])
    for nm, key in (("lq1", "diff_lq1"), ("lk1", "diff_lk1"), ("lq2", "diff_lq2"), ("lk2", "diff_lk2")):
        o, w = PC[nm]
        pc[:, o:o + 32] = I[key][l][None, :]
    m["pcols"] = pc
    return m

import numpy as np
f32 = np.float32

def prep_R(I, l, b, p, xT_own, yall):
    m = {}
    m["xT"] = np.ascontiguousarray(xT_own)
    m["yall"] = np.ascontiguousarray(yall) if yall is not None else None
    w_in = I["w_in"][l]
    m["wg8"] = np.ascontiguousarray(np.stack([np.concatenate([w_in[:, 1888 + 1024 * bb + 128 * dt:1888 + 1024 * bb + 128 * dt + 128] for bb in range(4)], axis=1) for dt in range(8)]))
    m["wbr"] = np.ascontiguousarray(I["w_branch"][l])
    m["wout"] = np.ascontiguousarray(I["w_out"][l])
    m["wglu"] = np.ascontiguousarray(I["s5_w_glu"][l])
    for nm, key in (("xwq", "x_wq"), ("xwk", "x_wk"), ("xwv", "x_wv"), ("xwo", "x_wo")):
        m[nm] = np.ascontiguousarray(I[key][l])
    m["memT"] = np.ascontiguousarray(I["mem"][b].T)
    pc = np.zeros((128, NP2), f32)
    def put(name, i, v):
        o, w = P2[name]
        pc[:len(v), o + i] = v
    for nm, key in (("gmix", "g_mix"), ("gcross", "g_cross"), ("gmem", "g_mem"), ("gffn", "g_ffn")):
        for kt in range(8):
            put(nm, kt, I[key][l][128 * kt:128 * kt + 128])
    for bb in range(4):
        for dt in range(8):
            put("bgate", bb * 8 + dt, I["b_gate"][l][bb][128 * dt:128 * dt + 128])
    for j in range(2):
        put("bglu", j, I["s5_b_glu"][l][128 * j:128 * j + 128])
    put("xgq", 0, np.tile(I["x_g_qn"][l], 2)); put("xgk", 0, np.tile(I["x_g_kn"][l], 2))
    m["pc2"] = pc
    if l % 2 == 0:
        m["fg"] = np.ascontiguousarray(I["ffn_w_gate"][l // 2][None]); m["fu"] = np.ascontiguousarray(I["ffn_w_up"][l // 2][None])
        m["fd"] = np.ascontiguousarray(I["ffn_w_down"][l // 2][None])
    else:
        m["fg"] = I["moe_w_gate"][l // 2]; m["fu"] = I["moe_w_up"][l // 2]; m["fd"] = I["moe_w_down"][l // 2]
        m["wrt"] = np.ascontiguousarray(I["moe_w_router"][l // 2])
    return m

import numpy as np
f32 = np.float32

def prep_fused(I, c):
    b, p = c // 2, c % 2
    m = {}
    xT = np.ascontiguousarray(I["x"][b].T)
    ts = slice(2048 * p, 2048 * p + 2048)
    for l in range(2):
        mm = prep_M(I, l, b, p, xT)
        if l == 1:
            mm.pop("xT")
        for k, v in mm.items():
            m[k + f"_m{l}"] = v
        mr = prep_R(I, l, b, p, xT[:, ts], None)
        mr.pop("yall")
        if l == 1:
            mr.pop("xT")
        for k, v in mr.items():
            m[k + f"_r{l}"] = v
    meq = np.zeros((128, 2), f32)
    meq[:, p] = 1.0
    m["meq"] = meq
    m["g1c"] = np.ascontiguousarray(I["g_mix"][1].reshape(8, 128).T)
    return m


def kernel(**inputs):
    I = {k: np.asarray(v) for k, v in inputs.items()}
    kb = build_fused()
    maps = [prep_fused(I, c) for c in range(8)]
    res = kb.run(maps, n=8)
    out = np.empty((4, 4096, 1024), np.float32)
    for c in range(8):
        b, p = c // 2, c % 2
        out[b, 2048 * p:2048 * p + 2048, :] = res.results[c]["xo_r1"].T
    return out
```

```python
import contextlib
import numpy as np
import concourse.bass as bass
import concourse.mybir as mybir
from concourse.bass_utils import run_bass_kernel_spmd

F32 = mybir.dt.float32
BF16 = mybir.dt.bfloat16
I32 = mybir.dt.int32
AF = mybir.ActivationFunctionType
ALU = mybir.AluOpType
AX = mybir.AxisListType


def _box(ap):
    t = ap.tensor
    shp = list(t.shape)
    row = 1
    for s in shp[1:]:
        row *= s
    off = ap.offset
    p0 = off // row
    f0 = off % row
    dims = list(ap.ap)
    pstride, pcount = dims[0]
    if pstride == 0:
        pcount = 1
    ext = 0
    for st, cn in dims[1:]:
        ext += abs(st) * (cn - 1)
    return (p0, p0 + pcount, f0, f0 + ext + 1)


def _ov(a, b):
    return a[0] < b[1] and b[0] < a[1] and a[2] < b[3] and b[2] < a[3]


def _covers(a, b):
    return a[0] <= b[0] and a[1] >= b[1] and a[2] <= b[2] and a[3] >= b[3]


class KB:
    NDMA = 32

    def __init__(self):
        self.nc = bass.Bass("TRN2", target_bir_lowering=False)
        nc = self.nc
        self.es = contextlib.ExitStack()
        self.eng = {"pe": nc.tensor, "act": nc.scalar, "dve": nc.vector, "pool": nc.gpsimd, "sp": nc.sync}
        self.sem = {}
        self.cnt = {}
        for e in ("pe", "act", "dve", "pool"):
            self.sem[e] = self.es.enter_context(nc.semaphore("sem_" + e))
            self.cnt[e] = 0
        self.dsem = [self.es.enter_context(nc.semaphore(f"sem_dma{i}")) for i in range(self.NDMA)]
        for i, s in enumerate(self.dsem):
            self.sem[f"d{i}"] = s
            self.cnt[f"d{i}"] = 0
        self.sem["cc"] = self.es.enter_context(nc.semaphore("sem_cc"))
        self.cnt["cc"] = 0
        self.dnext = 0
        self.dnext_sw = 0
        self.waited = {e: {} for e in self.eng}
        self.rec = {}
        self.nins = {e: 0 for e in self.eng}
        self.same_engine_sync = {"dve": True, "pool": True, "act": True, "pe": False, "sp": True}

    sfx = ""

    def sb(self, name, shape, dtype, es=None):
        return (es or self.es).enter_context(self.nc.sbuf_tensor(name + self.sfx, list(shape), dtype))

    def ps(self, name, shape, dtype=F32, es=None):
        return (es or self.es).enter_context(self.nc.psum_tensor(name, list(shape), dtype))

    def dram(self, name, shape, dtype, kind):
        return self.nc.dram_tensor(name, list(shape), dtype, kind=kind).ap()

    def _tracked(self, ap):
        nm = type(ap.tensor).__name__
        return nm.startswith("SB") or nm.startswith("PSum")

    def _collect(self, reads, writes, extra_r=(), extra_w=()):
        deps = {}

        def add(d):
            for k, v in d.items():
                if deps.get(k, 0) < v:
                    deps[k] = v
        for ap in reads:
            if ap is None or not self._tracked(ap):
                continue
            b = _box(ap)
            for r in self.rec.get(ap.tensor.name, ()):
                if _ov(r[0], b):
                    add(r[1])
        for ap in writes:
            if ap is None or not self._tracked(ap):
                continue
            b = _box(ap)
            for r in self.rec.get(ap.tensor.name, ()):
                if _ov(r[0], b):
                    add(r[1])
                    add(r[2])
        for key in extra_r:
            for r in self.rec.get(key, ()):
                add(r[1])
        for key in extra_w:
            for r in self.rec.get(key, ()):
                add(r[1]); add(r[2])
        return deps

    def _emit_waits(self, e, deps, force=False):
        eng = self.eng[e]
        w = self.waited[e]
        for k, v in deps.items():
            if k == e and not force and not self.same_engine_sync.get(e, True):
                continue
            if w.get(k, 0) >= v:
                continue
            eng.wait_ge(self.sem[k], v)
            self.nins[e] += 1
            w[k] = v

    def _update(self, ev, reads, writes, extra_r=(), extra_w=()):
        k, v = ev
        for ap in reads:
            if ap is None or not self._tracked(ap):
                continue
            b = _box(ap)
            lst = self.rec.setdefault(ap.tensor.name, [])
            for r in lst:
                if _ov(r[0], b):
                    if r[2].get(k, 0) < v:
                        r[2][k] = v
        for ap in writes:
            if ap is None or not self._tracked(ap):
                continue
            b = _box(ap)
            lst = self.rec.setdefault(ap.tensor.name, [])
            new = [r for r in lst if not _covers(b, r[0])]
            new.append([b, {k: v}, {}])
            self.rec[ap.tensor.name] = new
        for key in extra_r:
            for r in self.rec.setdefault(key, []):
                if r[2].get(k, 0) < v:
                    r[2][k] = v
        for key in extra_w:
            self.rec[key] = [[(0, 1, 0, 1), {k: v}, {}]]

    def op(self, e, fn, reads, writes, extra_r=(), extra_w=()):
        deps = self._collect(reads, writes, extra_r, extra_w)
        self._emit_waits(e, deps)
        ins = fn()
        self.cnt[e] += 1
        ins.then_inc(self.sem[e], 1)
        self.nins[e] += 1
        ev = (e, self.cnt[e])
        self._update(ev, reads, writes, extra_r, extra_w)
        return ev

    def dma(self, q, out, in_, extra_r=(), extra_w=(), **kw):
        half = self.NDMA // 2
        if q == "pool":
            i = half + self.dnext_sw
            self.dnext_sw = (self.dnext_sw + 1) % (self.NDMA - half)
        else:
            i = self.dnext
            self.dnext = (self.dnext + 1) % half
        dk = f"d{i}"
        deps = self._collect([in_], [out], extra_r, extra_w)
        if self.cnt[dk] > 0:
            deps[dk] = max(deps.get(dk, 0), self.cnt[dk])
        self._emit_waits(q, deps)
        ins = self.eng[q].dma_start(out=out, in_=in_, **kw)
        self.cnt[dk] += 16
        ins.then_inc(self.sem[dk], 16)
        self.nins[q] += 1
        ev = (dk, self.cnt[dk])
        self._update(ev, [in_], [out], extra_r, extra_w)
        return ev

    def collective(self, kind, op, rg, src, dst, rkey, wkey):
        deps = self._collect([], [], extra_r=(rkey,), extra_w=(wkey,))
        self._emit_waits("pool", deps)
        ins = self.nc.gpsimd.collective_compute(kind, op, replica_groups=rg, ins=[src.ap().opt()], outs=[dst.ap().opt()])
        self.cnt["cc"] += 1
        ins.then_inc(self.sem["cc"], 1)
        self.nins["pool"] += 1
        self._update(("cc", self.cnt["cc"]), [], [], extra_r=(rkey,), extra_w=(wkey,))

    def barrier(self):
        deps = {k: v for k, v in self.cnt.items() if v > 0}
        for e in self.eng:
            self._emit_waits(e, dict(deps), force=True)
        self.rec = {}

    def wait_all(self, e="sp"):
        deps = {k: v for k, v in self.cnt.items() if v > 0}
        self._emit_waits(e, deps)

    def mm(self, out, lhsT, rhs, start=True, stop=True, **kw):
        return self.op("pe", lambda: self.nc.tensor.matmul(out, lhsT=lhsT, rhs=rhs, start=start, stop=stop, **kw),
                       [lhsT, rhs] + ([] if start else [out]), [out])

    def tr(self, out, in_, ident):
        return self.op("pe", lambda: self.nc.tensor.transpose(out, in_, ident), [in_, ident], [out])

    def act(self, out, in_, func, bias=0.0, scale=1.0, accum_out=None, e="act"):
        rd = [in_]
        kw = {}
        if not isinstance(bias, (int, float)):
            rd.append(bias)
        if not isinstance(scale, (int, float)):
            rd.append(scale)
        wr = [out]
        if accum_out is not None:
            wr.append(accum_out)
            kw["accum_out"] = accum_out
        return self.op("act", lambda: self.nc.scalar.activation(out=out, in_=in_, func=func, bias=bias, scale=scale, **kw), rd, wr)

    def tt(self, out, in0, in1, op, e="dve"):
        return self.op(e, lambda: self.eng[e].tensor_tensor(out=out, in0=in0, in1=in1, op=op), [in0, in1], [out])

    def ts(self, out, in0, s1, s2, op0, op1=None, e="dve", accum_out=None):
        rd = [in0]
        if not isinstance(s1, (int, float)):
            rd.append(s1)
        if s2 is not None and not isinstance(s2, (int, float)):
            rd.append(s2)
        kw = {}
        if op1 is not None:
            kw["op1"] = op1
        wr = [out]
        if accum_out is not None:
            kw["accum_out"] = accum_out
            wr.append(accum_out)
        return self.op(e, lambda: self.eng[e].tensor_scalar(out=out, in0=in0, scalar1=s1, scalar2=s2, op0=op0, **kw), rd, wr)

    def stt(self, out, in0, scalar, in1, op0, op1, e="dve"):
        rd = [in0, in1]
        if not isinstance(scalar, (int, float)):
            rd.append(scalar)
        return self.op(e, lambda: self.eng[e].scalar_tensor_tensor(out=out, in0=in0, scalar=scalar, in1=in1, op0=op0, op1=op1), rd, [out])

    def cp(self, out, in_, e="dve"):
        return self.op(e, lambda: self.eng[e].tensor_copy(out=out, in_=in_), [in_], [out])

    def memset(self, ap, val, e="dve"):
        return self.op(e, lambda: self.eng[e].memset(ap, val), [], [ap])

    def recip(self, out, in_):
        return self.op("dve", lambda: self.nc.vector.reciprocal(out=out, in_=in_), [in_], [out])

    def scan(self, out, d0, d1, initial, op0=ALU.mult, op1=ALU.add):
        rd = [d0, d1]
        if not isinstance(initial, (int, float)):
            rd.append(initial)
        return self.op("dve", lambda: self.nc.vector.tensor_tensor_scan(out=out, data0=d0, data1=d1, initial=initial, op0=op0, op1=op1), rd, [out])

    def run(self, in_maps, n=8, trace=False):
        res = run_bass_kernel_spmd(self.nc, in_maps, core_ids=list(range(n)), trace=trace)
        return res

import math

T = 4096
CH = 512
NCH = T // CH
PI = math.pi
EPS = 1e-6
NJ = 1151

PC_LAYOUT = [("gmix", 8), ("gcq", 2), ("gckv", 1), ("mgq", 1), ("mgk", 1), ("inv", 1), ("dgq", 1), ("dgk", 1),
             ("gsub", 1), ("tb31", 2), ("s5lr", 4), ("s5li", 4), ("s5ls", 4), ("s5d", 1),
             ("cw", 4), ("cb", 1), ("br", 1), ("bi", 1), ("llam", 1),
             ("lq1", 32), ("lk1", 32), ("lq2", 32), ("lk2", 32)]
PC = {}
_o = 0
for _n, _w in PC_LAYOUT:
    PC[_n] = (_o, _w)
    _o += _w
NPC = _o


def build_M(lam_init, debug=False, kb=None, sfx="", PS=None, fin=None, fout=None):
    standalone = kb is None
    if standalone:
        kb = KB()
    nc = kb.nc
    old_es = kb.es
    kb.es = contextlib.ExitStack()
    old_sfx = kb.sfx
    kb.sfx = sfx
    _dram = kb.dram
    class _K:
        pass
    def dram(name, shape, dt, kind):
        return _dram(name + sfx, shape, dt, kind)
    kb_dram = dram
    D = {}
    if fin is None:
        D["xT"] = kb_dram("xT", [1024, T], F32, "ExternalInput")
    D["wmix"] = kb_dram("wmix", [1024, 1120], F32, "ExternalInput")
    D["pcols"] = kb_dram("pcols", [128, NPC], F32, "ExternalInput")
    D["pos"] = kb_dram("pos", [1, T], I32, "ExternalInput")
    D["wuq"] = kb_dram("wuq", [192, 192], F32, "ExternalInput")
    D["wkn"] = kb_dram("wkn", [128, 128], F32, "ExternalInput")
    D["wvm"] = kb_dram("wvm", [128, 128], F32, "ExternalInput")
    D["rotT"] = kb_dram("rotT", [96, 96], F32, "ExternalInput")
    D["wr"] = kb_dram("wr", [128, 128], F32, "ExternalInput")
    D["wi"] = kb_dram("wi", [128, 128], F32, "ExternalInput")
    D["s5rep"] = kb_dram("s5rep", [32, 3, 512], F32, "ExternalInput")
    D["s5bT"] = kb_dram("s5bT", [32, 2, 512], F32, "ExternalInput")
    D["s5cT"] = kb_dram("s5cT", [128, 2, 4, 32], F32, "ExternalInput")
    D["oh"] = kb_dram("oh", [32, NJ], F32, "ExternalInput")
    D["maskrows"] = kb_dram("maskrows", [3, NJ], F32, "ExternalInput")
    D["relmy"] = kb_dram("relmy", [32, 2], F32, "ExternalInput")
    Y = kb_dram("Y", [4, 128, T], BF16, "ExternalOutput") if fout is None else None
    DBG = kb_dram("DBG", [4, 128, T], F32, "ExternalOutput") if debug else None
    RL = NJ + 128
    scratch = nc.dram_tensor("ebscratch" + sfx, [3, 128, RL], F32, kind="Internal")

    es = kb.es
    pc = kb.sb("pc", [128, NPC], F32)
    def col(name, i=0, rows=slice(0, 128)):
        o, w = PC[name]
        return pc[rows, o + i:o + i + 1]
    ones = kb.sb("ones", [128, 128], BF16)
    blk32 = kb.sb("blk32", [128, 128], BF16)
    uT = kb.sb("uT", [128, T], BF16)
    QT = kb.sb("QT", [96, 2, T], BF16)
    KT = kb.sb("KT", [96, 2, T], BF16)
    QdT = kb.sb("QdT", [128, T], BF16)
    KdT = kb.sb("KdT", [128, T], BF16)
    VA = kb.sb("VA", [128, 32, 2, 128], BF16)
    VD = kb.sb("VD", [128, 32, 2, 128], BF16)
    neglam = kb.sb("neglam", [128, 1], F32)
    if PS is None:
        PS = [kb.ps(f"ps{i}", [128, 512], F32, es=old_es) for i in range(8)]
    pctr = [0]
    pbanks = [list(range(8))]

    def pnext():
        b = pbanks[0]
        p = PS[b[pctr[0] % len(b)]]
        pctr[0] += 1
        return p

    kb.dma("sp", pc[:], D["pcols"][:, :])
    kb.memset(ones[:], 1.0)
    wtile = kb.sb("wtile", [128, 512], BF16)
    kb.memset(wtile[:], 1.0)

    def warm_pe(banks, n=24):
        for i in range(n):
            kb.mm(PS[banks[i % len(banks)]][:, :], ones[:, :], wtile[:, :])

    kb.memset(blk32[:], 0.0, e="pool")
    for g in range(4):
        kb.memset(blk32[32 * g:32 * g + 32, 32 * g:32 * g + 32], 1.0, e="pool")
    kb.memset(VA[:, :, :, 64:128], 1.0, e="pool")
    kb.memset(VD[:, :, :, 64:128], 1.0, e="pool")

    ystage = [kb.sb(f"yst{i}", [128, CH], BF16) for i in range(3)]
    ystage_a = [kb.sb(f"ysta{i}", [128, CH], BF16) for i in range(2)]
    yai = [0]

    def ynext_a():
        y = ystage_a[yai[0] % 2]
        yai[0] += 1
        return y
    ymk = [kb.sb(f"ymk{i}", [128, CH], BF16) for i in range(4)] if fout is not None else None
    BL = kb.sb("BL", [128, 2, 128], BF16)
    cTb = kb.sb("cTb", [128, 2, 4, 32], BF16)
    magp = kb.sb("magp", [128, 4], F32)
    thp = kb.sb("thp", [128, 4], F32)
    tmpi = [0]
    sqs = [kb.sb(f"sq{i}", [128, 512], BF16) for i in range(4)]
    lnvs = [kb.sb(f"lnv{i}", [128, 512], F32) for i in range(2)]
    rstds = [kb.sb(f"rstd{i}", [128, 512], F32) for i in range(2)]
    es_lru = contextlib.ExitStack()
    xlT = kb.sb("xlT", [128, T], F32, es=es_lru)
    ggT = kb.sb("ggT", [128, T], BF16, es=es_lru)

    def rms_T(srcs, gains, outs, onesT, nfeat, npo, lnbias=0.0, W=512):
        i0 = tmpi[0]
        tmpi[0] += 1
        ssp = pnext()
        n = len(srcs)
        for i, s in enumerate(srcs):
            p0, p1 = _box(s)[0], _box(s)[1]
            sq = sqs[(i0 * 2 + i) % 4]
            kb.act(sq[p0:p1, 0:W], s, AF.Square)
            kb.mm(ssp[0:npo, 0:W], onesT[i], sq[p0:p1, 0:W], start=(i == 0), stop=(i == n - 1))
        lnv = lnvs[i0 % 2]
        rstd = rstds[i0 % 2]
        kb.act(lnv[0:npo, 0:W], ssp[0:npo, 0:W], AF.Ln, bias=EPS, scale=1.0 / nfeat)
        kb.act(rstd[0:npo, 0:W], lnv[0:npo, 0:W], AF.Exp, scale=-0.5, bias=lnbias)
        for i, s in enumerate(srcs):
            p0, p1 = _box(s)[0], _box(s)[1]
            kb.stt(outs[i], s, gains[i], rstd[p0:p1, 0:W], ALU.mult, ALU.mult)

    def sincos(x, s_out, c_out, ki, r, h, dve_cos=False):
        kb.ts(ki, x, 1.0 / (2 * PI), None, ALU.mult)
        kb.stt(r, ki, -2 * PI, x, ALU.mult, ALU.add)
        kb.ts(r, r, PI, -PI, ALU.min, ALU.max)
        kb.act(s_out, r, AF.Sin)
        kb.act(h, r, AF.Sin, scale=0.5)
        if dve_cos:
            kb.tt(h, h, h, ALU.mult)
            kb.ts(c_out, h, -2.0, 1.0, ALU.mult, ALU.add)
        else:
            kb.act(h, h, AF.Square)
            kb.act(c_out, h, AF.Identity, scale=-2.0, bias=1.0)

    with contextlib.ExitStack() as s0:
        ohs = kb.sb("ohs", [32, NJ], F32, es=s0)
        rel = kb.sb("rel", [32, 2], F32, es=s0)
        relb = kb.sb("relb", [32, 128], F32, es=s0)
        ebf = [kb.sb(f"ebf{i}", [128, NJ], F32, es=s0) for i in range(3)]
        msk = kb.sb("msk", [128, NJ], F32, es=s0)
        kb.dma("sp", ohs[:], D["oh"][:, :])
        kb.dma("sp", rel[:], D["relmy"][:, :])
        kb.dma("sp", msk[:], D["maskrows"][0:1, :].partition_broadcast(128))
        for r in range(3):
            if r < 2:
                kb.cp(relb[:, :], rel[:, r:r + 1].to_broadcast([32, 128]))
                for j0 in range(0, NJ, 512):
                    n = min(512, NJ - j0)
                    bp = pnext()
                    kb.mm(bp[:, 0:n], relb[:, :], ohs[:, j0:j0 + n])
                    kb.act(ebf[r][:, j0:j0 + n], bp[:, 0:n], AF.Exp)
                kb.tt(ebf[r][:, :], ebf[r][:, :], msk[:, :], ALU.mult)
                srcsb = ebf[r]
            else:
                srcsb = msk
            dst = bass.AP(tensor=scratch, offset=r * 128 * RL, ap=[[RL + 1, 128], [1, NJ]])
            kb.dma("sp", dst, srcsb[:, :])
        lt = kb.sb("lt", [128, 32], F32, es=s0)
        ssum = kb.sb("ssum", [128, 2], F32, es=s0)
        ee = kb.sb("ee", [128, 2], F32, es=s0)
        for i, (a, b) in enumerate((("lq1", "lk1"), ("lq2", "lk2"))):
            oa, ob = PC[a][0], PC[b][0]
            kb.tt(lt[:], pc[:, oa:oa + 32], pc[:, ob:ob + 32], ALU.mult)
            kb.op("dve", lambda i=i: nc.vector.reduce_sum(out=ssum[:, i:i + 1], in_=lt[:], axis=AX.X), [lt[:]], [ssum[:, i:i + 1]])
        kb.act(ee[:], ssum[:], AF.Exp)
        kb.tt(neglam[:], ee[:, 1:2], ee[:, 0:1], ALU.subtract)
        kb.ts(neglam[:], neglam[:], -lam_init, None, ALU.add)
        kb.barrier()

    with contextlib.ExitStack() as s1:
        wmixb = kb.sb("wmixb", [128, 8, 1120], BF16, es=s1)
        wuqb = kb.sb("wuqb", [128, 2, 192], BF16, es=s1)
        wknb = kb.sb("wknb", [128, 128], BF16, es=s1)
        wvmb = kb.sb("wvmb", [128, 128], BF16, es=s1)
        rotb = kb.sb("rotb", [96, 96], BF16, es=s1)
        xbuf = [kb.sb(f"xbuf{i}", [128, 8, CH], F32, es=s1) for i in range(1)]
        hT = kb.sb("hT", [128, 8, CH], BF16, es=s1)
        cqa = kb.sb("cqa", [128, CH], BF16, es=s1)
        cqb = kb.sb("cqb", [64, CH], BF16, es=s1)
        ckvn = kb.sb("ckvn", [128, CH], BF16, es=s1)
        qn = [kb.sb(f"qn{i}", [96, CH], BF16, es=s1) for i in range(2)]
        t1 = [kb.sb(f"rt1_{i}", [96, CH], F32, es=s1) for i in range(1)] * 2
        t2 = [kb.sb(f"rt2_{i}", [96, CH], F32, es=s1) for i in range(1)] * 2
        COSs = [kb.sb(f"COS{i}", [96, CH], BF16, es=s1) for i in range(2)]
        SINs = [kb.sb(f"SIN{i}", [96, CH], BF16, es=s1) for i in range(2)]
        posf = kb.sb("posf", [96, CH], F32, es=s1)
        tmpa = kb.sb("tmpa", [96, CH], F32, es=s1)
        tmpk = kb.sb("tmpk", [96, CH], I32, es=s1)
        tmpr = kb.sb("tmpr", [96, CH], F32, es=s1)
        tmph = tmpa
        posi = tmpk
        for i in range(2):
            kb.memset(COSs[i][0:64, :], 1.0, e="pool")
            kb.memset(SINs[i][0:64, :], 0.0, e="pool")
        wv = D["wmix"].rearrange("(kt p) n -> p kt n", p=128)
        for kt in range(8):
            kb.dma("pool", wmixb[:, kt, :], wv[:, kt, :])
        kb.dma("pool", wuqb[:, 0, :], D["wuq"][0:128, :])
        kb.dma("pool", wuqb[0:64, 1, :], D["wuq"][128:192, :])
        kb.dma("pool", wknb[:], D["wkn"][:, :])
        kb.dma("pool", wvmb[:], D["wvm"][:, :])
        kb.dma("pool", rotb[:], D["rotT"][:, :])
        xv = D["xT"].rearrange("(kt p) t -> p kt t", p=128) if fin is None else None
        hv = [fin[hh].rearrange("(kt p) t -> p kt t", p=128) for hh in range(2)] if fin is not None else None
        ri = [0]

        def proj(c0, n, outp):
            for kt in range(8):
                kb.mm(outp, wmixb[:, kt, c0:c0 + n], hT[:, kt, :], start=(kt == 0), stop=(kt == 7))

        def rope(src, dst, c):
            i = ri[0] % 2
            ri[0] += 1
            rp = pnext()
            kb.mm(rp[0:96, :], rotb[:, :], src)
            kb.tt(t1[i][:], src, COSs[c % 2][:, :], ALU.mult)
            kb.tt(t2[i][:], rp[0:96, :], SINs[c % 2][:, :], ALU.mult)
            kb.tt(dst, t1[i][:], t2[i][:], ALU.add, e="pool")

        warm_pe([0, 1, 2, 3, 4, 5, 6, 7])
        for c in range(NCH):
            cs = slice(c * CH, (c + 1) * CH)
            xc = xbuf[0]
            if fin is None:
                kb.dma("sp", xc[:], xv[:, :, cs])
            else:
                kb.dma("sp", hT[:], hv[c // 4][:, :, (c % 4) * CH:(c % 4 + 1) * CH], extra_r=("hdst",))
            kb.dma("sp", posi[64:96, :], D["pos"][0:1, cs].partition_broadcast(32))
            kb.cp(posf[64:96, :], posi[64:96, :])
            kb.ts(tmpa[64:96, :], posf[64:96, :], col("inv", 0, slice(64, 96)), None, ALU.mult)
            sincos(tmpa[64:96, :], SINs[c % 2][64:96, :], COSs[c % 2][64:96, :], tmpk[64:96, :], tmpr[64:96, :], tmph[64:96, :])
            if fin is None:
                rms_T([xc[:, kt, :] for kt in range(8)], [col("gmix", kt) for kt in range(8)],
                      [hT[:, kt, :] for kt in range(8)], [ones[:, :]] * 8, 1024, 128)
            p = pnext(); proj(0, 128, p[:, :]); kb.act(uT[:, cs], p[:, :], AF.Copy)
            pa = pnext(); proj(128, 128, pa[:, :])
            pb = pnext(); proj(256, 64, pb[0:64, :])
            rms_T([pa[:, :], pb[0:64, :]], [col("gcq", 0), col("gcq", 1, slice(0, 64))], [cqa[:], cqb[:]],
                  [ones[:, :], ones[0:64, :]], 192, 128)
            for h in range(2):
                qp = pnext()
                kb.mm(qp[0:96, :], wuqb[:, 0, 96 * h:96 * h + 96], cqa[:], start=True, stop=False)
                kb.mm(qp[0:96, :], wuqb[0:64, 1, 96 * h:96 * h + 96], cqb[:], start=False, stop=True)
                rms_T([qp[0:96, :]], [col("mgq", 0, slice(0, 96))], [qn[h][:]], [ones[0:96, 0:96]], 96, 96,
                      lnbias=math.log(96 ** -0.5))
                rope(qn[h][:], QT[:, h, cs], c)
            p = pnext(); proj(320, 128, p[:, :])
            rms_T([p[:, :]], [col("gckv", 0)], [ckvn[:]], [ones[:, :]], 128, 128)
            for h in range(2):
                kp = pnext()
                kb.mm(kp[0:64, :], wknb[:, 64 * h:64 * h + 64], ckvn[:])
                proj(448, 32, kp[64:96, :])
                rms_T([kp[0:96, :]], [col("mgk", 0, slice(0, 96))], [qn[h][:]], [ones[0:96, 0:96]], 96, 96)
                rope(qn[h][:], KT[:, h, cs], c)
            vp = pnext()
            for j in range(4):
                kb.mm(vp[:, j * 128:(j + 1) * 128], ckvn[:, j * 128:(j + 1) * 128], wvmb[:, :])
            kb.act(VA[:, 4 * c:4 * c + 4, :, 0:64], vp[:, :].rearrange("p (j h d) -> p j h d", j=4, h=2), AF.Copy)
            p = pnext(); proj(480, 128, p[:, :]); kb.act(xlT[:, cs], p[:, :], AF.Copy)
            p = pnext(); proj(608, 128, p[:, :]); kb.act(ggT[:, cs], p[:, :], AF.Gelu_apprx_tanh)
            p = pnext(); proj(736, 128, p[:, :])
            rms_T([p[:, :]], [col("dgq", 0)], [QdT[:, cs]], [blk32[:, :]], 32, 128, lnbias=math.log(32 ** -0.5))
            p = pnext(); proj(864, 128, p[:, :])
            rms_T([p[:, :]], [col("dgk", 0)], [KdT[:, cs]], [blk32[:, :]], 32, 128)
            vp = pnext()
            for j in range(4):
                for kt in range(8):
                    kb.mm(vp[:, j * 128:(j + 1) * 128], hT[:, kt, j * 128:(j + 1) * 128], wmixb[:, kt, 992:1120],
                          start=(kt == 0), stop=(kt == 7))
            kb.act(VD[:, 4 * c:4 * c + 4, :, 0:64], vp[:, :].rearrange("p (j h d) -> p j h d", j=4, h=2), AF.Copy)
        kb.barrier()

    yi = [0]

    ymi = [0]

    def yout(br, rs, cs, yo_ap):
        if fout is None:
            kb.dma("sp", Y[br, rs, cs], yo_ap)
            return
        q = cs.start // 2048
        tsl = slice(cs.start - 2048 * q, cs.stop - 2048 * q)
        for s in range(2):
            t = ymk[ymi[0] % 4]
            ymi[0] += 1
            kb.ts(t[rs, :], yo_ap, fout["meq"][rs, s:s + 1], None, ALU.mult)
            kb.dma("sp", fout["ysrc"].ap()[q, br, s, rs, tsl], t[rs, :], extra_w=("ysrc",))

    def ynext():
        y = ystage[yi[0] % 3]
        yi[0] += 1
        return y

    with contextlib.ExitStack() as s3:
        wrb = kb.sb("wrb", [128, 128], BF16, es=s3)
        wib = kb.sb("wib", [128, 128], BF16, es=s3)
        kb.dma("pool", wrb[:], D["wr"][:, :])
        kb.dma("pool", wib[:], D["wi"][:, :])
        xc = kb.sb("lxc", [128, T], F32, es=s3)
        xcb = kb.sb("lxcb", [128, T], BF16, es=s3)
        av = kb.sb("lav", [128, T], F32, es=s3)
        inp = kb.sb("linp", [128, T], F32, es=s3)
        lt = [kb.sb(f"lt{i}", [128, CH], F32, es=s3) for i in range(4)]
        spc = kb.sb("spc", [128, 1], F32, es=s3)
        kb.act(spc[:], col("llam", 0), AF.Exp, scale=-1.0)
        kb.act(spc[:], spc[:], AF.Ln, bias=1.0)
        kb.ts(spc[:], spc[:], -8.0, None, ALU.mult)
        kb.ts(xc[:], xlT[:], col("cw", 3), col("cb", 0), ALU.mult, ALU.add)
        for k in range(3):
            sh = 3 - k
            kb.stt(xc[:, sh:T], xlT[:, 0:T - sh], col("cw", k), xc[:, sh:T], ALU.mult, ALU.add)
        kb.act(xcb[:], xc[:], AF.Copy)
        for c in range(NCH):
            cs = slice(c * CH, (c + 1) * CH)
            rp = pnext(); ip = pnext()
            kb.mm(rp[:, :], wrb[:, :], xcb[:, cs])
            kb.mm(ip[:, :], wib[:, :], xcb[:, cs])
            kb.act(lt[0][:], rp[:, :], AF.Sigmoid, bias=col("br", 0))
            kb.act(lt[1][:], ip[:, :], AF.Sigmoid, bias=col("bi", 0))
            kb.act(av[:, cs], lt[0][:], AF.Exp, scale=spc[:, 0:1])
            kb.tt(lt[2][:], av[:, cs], av[:, cs], ALU.mult)
            kb.ts(lt[2][:], lt[2][:], -1.0, 1.0, ALU.mult, ALU.add)
            kb.act(lt[3][:], lt[2][:], AF.Sqrt)
            kb.tt(lt[1][:], lt[1][:], xc[:, cs], ALU.mult, e="pool")
            kb.tt(inp[:, cs], lt[3][:], lt[1][:], ALU.mult)
        for c in range(NCH):
            cs = slice(c * CH, (c + 1) * CH)
            init = 0.0 if c == 0 else inp[:, c * CH - 1:c * CH]
            kb.scan(inp[:, cs], av[:, cs], inp[:, cs], init)
            yo = ynext()
            kb.tt(yo[:], inp[:, cs], ggT[:, cs], ALU.mult, e="pool")
            yout(2, slice(0, 128), cs, yo[:])
        if debug:
            kb.dma("sp", DBG[0], xc[:]); kb.dma("sp", DBG[1], av[:]); kb.dma("sp", DBG[2], inp[:]); kb.dma("sp", DBG[3], xlT[:])
        kb.barrier()

    es_lru.close()

    with contextlib.ExitStack() as s2:
        rep = kb.sb("rep", [32, 3, 512], F32, es=s2)
        bT = kb.sb("bT", [32, 2, 512], F32, es=s2)
        kb.dma("sp", rep[:], D["s5rep"][:, :, :])
        kb.dma("sp", bT[:], D["s5bT"][:, :, :])
        kb.dma("pool", cTb[:], D["s5cT"][:, :, :, :])

        def derived(lr, li, ls, shape, nm, est, keep):
            names = ["dt", "mag", "ang", "sn", "cs", "tmp", "tmp2", "ar1", "ai", "den", "zr", "zi", "th"]
            tt_ = {}
            for n in names:
                if n in keep:
                    tt_[n] = kb.sb(f"{nm}_{n}", shape, F32, es=s2)
            for n in names:
                if n not in keep:
                    tt_[n] = kb.sb(f"{nm}_{n}", shape, F32, es=est)
            dt = tt_["dt"]; mag = tt_["mag"]; ang = tt_["ang"]; sn = tt_["sn"]; cs_ = tt_["cs"]; tmp = tt_["tmp"]; tmp2 = tt_["tmp2"]
            ar1 = tt_["ar1"]; ai = tt_["ai"]; den = tt_["den"]; zr = tt_["zr"]; zi = tt_["zi"]; th = tt_["th"]
            kb.act(dt[:], ls, AF.Exp)
            kb.tt(tmp[:], lr, dt[:], ALU.mult)
            kb.act(mag[:], tmp[:], AF.Exp)
            kb.tt(ang[:], li, dt[:], ALU.mult)
            kii = kb.sb(f"{nm}_kii", shape, I32, es=est)
            sincos(ang[:], sn[:], cs_[:], kii[:], th[:], tmp[:])
            kb.tt(ai[:], mag[:], sn[:], ALU.mult)
            kb.tt(ar1[:], mag[:], cs_[:], ALU.mult)
            kb.ts(ar1[:], ar1[:], -1.0, None, ALU.add)
            kb.tt(den[:], lr, lr, ALU.mult)
            kb.tt(tmp[:], li, li, ALU.mult)
            kb.tt(den[:], den[:], tmp[:], ALU.add)
            kb.recip(den[:], den[:])
            kb.tt(tmp[:], ar1[:], lr, ALU.mult)
            kb.tt(tmp2[:], ai[:], li, ALU.mult)
            kb.tt(tmp[:], tmp[:], tmp2[:], ALU.add)
            kb.tt(zr[:], tmp[:], den[:], ALU.mult)
            kb.tt(tmp[:], ai[:], lr, ALU.mult)
            kb.tt(tmp2[:], ar1[:], li, ALU.mult)
            kb.tt(tmp[:], tmp[:], tmp2[:], ALU.subtract)
            kb.tt(zi[:], tmp[:], den[:], ALU.mult)
            return dict(mag=mag, th=th, zr=zr, zi=zi)

        o_lr, o_li, o_ls = PC["s5lr"][0], PC["s5li"][0], PC["s5ls"][0]
        dc = derived(pc[:, o_lr:o_lr + 4], pc[:, o_li:o_li + 4], pc[:, o_ls:o_ls + 4], [128, 4], "c4", s2,
                     ("mag", "th", "zr", "zi"))
        bbr = kb.sb("bbr", [32, 512], F32, es=s2)
        bbi = kb.sb("bbi", [32, 512], F32, es=s2)
        s2t = contextlib.ExitStack()
        dr = derived(rep[:, 0, :], rep[:, 1, :], rep[:, 2, :], [32, 512], "r32", s2t, ())
        tb = kb.sb("tb", [32, 512], F32, es=s2t)
        kb.tt(bbr[:], dr["zr"][:], bT[:, 0, :], ALU.mult)
        kb.tt(tb[:], dr["zi"][:], bT[:, 1, :], ALU.mult)
        kb.tt(bbr[:], bbr[:], tb[:], ALU.subtract)
        kb.tt(bbi[:], dr["zr"][:], bT[:, 1, :], ALU.mult)
        kb.tt(tb[:], dr["zi"][:], bT[:, 0, :], ALU.mult)
        kb.tt(bbi[:], bbi[:], tb[:], ALU.add)
        kb.barrier()
        s2t.close()
        for st in range(4):
            kb.act(BL[32 * st:32 * st + 32, 0, :], bbr[:, st * 128:(st + 1) * 128], AF.Copy)
            kb.act(BL[32 * st:32 * st + 32, 1, :], bbi[:, st * 128:(st + 1) * 128], AF.Copy)
        kb.cp(magp[:], dc["mag"][:])
        kb.cp(thp[:], dc["th"][:])
        kb.barrier()

    EB = kb.sb("EB", [128, 3, 5, 512], BF16)
    for r in range(3):
        src = bass.AP(tensor=scratch, offset=r * 128 * RL + 127, ap=[[RL, 128], [128, 5], [1, 512]])
        kb.dma("pool", EB[:, r, :, :], src)
    pbanks[0] = [0, 1, 6]
    Qz = [kb.sb(f"Qz{i}", [128, CH], BF16) for i in range(8)]
    for _q in Qz:
        kb.memset(_q[:], 0.0, e="pool")
    Pt = [kb.sb(f"Pt{i}", [128, CH], BF16) for i in range(4)]
    pi_ = [0]
    fin = [kb.sb(f"fin{i}", [128, CH], F32) for i in range(6)]
    SB_ = [PS[0], PS[1], PS[6]]
    sctr = [0]

    def snext():
        p = SB_[sctr[0] % 3]
        sctr[0] += 1
        return p

    def attn_gen():
        LOOK = 2
        for qc in range(NCH):
            qs = slice(qc * CH, (qc + 1) * CH)
            nk = 4 * qc + 4
            OA = [PS[4], PS[5]]
            steps = [(kt, h) for kt in range(nk) for h in range(2)]
            spt = {}

            def qk_mla(i):
                kt, h = steps[i]
                sp_ = snext()
                kb.mm(sp_[:, :], KT[:, h, kt * 128:(kt + 1) * 128], QT[:, h, qs])
                spt[i] = sp_
            for i in range(min(LOOK, len(steps))):
                qk_mla(i)
            for i, (kt, h) in enumerate(steps):
                sp_ = spt.pop(i)
                P = Pt[pi_[0] % 4]; pi_[0] += 1
                kb.act(P[:], sp_[:, :], AF.Exp)
                v = kt - 4 * qc + 1
                if v >= 1:
                    kb.tt(P[:], P[:], EB[:, 2, 4 - v, :], ALU.mult)
                if i + LOOK < len(steps):
                    qk_mla(i + LOOK)
                kb.mm(OA[h][:, :], VA[:, kt, h, :], P[:], start=(kt == 0), stop=(kt == nk - 1))
                yield
            yo = ynext_a()
            for h in range(2):
                rd = fin[h]
                kb.act(rd[0:64, :], OA[h][64:128, :], AF.Ln)
                kb.act(rd[0:64, :], rd[0:64, :], AF.Exp, scale=-1.0)
                kb.tt(yo[64 * h:64 * h + 64, :], OA[h][0:64, :], rd[0:64, :], ALU.mult)
            yout(1, slice(0, 128), qs, yo[:])
            qz = Qz[4 * (qc % 2):4 * (qc % 2) + 4]
            for _i in range(4):
                kb.act(qz[_i][32 * _i:32 * _i + 32, :], QdT[32 * _i:32 * _i + 32, qs], AF.Copy)
            yo = ynext_a()
            for h in range(2):
                OD = [PS[4], PS[5]]
                steps = [(kt, s) for kt in range(nk) for s in range(2)]
                spt = {}

                def qk_d(i):
                    kt, s = steps[i]
                    r0 = 64 * h + 32 * s
                    sp_ = snext()
                    kb.mm(sp_[:, :], KdT[:, kt * 128:(kt + 1) * 128], qz[2 * h + s][:, :])
                    spt[i] = sp_
                for i in range(min(LOOK, len(steps))):
                    qk_d(i)
                for i, (kt, s) in enumerate(steps):
                    v = kt - 4 * qc + 1
                    sp_ = spt.pop(i)
                    P = Pt[pi_[0] % 4]; pi_[0] += 1
                    if v >= 0:
                        kb.act(P[:], sp_[:, :], AF.Exp)
                        kb.tt(P[:], P[:], EB[:, h, 4 - v, :], ALU.mult)
                    else:
                        kb.act(P[:], sp_[:, :], AF.Exp, bias=col("tb31", h))
                    if i + LOOK < len(steps):
                        qk_d(i + LOOK)
                    kb.mm(OD[s][:, :], VD[:, kt, h, :], P[:], start=(kt == 0), stop=(kt == nk - 1))
                    yield
                r1, r2, o1, o2, dd = fin[0], fin[1], fin[2], fin[3], fin[4]
                kb.act(r1[0:64, :], OD[0][64:128, :], AF.Ln)
                kb.act(r1[0:64, :], r1[0:64, :], AF.Exp, scale=-1.0)
                kb.act(r2[0:64, :], OD[1][64:128, :], AF.Ln)
                kb.act(r2[0:64, :], r2[0:64, :], AF.Exp, scale=-1.0)
                kb.tt(o1[0:64, :], OD[0][0:64, :], r1[0:64, :], ALU.mult)
                kb.tt(o2[0:64, :], OD[1][0:64, :], r2[0:64, :], ALU.mult)
                kb.stt(dd[0:64, :], o2[0:64, :], neglam[0:64, 0:1], o1[0:64, :], ALU.mult, ALU.add)
                rms_T([dd[0:64, :]], [col("gsub", 0, slice(0, 64))], [yo[64 * h:64 * h + 64, :]], [ones[0:64, 0:64]], 64, 64,
                      lnbias=math.log(1.0 - lam_init))
            yout(3, slice(0, 128), qs, yo[:])

    p5c = [0]

    def p5next():
        p = PS[2 + (p5c[0] % 2)]
        p5c[0] += 1
        return p

    s2 = contextlib.ExitStack()
    if True:
        iot = kb.sb("iot", [128, CH], F32, es=s2)
        kb.op("pool", lambda: nc.gpsimd.iota(iot[:], pattern=[[1, CH]], base=0, channel_multiplier=0,
                                               allow_small_or_imprecise_dtypes=True), [], [iot[:]])
        basec = kb.sb("basec", [128, 32], F32, es=s2)
        ph0 = [kb.sb(f"ph0_{i}", [128, CH], F32, es=s2) for i in range(2)]
        NB = 2
        cosT = [kb.sb(f"cosT{i}", [128, CH], F32, es=s2) for i in range(NB)]
        sinT = [kb.sb(f"sinT{i}", [128, CH], F32, es=s2) for i in range(NB)]
        pr = [kb.sb(f"pr{i}", [128, CH], F32, es=s2) for i in range(NB)]
        pim = [kb.sb(f"pim{i}", [128, CH], F32, es=s2) for i in range(NB)]
        m = [kb.sb(f"s5m{i}", [128, CH], F32, es=s2) for i in range(4)]
        hrb = [kb.sb(f"hrb{i}", [128, CH], BF16, es=s2) for i in range(2)]
        hib = [kb.sb(f"hib{i}", [128, CH], BF16, es=s2) for i in range(2)]
        ytm = [kb.sb(f"ytm{i}", [128, CH], F32, es=s2) for i in range(2)]
        cmul = kb.sb("cmul", [128, 8], F32, es=s2)
        bx = kb.sb("bx", [128, 32], F32, es=s2)
        bki = kb.sb("bki", [128, 32], I32, es=s2)
        kb.op("pool", lambda: nc.gpsimd.iota(cmul[:], pattern=[[CH, 8]], base=0, channel_multiplier=0,
                                               allow_small_or_imprecise_dtypes=True), [], [cmul[:]])
        for st in range(4):
            kb.ts(bx[:, st * 8:st * 8 + 8], cmul[:], thp[:, st:st + 1], None, ALU.mult)
        kb.ts(bki[:], bx[:], 1.0 / (2 * PI), None, ALU.mult)
        kb.stt(basec[:], bki[:], -2 * PI, bx[:], ALU.mult, ALU.add)
        ski = [kb.sb(f"ski{i}", [128, CH], I32, es=s2) for i in range(2)]
        shh = [kb.sb(f"shh{i}", [128, CH], F32, es=s2) for i in range(2)]
        def s5_gen():
            its = [(st, c) for st in range(4) for c in range(NCH)]
            PRE, PIE, YP = PS[2], PS[3], PS[7]

            def tables(i):
                st, c = its[i]
                b = i % NB
                kb.ts(ph0[b][:], iot[:, :], thp[:, st:st + 1], basec[:, st * 8 + c:st * 8 + c + 1], ALU.mult, ALU.add)
                sincos(ph0[b][:], sinT[b][:], cosT[b][:], ski[b][:], ph0[b][:], shh[b][:], dve_cos=True)

            def bmm(i):
                st, c = its[i]
                cs = slice(c * CH, (c + 1) * CH)
                kb.mm(PRE[:, :], BL[32 * st:32 * st + 32, 0, :], uT[32 * st:32 * st + 32, cs], tile_position=(32 * st, 0))
                kb.mm(PIE[:, :], BL[32 * st:32 * st + 32, 1, :], uT[32 * st:32 * st + 32, cs], tile_position=(32 * st, 0))
            tables(0)
            bmm(0)
            for i, (st, c) in enumerate(its):
                cs = slice(c * CH, (c + 1) * CH)
                b = i % NB
                pb = (i - 1) % NB
                if i + 1 < len(its):
                    tables(i + 1)
                yield
                kb.tt(m[0][:], PRE[:, :], cosT[b][:], ALU.mult)
                kb.tt(m[1][:], PIE[:, :], sinT[b][:], ALU.mult)
                kb.tt(pr[b][:], m[0][:], m[1][:], ALU.add)
                yield
                kb.tt(m[2][:], PIE[:, :], cosT[b][:], ALU.mult)
                kb.tt(m[3][:], PRE[:, :], sinT[b][:], ALU.mult)
                kb.tt(pim[b][:], m[2][:], m[3][:], ALU.subtract)
                if i + 1 < len(its):
                    bmm(i + 1)
                yield
                for buf in (pr, pim):
                    init = 0.0 if c == 0 else buf[pb][:, CH - 1:CH]
                    kb.scan(buf[b][:], magp[:, st:st + 1].to_broadcast([128, CH]), buf[b][:], init)
                yield
                kb.tt(m[0][:], pr[b][:], cosT[b][:], ALU.mult)
                kb.tt(m[1][:], pim[b][:], sinT[b][:], ALU.mult)
                kb.tt(hrb[c % 2][:], m[0][:], m[1][:], ALU.subtract)
                yield
                kb.tt(m[2][:], pr[b][:], sinT[b][:], ALU.mult)
                kb.tt(m[3][:], pim[b][:], cosT[b][:], ALU.mult)
                kb.stt(hib[c % 2][:], m[2][:], -1.0, m[3][:], ALU.mult, ALU.subtract)
                kb.mm(YP[0:32, :], cTb[:, 0, st, :], hrb[c % 2][:], start=True, stop=False)
                kb.mm(YP[0:32, :], cTb[:, 1, st, :], hib[c % 2][:], start=False, stop=True)
                yield
                rs = slice(32 * st, 32 * st + 32)
                yt = ytm[c % 2]
                kb.cp(yt[rs, :], YP[0:32, :])
                kb.stt(yt[rs, :], uT[rs, cs], col("s5d", 0, rs), yt[rs, :], ALU.mult, ALU.add)
                yo = ynext()
                kb.act(yo[rs, :], yt[rs, :], AF.Gelu_apprx_tanh)
                yout(0, rs, cs, yo[rs, :])
                yield

    warm_pe([4, 5])
    g5 = s5_gen()
    ga = attn_gen()
    n_att = sum((4 * qc + 4) * 6 for qc in range(NCH))
    per = max(1, n_att // (32 * 7))
    done5 = False
    donea = False
    while not (done5 and donea):
        if not done5:
            try:
                next(g5)
            except StopIteration:
                done5 = True
        for _ in range(per if not done5 else 10 ** 9):
            try:
                next(ga)
            except StopIteration:
                donea = True
                break
    kb.barrier()
    s2.close()
    kb.barrier()
    kb.es.close()
    kb.es = old_es
    kb.sfx = old_sfx
    if standalone:
        kb.wait_all("sp")
    return kb

import math

TO = 2048
CH = 512
NC4 = TO // CH
EPS = 1e-6
NMEM = 256

P2_LAYOUT = [("gmix", 8), ("gcross", 8), ("gmem", 8), ("gffn", 8), ("bgate", 32), ("bglu", 2), ("xgq", 1), ("xgk", 1)]
P2 = {}
_o = 0
for _n, _w in P2_LAYOUT:
    P2[_n] = (_o, _w)
    _o += _w
NP2 = _o


def build_R(moe, debug=False, kb=None, sfx="", PS=None, xin=None, yin=None, xout=None, hout=None):
    standalone = kb is None
    if standalone:
        kb = KB()
    nc = kb.nc
    old_es = kb.es
    kb.es = contextlib.ExitStack()
    old_sfx = kb.sfx
    kb.sfx = sfx
    _dram = kb.dram
    def kb_dram(name, shape, dt, kind):
        return _dram(name + sfx, shape, dt, kind)
    D = {}
    D["xT"] = kb_dram("xT", [1024, TO], F32, "ExternalInput") if xin is None else xin
    D["yall"] = kb_dram("yall", [4, 256, TO], BF16, "ExternalInput") if yin is None else yin
    D["wg8"] = kb_dram("wg8", [8, 1024, 512], F32, "ExternalInput")
    D["wbr"] = kb_dram("wbr", [4, 256, 1024], F32, "ExternalInput")
    D["wout"] = kb_dram("wout", [1024, 1024], F32, "ExternalInput")
    D["pc2"] = kb_dram("pc2", [128, NP2], F32, "ExternalInput")
    D["wglu"] = kb_dram("wglu", [256, 256], F32, "ExternalInput")
    D["xwq"] = kb_dram("xwq", [1024, 256], F32, "ExternalInput")
    D["xwk"] = kb_dram("xwk", [1024, 256], F32, "ExternalInput")
    D["xwv"] = kb_dram("xwv", [1024, 256], F32, "ExternalInput")
    D["xwo"] = kb_dram("xwo", [256, 1024], F32, "ExternalInput")
    D["memT"] = kb_dram("memT", [1024, NMEM], F32, "ExternalInput")
    if moe:
        NE, FF = 8, 3584
        D["wr"] = kb_dram("wrt", [1024, 8], F32, "ExternalInput")
    else:
        NE, FF = 1, 2816
    D["fg"] = kb_dram("fg", [NE, 1024, FF], F32, "ExternalInput")
    D["fu"] = kb_dram("fu", [NE, 1024, FF], F32, "ExternalInput")
    D["fd"] = kb_dram("fd", [NE, FF, 1024], F32, "ExternalInput")
    XO = kb_dram("xo", [1024, TO], F32, "ExternalOutput") if xout is None else xout
    NFT = FF // 128

    pc = kb.sb("pc", [128, NP2], F32)

    def col(name, i=0, rows=slice(0, 128)):
        o, w = P2[name]
        return pc[rows, o + i:o + i + 1]
    ones = kb.sb("ones", [128, 128], BF16)
    blk64 = kb.sb("blk64", [128, 128], BF16)
    xT = kb.sb("xTs", [128, 8, TO], F32)
    if PS is None:
        PS = [kb.ps(f"ps{i}", [128, 512], F32, es=old_es) for i in range(8)]
    pctr = [0]
    pbanks = [list(range(8))]

    def pnext():
        b = pbanks[0]
        p = PS[b[pctr[0] % len(b)]]
        pctr[0] += 1
        return p
    kb.dma("sp", pc[:], D["pc2"][:, :])
    kb.memset(ones[:], 1.0)
    kb.memset(blk64[:], 0.0, e="pool")
    for g in range(2):
        kb.memset(blk64[64 * g:64 * g + 64, 64 * g:64 * g + 64], 1.0, e="pool")
    xv = D["xT"].rearrange("(kt p) t -> p kt t", p=128)
    for c in range(NC4):
        kb.dma("sp", xT[:, :, c * CH:(c + 1) * CH], xv[:, :, c * CH:(c + 1) * CH], extra_r=("xsp",))

    tmpi = [0]
    sqs = [kb.sb(f"sq{i}", [128, 512], BF16) for i in range(4)]
    lnvs = [kb.sb(f"lnv{i}", [128, 512], F32) for i in range(2)]
    rstds = [kb.sb(f"rstd{i}", [128, 512], F32) for i in range(2)]

    def rms_T(srcs, gains, outs, onesT, nfeat, npo, lnbias=0.0, W=512, rstd_out=None):
        i0 = tmpi[0]
        tmpi[0] += 1
        ssp = pnext()
        n = len(srcs)
        for i, s in enumerate(srcs):
            p0, p1 = _box(s)[0], _box(s)[1]
            sq = sqs[(i0 * 2 + i) % 4]
            kb.act(sq[p0:p1, 0:W], s, AF.Square)
            kb.mm(ssp[0:npo, 0:W], onesT[i], sq[p0:p1, 0:W], start=(i == 0), stop=(i == n - 1))
        lnv = lnvs[i0 % 2]
        rstd = rstds[i0 % 2]
        kb.act(lnv[0:npo, 0:W], ssp[0:npo, 0:W], AF.Ln, bias=EPS, scale=1.0 / nfeat)
        kb.act(rstd[0:npo, 0:W], lnv[0:npo, 0:W], AF.Exp, scale=-0.5, bias=lnbias)
        if rstd_out is not None:
            kb.act(rstd_out, rstd[0:1, 0:W], AF.Copy)
        for i, s in enumerate(srcs):
            p0, p1 = _box(s)[0], _box(s)[1]
            kb.stt(outs[i], s, gains[i], rstd[p0:p1, 0:W], ALU.mult, ALU.mult)

    def norm_x(hT, gname, rstd_row=None):
        for c in range(NC4):
            cs = slice(c * CH, (c + 1) * CH)
            rms_T([xT[:, kt, cs] for kt in range(8)], [col(gname, kt) for kt in range(8)],
                  [hT[:, kt, cs] for kt in range(8)], [ones[:, :]] * 8, 1024, 128,
                  rstd_out=(None if rstd_row is None else rstd_row[32 * c:32 * c + 1, :]))

    def resid_add(ot, cs, ps_ap):
        kb.tt(xT[:, ot, cs], xT[:, ot, cs], ps_ap, ALU.add)

    with contextlib.ExitStack() as e1:
        merged = kb.sb("merged", [128, 8, TO], BF16, es=e1)
        with contextlib.ExitStack() as e2:
            hT = kb.sb("hT", [128, 8, TO], BF16, es=e2)
            yb = kb.sb("yb", [128, 4, 2, TO], BF16, es=e2)
            wbrb = kb.sb("wbrb", [128, 4, 2, 1024], BF16, es=e2)
            wglub = kb.sb("wglub", [128, 2, 256], BF16, es=e2)
            wgb = [kb.sb(f"wgb{i}", [128, 8, 512], BF16, es=e2) for i in range(1)] * 2
            sg = [kb.sb(f"sg{i}", [128, CH], F32, es=e2) for i in range(2)] + [None]
            sg[2] = sg[0]
            tmpm = [kb.sb(f"tmpm{i}", [128, CH], F32, es=e2) for i in range(1)] * 2
            accm = [kb.sb(f"accm{i}", [128, CH], F32, es=e2) for i in range(2)]
            for b in range(4):
                for k2 in range(2):
                    kb.dma("sp", yb[:, b, k2, :], D["yall"][b, 128 * k2:128 * k2 + 128, :], extra_r=("ydst",))
                    kb.dma("pool", wbrb[:, b, k2, :], D["wbr"][b, 128 * k2:128 * k2 + 128, :])
            for k2 in range(2):
                kb.dma("pool", wglub[:, k2, :], D["wglu"][128 * k2:128 * k2 + 128, :])
            norm_x(hT, "gmix")
            for c in range(NC4):
                cs = slice(c * CH, (c + 1) * CH)
                gl = []
                for j in range(2):
                    gp = pnext()
                    for k2 in range(2):
                        kb.mm(gp[:, :], wglub[:, k2, 128 * j:128 * j + 128], yb[:, 0, k2, cs], start=(k2 == 0), stop=(k2 == 1))
                    kb.act(sg[j][:], gp[:, :], AF.Sigmoid, bias=col("bglu", j))
                for j in range(2):
                    kb.tt(yb[:, 0, j, cs], yb[:, 0, j, cs], sg[j][:], ALU.mult)
            gv = D["wg8"].rearrange("d (kt p) n -> d p kt n", p=128)
            si = 0
            for dt in range(8):
                wg = wgb[dt % 2]
                kb.dma("pool", wg[:], gv[dt])
                for c in range(NC4):
                    cs = slice(c * CH, (c + 1) * CH)
                    acc = accm[(dt * NC4 + c) % 2]
                    for b in range(4):
                        up = pnext()
                        for k2 in range(2):
                            kb.mm(up[:, :], wbrb[:, b, k2, 128 * dt:128 * dt + 128], yb[:, b, k2, cs], start=(k2 == 0), stop=(k2 == 1))
                        gp = pnext()
                        for kt in range(8):
                            kb.mm(gp[:, :], wg[:, kt, 128 * b:128 * b + 128], hT[:, kt, cs], start=(kt == 0), stop=(kt == 7))
                        s = sg[si % 2]
                        si += 1
                        kb.act(s[:], gp[:, :], AF.Sigmoid, bias=col("bgate", b * 8 + dt))
                        if b == 0:
                            kb.tt(acc[:], s[:], up[:, :], ALU.mult)
                        elif b < 3:
                            t = tmpm[b % 2]
                            kb.tt(t[:], s[:], up[:, :], ALU.mult)
                            kb.tt(acc[:], acc[:], t[:], ALU.add, e="pool")
                        else:
                            t = tmpm[b % 2]
                            kb.tt(t[:], s[:], up[:, :], ALU.mult)
                            kb.tt(merged[:, dt, cs], acc[:], t[:], ALU.add, e="pool")
            kb.barrier()
        woutb = kb.sb("woutb", [128, 8, 1024], BF16, es=e1)
        kb.dma("pool", woutb[:], D["wout"].rearrange("(kt p) n -> p kt n", p=128))
        for ot in range(8):
            for c in range(NC4):
                cs = slice(c * CH, (c + 1) * CH)
                op_ = pnext()
                for kt in range(8):
                    kb.mm(op_[:, :], woutb[:, kt, 128 * ot:128 * ot + 128], merged[:, kt, cs], start=(kt == 0), stop=(kt == 7))
                resid_add(ot, cs, op_[:, :])
        kb.barrier()
    if debug:
        DBG = kb_dram("dbgxa", [1024, TO], F32, "ExternalOutput")
        kb.dma("sp", DBG.rearrange("(kt p) t -> p kt t", p=128), xT[:])

    with contextlib.ExitStack() as e1:
        hT = kb.sb("hTc", [128, 8, TO], BF16, es=e1)
        wq = kb.sb("wq", [128, 8, 256], BF16, es=e1)
        wk = kb.sb("wk", [128, 8, 256], BF16, es=e1)
        wvv = kb.sb("wvv", [128, 8, 256], BF16, es=e1)
        wo = kb.sb("wo", [128, 2, 1024], BF16, es=e1)
        mT = kb.sb("mT", [128, 8, NMEM], F32, es=e1)
        mh = kb.sb("mh", [128, 8, NMEM], BF16, es=e1)
        QcT = kb.sb("QcT", [128, 2, TO], BF16, es=e1)
        KcT = kb.sb("KcT", [128, 2, NMEM], BF16, es=e1)
        VC = kb.sb("VC", [128, 2, 4, 128], BF16, es=e1)
        OcT = kb.sb("OcT", [128, 2, TO], BF16, es=e1)
        Pt = [kb.sb(f"Pt{i}", [128, CH], BF16, es=e1) for i in range(4)]
        rdt = [kb.sb(f"rdt{i}", [128, CH], F32, es=e1) for i in range(2)]
        for nm, t in (("xwq", wq), ("xwk", wk), ("xwv", wvv)):
            kb.dma("pool", t[:], D[nm].rearrange("(kt p) n -> p kt n", p=128))
        kb.dma("pool", wo[:], D["xwo"].rearrange("(kt p) n -> p kt n", p=128))
        kb.dma("sp", mT[:], D["memT"].rearrange("(kt p) t -> p kt t", p=128))
        kb.memset(VC[:, :, :, 64:128], 1.0, e="pool")
        rms_T([mT[:, kt, :] for kt in range(8)], [col("gmem", kt) for kt in range(8)], [mh[:, kt, :] for kt in range(8)],
              [ones[:, :]] * 8, 1024, 128, W=NMEM)
        for j in range(2):
            kp = pnext()
            for kt in range(8):
                kb.mm(kp[:, 0:NMEM], wk[:, kt, 128 * j:128 * j + 128], mh[:, kt, :], start=(kt == 0), stop=(kt == 7))
            rms_T([kp[:, 0:NMEM]], [col("xgk", 0)], [KcT[:, j, :]], [blk64[:, :]], 64, 128, W=NMEM)
        for mt in range(2):
            vp = pnext()
            for kt in range(8):
                kb.mm(vp[:, 0:256], mh[:, kt, 128 * mt:128 * mt + 128], wvv[:, kt, :], start=(kt == 0), stop=(kt == 7))
            kb.act(VC[:, mt, :, 0:64], vp[:, 0:256].rearrange("p (h d) -> p h d", h=4), AF.Copy)
        norm_x(hT, "gcross")
        for c in range(NC4):
            cs = slice(c * CH, (c + 1) * CH)
            for j in range(2):
                qp = pnext()
                for kt in range(8):
                    kb.mm(qp[:, :], wq[:, kt, 128 * j:128 * j + 128], hT[:, kt, cs], start=(kt == 0), stop=(kt == 7))
                rms_T([qp[:, :]], [col("xgq", 0)], [QcT[:, j, cs]], [blk64[:, :]], 64, 128, lnbias=math.log(64 ** -0.5))
        pbanks[0] = [0, 1, 2, 3]
        pi_ = 0
        for c in range(NC4):
            cs = slice(c * CH, (c + 1) * CH)
            for h in range(4):
                r0 = 64 * (h % 2)
                j = h // 2
                O = PS[4 + h]
                for mt in range(2):
                    sp_ = pnext()
                    kb.mm(sp_[:, :], KcT[r0:r0 + 64, j, 128 * mt:128 * mt + 128], QcT[r0:r0 + 64, j, cs])
                    P = Pt[pi_ % 4]
                    pi_ += 1
                    kb.act(P[:], sp_[:, :], AF.Exp)
                    kb.mm(O[:, :], VC[:, mt, h, :], P[:], start=(mt == 0), stop=(mt == 1))
                rd = rdt[h % 2]
                kb.recip(rd[0:64, :], O[64:128, :])
                kb.tt(OcT[r0:r0 + 64, j, cs], O[0:64, :], rd[0:64, :], ALU.mult)
        pbanks[0] = list(range(8))
        for ot in range(8):
            for c in range(NC4):
                cs = slice(c * CH, (c + 1) * CH)
                op_ = pnext()
                for k2 in range(2):
                    kb.mm(op_[:, :], wo[:, k2, 128 * ot:128 * ot + 128], OcT[:, k2, cs], start=(k2 == 0), stop=(k2 == 1))
                resid_add(ot, cs, op_[:, :])
        kb.barrier()
    if debug:
        DBG2 = kb_dram("dbgxb", [1024, TO], F32, "ExternalOutput")
        kb.dma("sp", DBG2.rearrange("(kt p) t -> p kt t", p=128), xT[:])

    TB = TO
    NTB = 1
    JB = 256 if moe else 128
    WDW = 256
    NH = NFT // 2
    with contextlib.ExitStack() as e1:
        hT = kb.sb("hTf", [128, 8, TO], BF16, es=e1)
        hmid = kb.sb("hmid", [128, NH, TB], BF16, es=e1)
        wgB = [kb.sb(f"wgB{i}", [128, 8, JB], BF16, es=e1) for i in range(2)]
        wuB = [kb.sb(f"wuB{i}", [128, 8, JB], BF16, es=e1) for i in range(2)]
        wdB = [kb.sb(f"wdB{i}", [128, NH, WDW], BF16, es=e1) for i in range(2)]
        av = [kb.sb(f"av{i}", [128, CH], F32, es=e1) for i in range(2 if not moe else 1)] * 2
        tv = [kb.sb(f"tv{i}", [128, CH], F32, es=e1) for i in range(1)] * 2
        rrow = None
        if moe:
            rrow = kb.sb("rrow", [128, CH], F32, es=e1)
            identf = kb.sb("identf", [128, 128], F32, es=e1)
            wrf = kb.sb("wrf", [128, 8, 8], F32, es=e1)
            Wtok = kb.sb("Wtok", [128, TO // 128, 8], F32, es=e1)
            lg = kb.sb("lg", [128, 8], F32, es=e1)
            mx = kb.sb("mx", [128, 8], F32, es=e1)
            ex = kb.sb("ex", [128, 8], F32, es=e1)
            mk = kb.sb("mk", [128, 8], F32, es=e1)
            sm = kb.sb("sm", [128, 4], F32, es=e1)
            rc = kb.sb("rc", [128, 1], F32, es=e1)
            onef = kb.sb("onef", [128, 1], F32, es=e1)
            wbc = kb.sb("wbc", [128, TB], BF16, es=e1)
            wb128 = kb.sb("wb128", [128, 128], F32, es=e1)
            kb.memset(onef[:], 1.0)
            iop = kb.sb("iop", [128, 128], F32, es=e1)
            kb.op("pool", lambda: nc.gpsimd.iota(iop[:], pattern=[[1, 128]], base=0, channel_multiplier=-1,
                                                   allow_small_or_imprecise_dtypes=True), [], [iop[:]])
            kb.ts(identf[:], iop[:], 0.0, None, ALU.is_equal)
            kb.dma("sp", wrf[:], D["wr"].rearrange("(kt p) n -> p kt n", p=128))
            for kt in range(8):
                kb.ts(wrf[:, kt, :], wrf[:, kt, :], col("gffn", kt), None, ALU.mult)
        norm_x(hT, "gffn", rstd_row=rrow)
        if moe:
            for tt_ in range(TO // 128):
                ts_ = slice(128 * tt_, 128 * tt_ + 128)
                lp = pnext()
                for kt in range(8):
                    kb.mm(lp[:, 0:8], xT[:, kt, ts_], wrf[:, kt, :], start=(kt == 0), stop=(kt == 7))
                rp = pnext()
                cq = tt_ // 4
                kb.mm(rp[:, 0:1], rrow[32 * cq:32 * cq + 1, 128 * (tt_ % 4):128 * (tt_ % 4) + 128], onef[32 * cq:32 * cq + 1, 0:1], tile_position=(32 * cq, 0))
                kb.act(rc[:], rp[:, 0:1], AF.Copy)
                kb.ts(lg[:], lp[:, 0:8], rc[:, 0:1], None, ALU.mult)
                kb.op("dve", lambda: nc.vector.max(out=mx[:], in_=lg[:]), [lg[:]], [mx[:]])
                kb.ts(mk[:], lg[:], mx[:, 1:2], None, ALU.is_ge)
                kb.ts(sm[:, 0:1], mx[:, 0:1], -1.0, None, ALU.mult)
                kb.act(ex[:], lg[:], AF.Exp, bias=sm[:, 0:1])
                kb.tt(ex[:], ex[:], mk[:], ALU.mult)
                kb.op("dve", lambda: nc.vector.reduce_sum(out=sm[:, 1:2], in_=ex[:], axis=AX.X), [ex[:]], [sm[:, 1:2]])
                kb.recip(sm[:, 2:3], sm[:, 1:2])
                kb.ts(Wtok[:, tt_, :], ex[:], sm[:, 2:3], None, ALU.mult)
        fgv = D["fg"].rearrange("e (kt p) n -> e p kt n", p=128)
        fuv = D["fu"].rearrange("e (kt p) n -> e p kt n", p=128)
        fdv = D["fd"].rearrange("e (kt p) n -> e p kt n", p=128)
        bi = 0
        di = 0
        ai = 0
        for e in range(NE):
            if moe:
                for q in range(TB // 128):
                    kb.cp(wb128[:, :], Wtok[:, q, e:e + 1].to_broadcast([128, 128]))
                    wp = pnext()
                    kb.mm(wp[:, 0:128], wb128[:, :], identf[:, :])
                    kb.act(wbc[:, 128 * q:128 * q + 128], wp[:, 0:128], AF.Copy)
            for jh in range(2):
                nblk = (NH * 128) // JB
                for jb in range(nblk):
                    wgt = wgB[bi % 2]
                    wut = wuB[bi % 2]
                    bi += 1
                    c0 = jh * NH * 128 + JB * jb
                    kb.dma("pool", wgt[:], fgv[e][:, :, c0:c0 + JB])
                    kb.dma("pool", wut[:], fuv[e][:, :, c0:c0 + JB])
                    for jj in range(JB // 128):
                        jl = jb * (JB // 128) + jj
                        for c2 in range(TB // CH):
                            cs = slice(c2 * CH, (c2 + 1) * CH)
                            gp = pnext()
                            for kt in range(8):
                                kb.mm(gp[:, :], wgt[:, kt, 128 * jj:128 * jj + 128], hT[:, kt, cs], start=(kt == 0), stop=(kt == 7))
                            up = pnext()
                            for kt in range(8):
                                kb.mm(up[:, :], wut[:, kt, 128 * jj:128 * jj + 128], hT[:, kt, cs], start=(kt == 0), stop=(kt == 7))
                            a = av[ai % 2]
                            t = tv[ai % 2]
                            ai += 1
                            kb.act(a[:], gp[:, :], AF.Silu)
                            if moe:
                                kb.tt(t[:], a[:], up[:, :], ALU.mult)
                                kb.tt(hmid[:, jl, cs], t[:], wbc[:, cs], ALU.mult, e="pool")
                            else:
                                kb.tt(hmid[:, jl, cs], a[:], up[:, :], ALU.mult)
                for op2 in range(1024 // WDW):
                    wdt = wdB[di % 2]
                    di += 1
                    kb.dma("pool", wdt[:], fdv[e][:, jh * NH:(jh + 1) * NH, WDW * op2:WDW * (op2 + 1)])
                    for o2 in range(WDW // 128):
                        ot = (WDW // 128) * op2 + o2
                        for c2 in range(TB // CH):
                            cs = slice(c2 * CH, (c2 + 1) * CH)
                            dp = pnext()
                            for jl in range(NH):
                                kb.mm(dp[:, :], wdt[:, jl, 128 * o2:128 * o2 + 128], hmid[:, jl, cs],
                                      start=(jl == 0), stop=(jl == NH - 1))
                            resid_add(ot, cs, dp[:, :])
        kb.barrier()
    xov = XO.rearrange("(kt p) t -> p kt t", p=128)
    for c in range(NC4):
        kb.dma("sp", xov[:, :, c * CH:(c + 1) * CH], xT[:, :, c * CH:(c + 1) * CH], extra_w=("xsp",))
    if hout is not None:
        with contextlib.ExitStack() as eh:
            hTn = kb.sb("hTn", [128, 8, TO], BF16, es=eh)
            hmk = [kb.sb(f"hmk{i}", [128, 8, CH], BF16, es=eh) for i in range(2)]
            for c in range(NC4):
                cs = slice(c * CH, (c + 1) * CH)
                rms_T([xT[:, kt, cs] for kt in range(8)], [hout["g"][:, kt:kt + 1] for kt in range(8)],
                      [hTn[:, kt, cs] for kt in range(8)], [ones[:, :]] * 8, 1024, 128)
                for s in range(2):
                    t = hmk[s]
                    kb.act(t[:], hTn[:, :, cs], AF.Copy, scale=hout["meq"][:, s:s + 1])
                    for q in range(2):
                        kb.dma("sp", hout["hsrc"].ap()[q, s].rearrange("(kt p) t -> p kt t", p=128)[:, :, cs], t[:], extra_w=("hsrc",))
            kb.barrier()
    kb.barrier()
    kb.es.close()
    kb.es = old_es
    kb.sfx = old_sfx
    if standalone:
        kb.wait_all("sp")
    return kb

import math

RG = [[0, 1], [2, 3], [4, 5], [6, 7]]

def build_fused():
    kb = KB()
    nc = kb.nc
    PS = [kb.ps(f"ps{i}", [128, 512], F32) for i in range(8)]
    meq = kb.sb("meq_sb", [128, 2], F32)
    g1 = kb.sb("g1c_sb", [128, 8], F32)
    meq_d = kb.dram("meq", [128, 2], F32, "ExternalInput")
    g1_d = kb.dram("g1c", [128, 8], F32, "ExternalInput")
    kb.dma("sp", meq[:], meq_d[:, :])
    kb.dma("sp", g1[:], g1_d[:, :])
    ysrc = nc.dram_tensor("ysrc", [2, 4, 2, 128, 2048], BF16)
    ydst = nc.dram_tensor("ydst", [4, 256, 2048], BF16)
    hsrc = nc.dram_tensor("hsrc", [2, 2, 1024, 2048], BF16)
    hdst = nc.dram_tensor("hdst", [2, 1024, 2048], BF16)
    xsp = nc.dram_tensor("xsp", [1024, 2048], F32)
    fo = dict(ysrc=ysrc, meq=meq)
    build_M(0.8 - 0.6 * math.exp(-0.3 * 0), kb=kb, sfx="_m0", PS=PS, fin=None, fout=fo)
    kb.collective("ReduceScatter", ALU.add, RG, ysrc, ydst, "ysrc", "ydst")
    build_R(False, kb=kb, sfx="_r0", PS=PS, xin=None, yin=ydst.ap(), xout=xsp.ap(), hout=dict(hsrc=hsrc, meq=meq, g=g1))
    kb.collective("ReduceScatter", ALU.add, RG, hsrc, hdst, "hsrc", "hdst")
    build_M(0.8 - 0.6 * math.exp(-0.3 * 1), kb=kb, sfx="_m1", PS=PS, fin=hdst.ap(), fout=fo)
    kb.collective("ReduceScatter", ALU.add, RG, ysrc, ydst, "ysrc", "ydst")
    build_R(True, kb=kb, sfx="_r1", PS=PS, xin=xsp.ap(), yin=ydst.ap(), xout=None, hout=None)
    kb.wait_all("sp")
    return kb

import math
import numpy as np

SP = [256, 448, 576, 608, 864, 1120, 1376, 1632, 1888]
f32 = np.float32


def bucket_consts():
    dist = np.arange(NJ, dtype=np.int64) - 511
    n = np.maximum(dist, 0)
    exact = 16
    lr = np.log(np.maximum(n, exact).astype(f32) / f32(exact)) / f32(math.log(128 / exact))
    large = np.minimum(exact + (lr * f32(32 - exact)).astype(np.int32), 31)
    bk = np.where(n < exact, n, large)
    oh = np.zeros((32, NJ), f32)
    oh[bk, np.arange(NJ)] = 1.0
    mask = (dist >= 0).astype(f32)
    return oh, np.tile(mask[None], (3, 1))


def rot_consts():
    rotT = np.zeros((96, 96), f32)
    for i in range(16):
        rotT[80 + i, 64 + i] = -1.0
        rotT[64 + i, 80 + i] = 1.0
    inv = (f32(10000.0) ** (-np.arange(16, dtype=f32) / f32(16))).astype(f32)
    return rotT, inv


def colpad(v, rows=128):
    out = np.zeros((rows,), f32)
    out[:len(v)] = v
    return out


def prep_M(I, l, b, p, xT):
    w_in = I["w_in"][l]
    cols = np.concatenate([
        np.arange(128 * p, 128 * p + 128),
        np.arange(256, 448), np.arange(448, 576), np.arange(576, 608),
        np.arange(608 + 128 * p, 608 + 128 * p + 128),
        np.arange(864 + 128 * p, 864 + 128 * p + 128),
        np.arange(1120 + 128 * p, 1120 + 128 * p + 128),
        np.arange(1376 + 128 * p, 1376 + 128 * p + 128),
        np.arange(1632 + 128 * p, 1632 + 128 * p + 128)])
    m = {}
    m["xT"] = np.ascontiguousarray(xT)
    m["wmix"] = np.ascontiguousarray(w_in[:, cols])
    m["pos"] = np.ascontiguousarray(I["positions"][b:b + 1].astype(np.int32))
    hs = [2 * p, 2 * p + 1]
    wuq = I["mla_w_uq"][l]
    m["wuq"] = np.ascontiguousarray(np.concatenate([wuq[:, 96 * h:96 * h + 96] for h in hs], axis=1))
    wukv = I["mla_w_ukv"][l]
    m["wkn"] = np.ascontiguousarray(np.concatenate([wukv[:, 128 * h:128 * h + 64] for h in hs], axis=1))
    m["wvm"] = np.ascontiguousarray(np.concatenate([wukv[:, 128 * h + 64:128 * h + 128] for h in hs], axis=1))
    rotT, inv = rot_consts()
    m["rotT"] = rotT
    for nm, key in (("wr", "lru_w_r"), ("wi", "lru_w_i")):
        w = np.zeros((128, 128), f32)
        for j in range(2):
            w[64 * j:64 * j + 64, 64 * j:64 * j + 64] = I[key][l][2 * p + j]
        m[nm] = w
    gs = slice(8 * p, 8 * p + 8)
    lr = I["s5_lam_re"][l][gs].reshape(512)
    li = I["s5_lam_im"][l][gs].reshape(512)
    ls = np.repeat(I["s5_log_step"][l][gs], 64)
    m["s5rep"] = np.ascontiguousarray(np.tile(np.stack([lr, li, ls])[None], (32, 1, 1)).astype(f32))
    bT = np.zeros((32, 2, 512), f32)
    cT = np.zeros((128, 2, 4, 32), f32)
    for gi in range(8):
        g = 8 * p + gi
        st, half = gi // 2, gi % 2
        for ri, key in enumerate(("s5_b_re", "s5_b_im")):
            bT[16 * half:16 * half + 16, ri, st * 128 + 64 * half: st * 128 + 64 * half + 64] = I[key][l][g].T
        for ri, key in enumerate(("s5_c_re", "s5_c_im")):
            cT[64 * half:64 * half + 64, ri, st, 16 * half:16 * half + 16] = I[key][l][g].T
    m["s5bT"] = bT
    m["s5cT"] = cT
    oh, maskrows = bucket_consts()
    m["oh"] = oh
    m["maskrows"] = maskrows
    m["relmy"] = np.ascontiguousarray(I["rel_table"][:, hs])
    pc = np.zeros((128, NPC), f32)

    def put(name, i, v):
        o, w = PC[name]
        pc[:len(v), o + i] = v
    for kt in range(8):
        put("gmix", kt, I["g_mix"][l][128 * kt:128 * kt + 128])
    put("gcq", 0, I["mla_g_cq"][l][0:128]); put("gcq", 1, I["mla_g_cq"][l][128:192])
    put("gckv", 0, I["mla_g_ckv"][l])
    put("mgq", 0, I["mla_g_qn"][l]); put("mgk", 0, I["mla_g_kn"][l])
    iv = np.zeros(128, f32); iv[64:96] = np.tile(inv, 2); put("inv", 0, iv)
    put("dgq", 0, np.tile(I["diff_g_qn"][l], 4)); put("dgk", 0, np.tile(I["diff_g_kn"][l], 4))
    put("gsub", 0, np.tile(I["diff_g_sub"][l], 2))
    for j, h in enumerate(hs):
        put("tb31", j, np.full(128, I["rel_table"][31, h], f32))
    for st in range(4):
        put("s5lr", st, lr[128 * st:128 * st + 128]); put("s5li", st, li[128 * st:128 * st + 128])
        put("s5ls", st, ls[128 * st:128 * st + 128])
    cs = slice(128 * p, 128 * p + 128)
    put("s5d", 0, I["s5_d"][l][cs])
    for k in range(4):
        put("cw", k, I["lru_conv_w"][l][k][cs])
    put("cb", 0, I["lru_conv_b"][l][cs]); put("br", 0, I["lru_b_r"][l][cs]); put("bi", 0, I["lru_b_i"][l][cs])
    put("llam", 0, I["lru_lam"][l][cs])
    for nm, key in (("lq1", "diff_lq1"), ("lk1", "diff_lk1"), ("lq2", "diff_lq2"), ("lk2", "diff_lk2")):
        o, w = PC[nm]
        pc[:, o:o + 32] = I[key][l][None, :]
    m["pcols"] = pc
    return m

import numpy as np
f32 = np.float32

def prep_R(I, l, b, p, xT_own, yall):
    m = {}
    m["xT"] = np.ascontiguousarray(xT_own)
    m["yall"] = np.ascontiguousarray(yall) if yall is not None else None
    w_in = I["w_in"][l]
    m["wg8"] = np.ascontiguousarray(np.stack([np.concatenate([w_in[:, 1888 + 1024 * bb + 128 * dt:1888 + 1024 * bb + 128 * dt + 128] for bb in range(4)], axis=1) for dt in range(8)]))
    m["wbr"] = np.ascontiguousarray(I["w_branch"][l])
    m["wout"] = np.ascontiguousarray(I["w_out"][l])
    m["wglu"] = np.ascontiguousarray(I["s5_w_glu"][l])
    for nm, key in (("xwq", "x_wq"), ("xwk", "x_wk"), ("xwv", "x_wv"), ("xwo", "x_wo")):
        m[nm] = np.ascontiguousarray(I[key][l])
    m["memT"] = np.ascontiguousarray(I["mem"][b].T)
    pc = np.zeros((128, NP2), f32)
    def put(name, i, v):
        o, w = P2[name]
        pc[:len(v), o + i] = v
    for nm, key in (("gmix", "g_mix"), ("gcross", "g_cross"), ("gmem", "g_mem"), ("gffn", "g_ffn")):
        for kt in range(8):
            put(nm, kt, I[key][l][128 * kt:128 * kt + 128])
    for bb in range(4):
        for dt in range(8):
            put("bgate", bb * 8 + dt, I["b_gate"][l][bb][128 * dt:128 * dt + 128])
    for j in range(2):
        put("bglu", j, I["s5_b_glu"][l][128 * j:128 * j + 128])
    put("xgq", 0, np.tile(I["x_g_qn"][l], 2)); put("xgk", 0, np.tile(I["x_g_kn"][l], 2))
    m["pc2"] = pc
    if l % 2 == 0:
        m["fg"] = np.ascontiguousarray(I["ffn_w_gate"][l // 2][None]); m["fu"] = np.ascontiguousarray(I["ffn_w_up"][l // 2][None])
        m["fd"] = np.ascontiguousarray(I["ffn_w_down"][l // 2][None])
    else:
        m["fg"] = I["moe_w_gate"][l // 2]; m["fu"] = I["moe_w_up"][l // 2]; m["fd"] = I["moe_w_down"][l // 2]
        m["wrt"] = np.ascontiguousarray(I["moe_w_router"][l // 2])
    return m

import numpy as np
f32 = np.float32

def prep_fused(I, c):
    b, p = c // 2, c % 2
    m = {}
    xT = np.ascontiguousarray(I["x"][b].T)
    ts = slice(2048 * p, 2048 * p + 2048)
    for l in range(2):
        mm = prep_M(I, l, b, p, xT)
        if l == 1:
            mm.pop("xT")
        for k, v in mm.items():
            m[k + f"_m{l}"] = v
        mr = prep_R(I, l, b, p, xT[:, ts], None)
        mr.pop("yall")
        if l == 1:
            mr.pop("xT")
        for k, v in mr.items():
            m[k + f"_r{l}"] = v
    meq = np.zeros((128, 2), f32)
    meq[:, p] = 1.0
    m["meq"] = meq
    m["g1c"] = np.ascontiguousarray(I["g_mix"][1].reshape(8, 128).T)
    return m


def kernel(**inputs):
    I = {k: np.asarray(v) for k, v in inputs.items()}
    kb = build_fused()
    maps = [prep_fused(I, c) for c in range(8)]
    res = kb.run(maps, n=8)
    out = np.empty((4, 4096, 1024), np.float32)
    for c in range(8):
        b, p = c // 2, c % 2
        out[b, 2048 * p:2048 * p + 2048, :] = res.results[c]["xo_r1"].T
    return out
```

```python
import contextlib
import numpy as np
import concourse.bass as bass
import concourse.mybir as mybir
from concourse.bass_utils import run_bass_kernel_spmd

F32 = mybir.dt.float32
BF16 = mybir.dt.bfloat16
I32 = mybir.dt.int32
AF = mybir.ActivationFunctionType
ALU = mybir.AluOpType
AX = mybir.AxisListType


def _box(ap):
    t = ap.tensor
    shp = list(t.shape)
    row = 1
    for s in shp[1:]:
        row *= s
    off = ap.offset
    p0 = off // row
    f0 = off % row
    dims = list(ap.ap)
    pstride, pcount = dims[0]
    if pstride == 0:
        pcount = 1
    ext = 0
    for st, cn in dims[1:]:
        ext += abs(st) * (cn - 1)
    return (p0, p0 + pcount, f0, f0 + ext + 1)


def _ov(a, b):
    return a[0] < b[1] and b[0] < a[1] and a[2] < b[3] and b[2] < a[3]


def _covers(a, b):
    return a[0] <= b[0] and a[1] >= b[1] and a[2] <= b[2] and a[3] >= b[3]


class KB:
    NDMA = 32

    def __init__(self):
        self.nc = bass.Bass("TRN2", target_bir_lowering=False)
        nc = self.nc
        self.es = contextlib.ExitStack()
        self.eng = {"pe": nc.tensor, "act": nc.scalar, "dve": nc.vector, "pool": nc.gpsimd, "sp": nc.sync}
        self.sem = {}
        self.cnt = {}
        for e in ("pe", "act", "dve", "pool"):
            self.sem[e] = self.es.enter_context(nc.semaphore("sem_" + e))
            self.cnt[e] = 0
        self.dsem = [self.es.enter_context(nc.semaphore(f"sem_dma{i}")) for i in range(self.NDMA)]
        for i, s in enumerate(self.dsem):
            self.sem[f"d{i}"] = s
            self.cnt[f"d{i}"] = 0
        self.sem["cc"] = self.es.enter_context(nc.semaphore("sem_cc"))
        self.cnt["cc"] = 0
        self.dnext = 0
        self.dnext_sw = 0
        self.waited = {e: {} for e in self.eng}
        self.rec = {}
        self.nins = {e: 0 for e in self.eng}
        self.same_engine_sync = {"dve": True, "pool": True, "act": True, "pe": False, "sp": True}

    sfx = ""

    def sb(self, name, shape, dtype, es=None):
        return (es or self.es).enter_context(self.nc.sbuf_tensor(name + self.sfx, list(shape), dtype))

    def ps(self, name, shape, dtype=F32, es=None):
        return (es or self.es).enter_context(self.nc.psum_tensor(name, list(shape), dtype))

    def dram(self, name, shape, dtype, kind):
        return self.nc.dram_tensor(name, list(shape), dtype, kind=kind).ap()

    def _tracked(self, ap):
        nm = type(ap.tensor).__name__
        return nm.startswith("SB") or nm.startswith("PSum")

    def _collect(self, reads, writes, extra_r=(), extra_w=()):
        deps = {}

        def add(d):
            for k, v in d.items():
                if deps.get(k, 0) < v:
                    deps[k] = v
        for ap in reads:
            if ap is None or not self._tracked(ap):
                continue
            b = _box(ap)
            for r in self.rec.get(ap.tensor.name, ()):
                if _ov(r[0], b):
                    add(r[1])
        for ap in writes:
            if ap is None or not self._tracked(ap):
                continue
            b = _box(ap)
            for r in self.rec.get(ap.tensor.name, ()):
                if _ov(r[0], b):
                    add(r[1])
                    add(r[2])
        for key in extra_r:
            for r in self.rec.get(key, ()):
                add(r[1])
        for key in extra_w:
            for r in self.rec.get(key, ()):
                add(r[1]); add(r[2])
        return deps

    def _emit_waits(self, e, deps, force=False):
        eng = self.eng[e]
        w = self.waited[e]
        for k, v in deps.items():
            if k == e and not force and not self.same_engine_sync.get(e, True):
                continue
            if w.get(k, 0) >= v:
                continue
            eng.wait_ge(self.sem[k], v)
            self.nins[e] += 1
            w[k] = v

    def _update(self, ev, reads, writes, extra_r=(), extra_w=()):
        k, v = ev
        for ap in reads:
            if ap is None or not self._tracked(ap):
                continue
            b = _box(ap)
            lst = self.rec.setdefault(ap.tensor.name, [])
            for r in lst:
                if _ov(r[0], b):
                    if r[2].get(k, 0) < v:
                        r[2][k] = v
        for ap in writes:
            if ap is None or not self._tracked(ap):
                continue
            b = _box(ap)
            lst = self.rec.setdefault(ap.tensor.name, [])
            new = [r for r in lst if not _covers(b, r[0])]
            new.append([b, {k: v}, {}])
            self.rec[ap.tensor.name] = new
        for key in extra_r:
            for r in self.rec.setdefault(key, []):
                if r[2].get(k, 0) < v:
                    r[2][k] = v
        for key in extra_w:
            self.rec[key] = [[(0, 1, 0, 1), {k: v}, {}]]

    def op(self, e, fn, reads, writes, extra_r=(), extra_w=()):
        deps = self._collect(reads, writes, extra_r, extra_w)
        self._emit_waits(e, deps)
        ins = fn()
        self.cnt[e] += 1
        ins.then_inc(self.sem[e], 1)
        self.nins[e] += 1
        ev = (e, self.cnt[e])
        self._update(ev, reads, writes, extra_r, extra_w)
        return ev

    def dma(self, q, out, in_, extra_r=(), extra_w=(), **kw):
        half = self.NDMA // 2
        if q == "pool":
            i = half + self.dnext_sw
            self.dnext_sw = (self.dnext_sw + 1) % (self.NDMA - half)
        else:
            i = self.dnext
            self.dnext = (self.dnext + 1) % half
        dk = f"d{i}"
        deps = self._collect([in_], [out], extra_r, extra_w)
        if self.cnt[dk] > 0:
            deps[dk] = max(deps.get(dk, 0), self.cnt[dk])
        self._emit_waits(q, deps)
        ins = self.eng[q].dma_start(out=out, in_=in_, **kw)
        self.cnt[dk] += 16
        ins.then_inc(self.sem[dk], 16)
        self.nins[q] += 1
        ev = (dk, self.cnt[dk])
        self._update(ev, [in_], [out], extra_r, extra_w)
        return ev

    def collective(self, kind, op, rg, src, dst, rkey, wkey):
        deps = self._collect([], [], extra_r=(rkey,), extra_w=(wkey,))
        self._emit_waits("pool", deps)
        ins = self.nc.gpsimd.collective_compute(kind, op, replica_groups=rg, ins=[src.ap().opt()], outs=[dst.ap().opt()])
        self.cnt["cc"] += 1
        ins.then_inc(self.sem["cc"], 1)
        self.nins["pool"] += 1
        self._update(("cc", self.cnt["cc"]), [], [], extra_r=(rkey,), extra_w=(wkey,))

    def barrier(self):
        deps = {k: v for k, v in self.cnt.items() if v > 0}
        for e in self.eng:
            self._emit_waits(e, dict(deps), force=True)
        self.rec = {}

    def wait_all(self, e="sp"):
        deps = {k: v for k, v in self.cnt.items() if v > 0}
        self._emit_waits(e, deps)

    def mm(self, out, lhsT, rhs, start=True, stop=True, **kw):
        return self.op("pe", lambda: self.nc.tensor.matmul(out, lhsT=lhsT, rhs=rhs, start=start, stop=stop, **kw),
                       [lhsT, rhs] + ([] if start else [out]), [out])

    def tr(self, out, in_, ident):
        return self.op("pe", lambda: self.nc.tensor.transpose(out, in_, ident), [in_, ident], [out])

    def act(self, out, in_, func, bias=0.0, scale=1.0, accum_out=None, e="act"):
        rd = [in_]
        kw = {}
        if not isinstance(bias, (int, float)):
            rd.append(bias)
        if not isinstance(scale, (int, float)):
            rd.append(scale)
        wr = [out]
        if accum_out is not None:
            wr.append(accum_out)
            kw["accum_out"] = accum_out
        return self.op("act", lambda: self.nc.scalar.activation(out=out, in_=in_, func=func, bias=bias, scale=scale, **kw), rd, wr)

    def tt(self, out, in0, in1, op, e="dve"):
        return self.op(e, lambda: self.eng[e].tensor_tensor(out=out, in0=in0, in1=in1, op=op), [in0, in1], [out])

    def ts(self, out, in0, s1, s2, op0, op1=None, e="dve", accum_out=None):
        rd = [in0]
        if not isinstance(s1, (int, float)):
            rd.append(s1)
        if s2 is not None and not isinstance(s2, (int, float)):
            rd.append(s2)
        kw = {}
        if op1 is not None:
            kw["op1"] = op1
        wr = [out]
        if accum_out is not None:
            kw["accum_out"] = accum_out
            wr.append(accum_out)
        return self.op(e, lambda: self.eng[e].tensor_scalar(out=out, in0=in0, scalar1=s1, scalar2=s2, op0=op0, **kw), rd, wr)

    def stt(self, out, in0, scalar, in1, op0, op1, e="dve"):
        rd = [in0, in1]
        if not isinstance(scalar, (int, float)):
            rd.append(scalar)
        return self.op(e, lambda: self.eng[e].scalar_tensor_tensor(out=out, in0=in0, scalar=scalar, in1=in1, op0=op0, op1=op1), rd, [out])

    def cp(self, out, in_, e="dve"):
        return self.op(e, lambda: self.eng[e].tensor_copy(out=out, in_=in_), [in_], [out])

    def memset(self, ap, val, e="dve"):
        return self.op(e, lambda: self.eng[e].memset(ap, val), [], [ap])

    def recip(self, out, in_):
        return self.op("dve", lambda: self.nc.vector.reciprocal(out=out, in_=in_), [in_], [out])

    def scan(self, out, d0, d1, initial, op0=ALU.mult, op1=ALU.add):
        rd = [d0, d1]
        if not isinstance(initial, (int, float)):
            rd.append(initial)
        return self.op("dve", lambda: self.nc.vector.tensor_tensor_scan(out=out, data0=d0, data1=d1, initial=initial, op0=op0, op1=op1), rd, [out])

    def run(self, in_maps, n=8, trace=False):
        res = run_bass_kernel_spmd(self.nc, in_maps, core_ids=list(range(n)), trace=trace)
        return res

import math

T = 4096
CH = 512
NCH = T // CH
PI = math.pi
EPS = 1e-6
NJ = 1151

PC_LAYOUT = [("gmix", 8), ("gcq", 2), ("gckv", 1), ("mgq", 1), ("mgk", 1), ("inv", 1), ("dgq", 1), ("dgk", 1),
             ("gsub", 1), ("tb31", 2), ("s5lr", 4), ("s5li", 4), ("s5ls", 4), ("s5d", 1),
             ("cw", 4), ("cb", 1), ("br", 1), ("bi", 1), ("llam", 1),
             ("lq1", 32), ("lk1", 32), ("lq2", 32), ("lk2", 32)]
PC = {}
_o = 0
for _n, _w in PC_LAYOUT:
    PC[_n] = (_o, _w)
    _o += _w
NPC = _o


def build_M(lam_init, debug=False, kb=None, sfx="", PS=None, fin=None, fout=None):
    standalone = kb is None
    if standalone:
        kb = KB()
    nc = kb.nc
    old_es = kb.es
    kb.es = contextlib.ExitStack()
    old_sfx = kb.sfx
    kb.sfx = sfx
    _dram = kb.dram
    class _K:
        pass
    def dram(name, shape, dt, kind):
        return _dram(name + sfx, shape, dt, kind)
    kb_dram = dram
    D = {}
    if fin is None:
        D["xT"] = kb_dram("xT", [1024, T], F32, "ExternalInput")
    D["wmix"] = kb_dram("wmix", [1024, 1120], F32, "ExternalInput")
    D["pcols"] = kb_dram("pcols", [128, NPC], F32, "ExternalInput")
    D["pos"] = kb_dram("pos", [1, T], I32, "ExternalInput")
    D["wuq"] = kb_dram("wuq", [192, 192], F32, "ExternalInput")
    D["wkn"] = kb_dram("wkn", [128, 128], F32, "ExternalInput")
    D["wvm"] = kb_dram("wvm", [128, 128], F32, "ExternalInput")
    D["rotT"] = kb_dram("rotT", [96, 96], F32, "ExternalInput")
    D["wr"] = kb_dram("wr", [128, 128], F32, "ExternalInput")
    D["wi"] = kb_dram("wi", [128, 128], F32, "ExternalInput")
    D["s5rep"] = kb_dram("s5rep", [32, 3, 512], F32, "ExternalInput")
    D["s5bT"] = kb_dram("s5bT", [32, 2, 512], F32, "ExternalInput")
    D["s5cT"] = kb_dram("s5cT", [128, 2, 4, 32], F32, "ExternalInput")
    D["oh"] = kb_dram("oh", [32, NJ], F32, "ExternalInput")
    D["maskrows"] = kb_dram("maskrows", [3, NJ], F32, "ExternalInput")
    D["relmy"] = kb_dram("relmy", [32, 2], F32, "ExternalInput")
    Y = kb_dram("Y", [4, 128, T], BF16, "ExternalOutput") if fout is None else None
    DBG = kb_dram("DBG", [4, 128, T], F32, "ExternalOutput") if debug else None
    RL = NJ + 128
    scratch = nc.dram_tensor("ebscratch" + sfx, [3, 128, RL], F32, kind="Internal")

    es = kb.es
    pc = kb.sb("pc", [128, NPC], F32)
    def col(name, i=0, rows=slice(0, 128)):
        o, w = PC[name]
        return pc[rows, o + i:o + i + 1]
    ones = kb.sb("ones", [128, 128], BF16)
    blk32 = kb.sb("blk32", [128, 128], BF16)
    uT = kb.sb("uT", [128, T], BF16)
    QT = kb.sb("QT", [96, 2, T], BF16)
    KT = kb.sb("KT", [96, 2, T], BF16)
    QdT = kb.sb("QdT", [128, T], BF16)
    KdT = kb.sb("KdT", [128, T], BF16)
    VA = kb.sb("VA", [128, 32, 2, 128], BF16)
    VD = kb.sb("VD", [128, 32, 2, 128], BF16)
    neglam = kb.sb("neglam", [128, 1], F32)
    if PS is None:
        PS = [kb.ps(f"ps{i}", [128, 512], F32, es=old_es) for i in range(8)]
    pctr = [0]
    pbanks = [list(range(8))]

    def pnext():
        b = pbanks[0]
        p = PS[b[pctr[0] % len(b)]]
        pctr[0] += 1
        return p

    kb.dma("sp", pc[:], D["pcols"][:, :])
    kb.memset(ones[:], 1.0)
    wtile = kb.sb("wtile", [128, 512], BF16)
    kb.memset(wtile[:], 1.0)

    def warm_pe(banks, n=24):
        for i in range(n):
            kb.mm(PS[banks[i % len(banks)]][:, :], ones[:, :], wtile[:, :])

    kb.memset(blk32[:], 0.0, e="pool")
    for g in range(4):
        kb.memset(blk32[32 * g:32 * g + 32, 32 * g:32 * g + 32], 1.0, e="pool")
    kb.memset(VA[:, :, :, 64:128], 1.0, e="pool")
    kb.memset(VD[:, :, :, 64:128], 1.0, e="pool")

    ystage = [kb.sb(f"yst{i}", [128, CH], BF16) for i in range(3)]
    ystage_a = [kb.sb(f"ysta{i}", [128, CH], BF16) for i in range(2)]
    yai = [0]

    def ynext_a():
        y = ystage_a[yai[0] % 2]
        yai[0] += 1
        return y
    ymk = [kb.sb(f"ymk{i}", [128, CH], BF16) for i in range(4)] if fout is not None else None
    BL = kb.sb("BL", [128, 2, 128], BF16)
    cTb = kb.sb("cTb", [128, 2, 4, 32], BF16)
    magp = kb.sb("magp", [128, 4], F32)
    thp = kb.sb("thp", [128, 4], F32)
    tmpi = [0]
    sqs = [kb.sb(f"sq{i}", [128, 512], BF16) for i in range(4)]
    lnvs = [kb.sb(f"lnv{i}", [128, 512], F32) for i in range(2)]
    rstds = [kb.sb(f"rstd{i}", [128, 512], F32) for i in range(2)]
    es_lru = contextlib.ExitStack()
    xlT = kb.sb("xlT", [128, T], F32, es=es_lru)
    ggT = kb.sb("ggT", [128, T], BF16, es=es_lru)

    def rms_T(srcs, gains, outs, onesT, nfeat, npo, lnbias=0.0, W=512):
        i0 = tmpi[0]
        tmpi[0] += 1
        ssp = pnext()
        n = len(srcs)
        for i, s in enumerate(srcs):
            p0, p1 = _box(s)[0], _box(s)[1]
            sq = sqs[(i0 * 2 + i) % 4]
            kb.act(sq[p0:p1, 0:W], s, AF.Square)
            kb.mm(ssp[0:npo, 0:W], onesT[i], sq[p0:p1, 0:W], start=(i == 0), stop=(i == n - 1))
        lnv = lnvs[i0 % 2]
        rstd = rstds[i0 % 2]
        kb.act(lnv[0:npo, 0:W], ssp[0:npo, 0:W], AF.Ln, bias=EPS, scale=1.0 / nfeat)
        kb.act(rstd[0:npo, 0:W], lnv[0:npo, 0:W], AF.Exp, scale=-0.5, bias=lnbias)
        for i, s in enumerate(srcs):
            p0, p1 = _box(s)[0], _box(s)[1]
            kb.stt(outs[i], s, gains[i], rstd[p0:p1, 0:W], ALU.mult, ALU.mult)

    def sincos(x, s_out, c_out, ki, r, h, dve_cos=False):
        kb.ts(ki, x, 1.0 / (2 * PI), None, ALU.mult)
        kb.stt(r, ki, -2 * PI, x, ALU.mult, ALU.add)
        kb.ts(r, r, PI, -PI, ALU.min, ALU.max)
        kb.act(s_out, r, AF.Sin)
        kb.act(h, r, AF.Sin, scale=0.5)
        if dve_cos:
            kb.tt(h, h, h, ALU.mult)
            kb.ts(c_out, h, -2.0, 1.0, ALU.mult, ALU.add)
        else:
            kb.act(h, h, AF.Square)
            kb.act(c_out, h, AF.Identity, scale=-2.0, bias=1.0)

    with contextlib.ExitStack() as s0:
        ohs = kb.sb("ohs", [32, NJ], F32, es=s0)
        rel = kb.sb("rel", [32, 2], F32, es=s0)
        relb = kb.sb("relb", [32, 128], F32, es=s0)
        ebf = [kb.sb(f"ebf{i}", [128, NJ], F32, es=s0) for i in range(3)]
        msk = kb.sb("msk", [128, NJ], F32, es=s0)
        kb.dma("sp", ohs[:], D["oh"][:, :])
        kb.dma("sp", rel[:], D["relmy"][:, :])
        kb.dma("sp", msk[:], D["maskrows"][0:1, :].partition_broadcast(128))
        for r in range(3):
            if r < 2:
                kb.cp(relb[:, :], rel[:, r:r + 1].to_broadcast([32, 128]))
                for j0 in range(0, NJ, 512):
                    n = min(512, NJ - j0)
                    bp = pnext()
                    kb.mm(bp[:, 0:n], relb[:, :], ohs[:, j0:j0 + n])
                    kb.act(ebf[r][:, j0:j0 + n], bp[:, 0:n], AF.Exp)
                kb.tt(ebf[r][:, :], ebf[r][:, :], msk[:, :], ALU.mult)
                srcsb = ebf[r]
            else:
                srcsb = msk
            dst = bass.AP(tensor=scratch, offset=r * 128 * RL, ap=[[RL + 1, 128], [1, NJ]])
            kb.dma("sp", dst, srcsb[:, :])
        lt = kb.sb("lt", [128, 32], F32, es=s0)
        ssum = kb.sb("ssum", [128, 2], F32, es=s0)
        ee = kb.sb("ee", [128, 2], F32, es=s0)
        for i, (a, b) in enumerate((("lq1", "lk1"), ("lq2", "lk2"))):
            oa, ob = PC[a][0], PC[b][0]
            kb.tt(lt[:], pc[:, oa:oa + 32], pc[:, ob:ob + 32], ALU.mult)
            kb.op("dve", lambda i=i: nc.vector.reduce_sum(out=ssum[:, i:i + 1], in_=lt[:], axis=AX.X), [lt[:]], [ssum[:, i:i + 1]])
        kb.act(ee[:], ssum[:], AF.Exp)
        kb.tt(neglam[:], ee[:, 1:2], ee[:, 0:1], ALU.subtract)
        kb.ts(neglam[:], neglam[:], -lam_init, None, ALU.add)
        kb.barrier()

    with contextlib.ExitStack() as s1:
        wmixb = kb.sb("wmixb", [128, 8, 1120], BF16, es=s1)
        wuqb = kb.sb("wuqb", [128, 2, 192], BF16, es=s1)
        wknb = kb.sb("wknb", [128, 128], BF16, es=s1)
        wvmb = kb.sb("wvmb", [128, 128], BF16, es=s1)
        rotb = kb.sb("rotb", [96, 96], BF16, es=s1)
        if fin is None:
            xbuf = [kb.sb(f"xbuf{i}", [128, 8, CH], F32, es=s1) for i in range(1)]
            hTs = [kb.sb("hT", [128, 8, CH], BF16, es=s1)] * 2
        else:
            xbuf = [None]
            hTs = [kb.sb(f"hT{i}", [128, 8, CH], BF16, es=s1) for i in range(2)]
        hT = hTs[0]
        cqa = kb.sb("cqa", [128, CH], BF16, es=s1)
        cqb = kb.sb("cqb", [64, CH], BF16, es=s1)
        ckvn = kb.sb("ckvn", [128, CH], BF16, es=s1)
        qn = [kb.sb(f"qn{i}", [96, CH], BF16, es=s1) for i in range(2)]
        t1 = [kb.sb(f"rt1_{i}", [96, CH], F32, es=s1) for i in range(1)] * 2
        t2 = [kb.sb(f"rt2_{i}", [96, CH], F32, es=s1) for i in range(1)] * 2
        COSs = [kb.sb(f"COS{i}", [96, CH], BF16, es=s1) for i in range(2)]
        SINs = [kb.sb(f"SIN{i}", [96, CH], BF16, es=s1) for i in range(2)]
        posf = kb.sb("posf", [96, CH], F32, es=s1)
        tmpa = kb.sb("tmpa", [96, CH], F32, es=s1)
        tmpk = kb.sb("tmpk", [96, CH], I32, es=s1)
        tmpr = kb.sb("tmpr", [96, CH], F32, es=s1)
        tmph = tmpa
        posi = tmpk
        for i in range(2):
            kb.memset(COSs[i][0:64, :], 1.0, e="pool")
            kb.memset(SINs[i][0:64, :], 0.0, e="pool")
        wv = D["wmix"].rearrange("(kt p) n -> p kt n", p=128)
        for kt in range(8):
            kb.dma("pool", wmixb[:, kt, :], wv[:, kt, :])
        kb.dma("pool", wuqb[:, 0, :], D["wuq"][0:128, :])
        kb.dma("pool", wuqb[0:64, 1, :], D["wuq"][128:192, :])
        kb.dma("pool", wknb[:], D["wkn"][:, :])
        kb.dma("pool", wvmb[:], D["wvm"][:, :])
        kb.dma("pool", rotb[:], D["rotT"][:, :])
        xv = D["xT"].rearrange("(kt p) t -> p kt t", p=128) if fin is None else None
        hv = [fin[hh].rearrange("(kt p) t -> p kt t", p=128) for hh in range(2)] if fin is not None else None
        ri = [0]

        def proj(c0, n, outp):
            for kt in range(8):
                kb.mm(outp, wmixb[:, kt, c0:c0 + n], hT[:, kt, :], start=(kt == 0), stop=(kt == 7))

        def rope(src, dst, c):
            i = ri[0] % 2
            ri[0] += 1
            rp = pnext()
            kb.mm(rp[0:96, :], rotb[:, :], src)
            kb.tt(t1[i][:], src, COSs[c % 2][:, :], ALU.mult)
            kb.tt(t2[i][:], rp[0:96, :], SINs[c % 2][:, :], ALU.mult)
            kb.tt(dst, t1[i][:], t2[i][:], ALU.add, e="pool")

        warm_pe([0, 1, 2, 3, 4, 5, 6, 7])
        for c in range(NCH):
            cs = slice(c * CH, (c + 1) * CH)
            xc = xbuf[0]
            hT = hTs[c % 2]
            if fin is None:
                kb.dma("sp", xc[:], xv[:, :, cs])
            else:
                kb.dma("sp", hT[:], hv[c // 4][:, :, (c % 4) * CH:(c % 4 + 1) * CH], extra_r=("hdst",))
            kb.dma("sp", posi[64:96, :], D["pos"][0:1, cs].partition_broadcast(32))
            kb.cp(posf[64:96, :], posi[64:96, :])
            kb.ts(tmpa[64:96, :], posf[64:96, :], col("inv", 0, slice(64, 96)), None, ALU.mult)
            sincos(tmpa[64:96, :], SINs[c % 2][64:96, :], COSs[c % 2][64:96, :], tmpk[64:96, :], tmpr[64:96, :], tmph[64:96, :])
            if fin is None:
                rms_T([xc[:, kt, :] for kt in range(8)], [col("gmix", kt) for kt in range(8)],
                      [hT[:, kt, :] for kt in range(8)], [ones[:, :]] * 8, 1024, 128)
            p = pnext(); proj(0, 128, p[:, :]); kb.act(uT[:, cs], p[:, :], AF.Copy)
            pa = pnext(); proj(128, 128, pa[:, :])
            pb = pnext(); proj(256, 64, pb[0:64, :])
            rms_T([pa[:, :], pb[0:64, :]], [col("gcq", 0), col("gcq", 1, slice(0, 64))], [cqa[:], cqb[:]],
                  [ones[:, :], ones[0:64, :]], 192, 128)
            for h in range(2):
                qp = pnext()
                kb.mm(qp[0:96, :], wuqb[:, 0, 96 * h:96 * h + 96], cqa[:], start=True, stop=False)
                kb.mm(qp[0:96, :], wuqb[0:64, 1, 96 * h:96 * h + 96], cqb[:], start=False, stop=True)
                rms_T([qp[0:96, :]], [col("mgq", 0, slice(0, 96))], [qn[h][:]], [ones[0:96, 0:96]], 96, 96,
                      lnbias=math.log(96 ** -0.5))
                rope(qn[h][:], QT[:, h, cs], c)
            p = pnext(); proj(320, 128, p[:, :])
            rms_T([p[:, :]], [col("gckv", 0)], [ckvn[:]], [ones[:, :]], 128, 128)
            for h in range(2):
                kp = pnext()
                kb.mm(kp[0:64, :], wknb[:, 64 * h:64 * h + 64], ckvn[:])
                proj(448, 32, kp[64:96, :])
                rms_T([kp[0:96, :]], [col("mgk", 0, slice(0, 96))], [qn[h][:]], [ones[0:96, 0:96]], 96, 96)
                rope(qn[h][:], KT[:, h, cs], c)
            vp = pnext()
            for j in range(4):
                kb.mm(vp[:, j * 128:(j + 1) * 128], ckvn[:, j * 128:(j + 1) * 128], wvmb[:, :])
            kb.act(VA[:, 4 * c:4 * c + 4, :, 0:64], vp[:, :].rearrange("p (j h d) -> p j h d", j=4, h=2), AF.Copy)
            p = pnext(); proj(480, 128, p[:, :]); kb.act(xlT[:, cs], p[:, :], AF.Copy)
            p = pnext(); proj(608, 128, p[:, :]); kb.act(ggT[:, cs], p[:, :], AF.Gelu_apprx_tanh)
            p = pnext(); proj(736, 128, p[:, :])
            rms_T([p[:, :]], [col("dgq", 0)], [QdT[:, cs]], [blk32[:, :]], 32, 128, lnbias=math.log(32 ** -0.5))
            p = pnext(); proj(864, 128, p[:, :])
            rms_T([p[:, :]], [col("dgk", 0)], [KdT[:, cs]], [blk32[:, :]], 32, 128)
            vp = pnext()
            for j in range(4):
                for kt in range(8):
                    kb.mm(vp[:, j * 128:(j + 1) * 128], hT[:, kt, j * 128:(j + 1) * 128], wmixb[:, kt, 992:1120],
                          start=(kt == 0), stop=(kt == 7))
            kb.act(VD[:, 4 * c:4 * c + 4, :, 0:64], vp[:, :].rearrange("p (j h d) -> p j h d", j=4, h=2), AF.Copy)
        kb.barrier()

    yi = [0]

    ymi = [0]

    def yout(br, rs, cs, yo_ap):
        if fout is None:
            kb.dma("sp", Y[br, rs, cs], yo_ap)
            return
        q = cs.start // 2048
        tsl = slice(cs.start - 2048 * q, cs.stop - 2048 * q)
        for s in range(2):
            t = ymk[ymi[0] % 4]
            ymi[0] += 1
            kb.ts(t[rs, :], yo_ap, fout["meq"][rs, s:s + 1], None, ALU.mult)
            kb.dma("sp", fout["ysrc"].ap()[q, br, s, rs, tsl], t[rs, :], extra_w=("ysrc",))

    def ynext():
        y = ystage[yi[0] % 3]
        yi[0] += 1
        return y

    with contextlib.ExitStack() as s3:
        wrb = kb.sb("wrb", [128, 128], BF16, es=s3)
        wib = kb.sb("wib", [128, 128], BF16, es=s3)
        kb.dma("pool", wrb[:], D["wr"][:, :])
        kb.dma("pool", wib[:], D["wi"][:, :])
        xc = kb.sb("lxc", [128, T], F32, es=s3)
        xcb = kb.sb("lxcb", [128, T], BF16, es=s3)
        av = kb.sb("lav", [128, T], F32, es=s3)
        inp = kb.sb("linp", [128, T], F32, es=s3)
        lt = [kb.sb(f"lt{i}", [128, CH], F32, es=s3) for i in range(4)]
        spc = kb.sb("spc", [128, 1], F32, es=s3)
        kb.act(spc[:], col("llam", 0), AF.Exp, scale=-1.0)
        kb.act(spc[:], spc[:], AF.Ln, bias=1.0)
        kb.ts(spc[:], spc[:], -8.0, None, ALU.mult)
        kb.ts(xc[:], xlT[:], col("cw", 3), col("cb", 0), ALU.mult, ALU.add)
        for k in range(3):
            sh = 3 - k
            kb.stt(xc[:, sh:T], xlT[:, 0:T - sh], col("cw", k), xc[:, sh:T], ALU.mult, ALU.add)
        kb.act(xcb[:], xc[:], AF.Copy)
        for c in range(NCH):
            cs = slice(c * CH, (c + 1) * CH)
            rp = pnext(); ip = pnext()
            kb.mm(rp[:, :], wrb[:, :], xcb[:, cs])
            kb.mm(ip[:, :], wib[:, :], xcb[:, cs])
            kb.act(lt[0][:], rp[:, :], AF.Sigmoid, bias=col("br", 0))
            kb.act(lt[1][:], ip[:, :], AF.Sigmoid, bias=col("bi", 0))
            kb.act(av[:, cs], lt[0][:], AF.Exp, scale=spc[:, 0:1])
            kb.tt(lt[2][:], av[:, cs], av[:, cs], ALU.mult)
            kb.ts(lt[2][:], lt[2][:], -1.0, 1.0, ALU.mult, ALU.add)
            kb.act(lt[3][:], lt[2][:], AF.Sqrt)
            kb.tt(lt[1][:], lt[1][:], xc[:, cs], ALU.mult, e="pool")
            kb.tt(inp[:, cs], lt[3][:], lt[1][:], ALU.mult)
        for c in range(NCH):
            cs = slice(c * CH, (c + 1) * CH)
            init = 0.0 if c == 0 else inp[:, c * CH - 1:c * CH]
            kb.scan(inp[:, cs], av[:, cs], inp[:, cs], init)
            yo = ynext()
            kb.tt(yo[:], inp[:, cs], ggT[:, cs], ALU.mult, e="pool")
            yout(2, slice(0, 128), cs, yo[:])
        if debug:
            kb.dma("sp", DBG[0], xc[:]); kb.dma("sp", DBG[1], av[:]); kb.dma("sp", DBG[2], inp[:]); kb.dma("sp", DBG[3], xlT[:])
        kb.barrier()

    es_lru.close()

    with contextlib.ExitStack() as s2:
        rep = kb.sb("rep", [32, 3, 512], F32, es=s2)
        bT = kb.sb("bT", [32, 2, 512], F32, es=s2)
        kb.dma("sp", rep[:], D["s5rep"][:, :, :])
        kb.dma("sp", bT[:], D["s5bT"][:, :, :])
        kb.dma("pool", cTb[:], D["s5cT"][:, :, :, :])

        def derived(lr, li, ls, shape, nm, est, keep):
            names = ["dt", "mag", "ang", "sn", "cs", "tmp", "tmp2", "ar1", "ai", "den", "zr", "zi", "th"]
            tt_ = {}
            for n in names:
                if n in keep:
                    tt_[n] = kb.sb(f"{nm}_{n}", shape, F32, es=s2)
            for n in names:
                if n not in keep:
                    tt_[n] = kb.sb(f"{nm}_{n}", shape, F32, es=est)
            dt = tt_["dt"]; mag = tt_["mag"]; ang = tt_["ang"]; sn = tt_["sn"]; cs_ = tt_["cs"]; tmp = tt_["tmp"]; tmp2 = tt_["tmp2"]
            ar1 = tt_["ar1"]; ai = tt_["ai"]; den = tt_["den"]; zr = tt_["zr"]; zi = tt_["zi"]; th = tt_["th"]
            kb.act(dt[:], ls, AF.Exp)
            kb.tt(tmp[:], lr, dt[:], ALU.mult)
            kb.act(mag[:], tmp[:], AF.Exp)
            kb.tt(ang[:], li, dt[:], ALU.mult)
            kii = kb.sb(f"{nm}_kii", shape, I32, es=est)
            sincos(ang[:], sn[:], cs_[:], kii[:], th[:], tmp[:])
            kb.tt(ai[:], mag[:], sn[:], ALU.mult)
            kb.tt(ar1[:], mag[:], cs_[:], ALU.mult)
            kb.ts(ar1[:], ar1[:], -1.0, None, ALU.add)
            kb.tt(den[:], lr, lr, ALU.mult)
            kb.tt(tmp[:], li, li, ALU.mult)
            kb.tt(den[:], den[:], tmp[:], ALU.add)
            kb.recip(den[:], den[:])
            kb.tt(tmp[:], ar1[:], lr, ALU.mult)
            kb.tt(tmp2[:], ai[:], li, ALU.mult)
            kb.tt(tmp[:], tmp[:], tmp2[:], ALU.add)
            kb.tt(zr[:], tmp[:], den[:], ALU.mult)
            kb.tt(tmp[:], ai[:], lr, ALU.mult)
            kb.tt(tmp2[:], ar1[:], li, ALU.mult)
            kb.tt(tmp[:], tmp[:], tmp2[:], ALU.subtract)
            kb.tt(zi[:], tmp[:], den[:], ALU.mult)
            return dict(mag=mag, th=th, zr=zr, zi=zi)

        o_lr, o_li, o_ls = PC["s5lr"][0], PC["s5li"][0], PC["s5ls"][0]
        dc = derived(pc[:, o_lr:o_lr + 4], pc[:, o_li:o_li + 4], pc[:, o_ls:o_ls + 4], [128, 4], "c4", s2,
                     ("mag", "th", "zr", "zi"))
        bbr = kb.sb("bbr", [32, 512], F32, es=s2)
        bbi = kb.sb("bbi", [32, 512], F32, es=s2)
        s2t = contextlib.ExitStack()
        dr = derived(rep[:, 0, :], rep[:, 1, :], rep[:, 2, :], [32, 512], "r32", s2t, ())
        tb = kb.sb("tb", [32, 512], F32, es=s2t)
        kb.tt(bbr[:], dr["zr"][:], bT[:, 0, :], ALU.mult)
        kb.tt(tb[:], dr["zi"][:], bT[:, 1, :], ALU.mult)
        kb.tt(bbr[:], bbr[:], tb[:], ALU.subtract)
        kb.tt(bbi[:], dr["zr"][:], bT[:, 1, :], ALU.mult)
        kb.tt(tb[:], dr["zi"][:], bT[:, 0, :], ALU.mult)
        kb.tt(bbi[:], bbi[:], tb[:], ALU.add)
        kb.barrier()
        s2t.close()
        for st in range(4):
            kb.act(BL[32 * st:32 * st + 32, 0, :], bbr[:, st * 128:(st + 1) * 128], AF.Copy)
            kb.act(BL[32 * st:32 * st + 32, 1, :], bbi[:, st * 128:(st + 1) * 128], AF.Copy)
        kb.cp(magp[:], dc["mag"][:])
        kb.cp(thp[:], dc["th"][:])
        kb.barrier()

    EB = kb.sb("EB", [128, 3, 5, 512], BF16)
    for r in range(3):
        src = bass.AP(tensor=scratch, offset=r * 128 * RL + 127, ap=[[RL, 128], [128, 5], [1, 512]])
        kb.dma("pool", EB[:, r, :, :], src)
    pbanks[0] = [0, 1, 6]
    Qz = [kb.sb(f"Qz{i}", [128, CH], BF16) for i in range(8)]
    for _q in Qz:
        kb.memset(_q[:], 0.0, e="pool")
    Pt = [kb.sb(f"Pt{i}", [128, CH], BF16) for i in range(4)]
    pi_ = [0]
    fin = [kb.sb(f"fin{i}", [128, CH], F32) for i in range(6)]
    SB_ = [PS[0], PS[1], PS[6]]
    sctr = [0]

    def snext():
        p = SB_[sctr[0] % 3]
        sctr[0] += 1
        return p

    def attn_gen():
        LOOK = 2
        for qc in range(NCH):
            qs = slice(qc * CH, (qc + 1) * CH)
            nk = 4 * qc + 4
            OA = [PS[4], PS[5]]
            steps = [(kt, h) for kt in range(nk) for h in range(2)]
            spt = {}

            def qk_mla(i):
                kt, h = steps[i]
                sp_ = snext()
                kb.mm(sp_[:, :], KT[:, h, kt * 128:(kt + 1) * 128], QT[:, h, qs])
                spt[i] = sp_
            for i in range(min(LOOK, len(steps))):
                qk_mla(i)
            for i, (kt, h) in enumerate(steps):
                sp_ = spt.pop(i)
                P = Pt[pi_[0] % 4]; pi_[0] += 1
                kb.act(P[:], sp_[:, :], AF.Exp)
                v = kt - 4 * qc + 1
                if v >= 1:
                    kb.tt(P[:], P[:], EB[:, 2, 4 - v, :], ALU.mult)
                if i + LOOK < len(steps):
                    qk_mla(i + LOOK)
                kb.mm(OA[h][:, :], VA[:, kt, h, :], P[:], start=(kt == 0), stop=(kt == nk - 1))
                yield
            yo = ynext_a()
            for h in range(2):
                rd = fin[h]
                kb.act(rd[0:64, :], OA[h][64:128, :], AF.Ln)
                kb.act(rd[0:64, :], rd[0:64, :], AF.Exp, scale=-1.0)
                kb.tt(yo[64 * h:64 * h + 64, :], OA[h][0:64, :], rd[0:64, :], ALU.mult)
            yout(1, slice(0, 128), qs, yo[:])
            qz = Qz[4 * (qc % 2):4 * (qc % 2) + 4]
            for _i in range(4):
                kb.act(qz[_i][32 * _i:32 * _i + 32, :], QdT[32 * _i:32 * _i + 32, qs], AF.Copy)
            yo = ynext_a()
            for h in range(2):
                OD = [PS[4], PS[5]]
                steps = [(kt, s) for kt in range(nk) for s in range(2)]
                spt = {}

                def qk_d(i):
                    kt, s = steps[i]
                    r0 = 64 * h + 32 * s
                    sp_ = snext()
                    kb.mm(sp_[:, :], KdT[:, kt * 128:(kt + 1) * 128], qz[2 * h + s][:, :])
                    spt[i] = sp_
                for i in range(min(LOOK, len(steps))):
                    qk_d(i)
                for i, (kt, s) in enumerate(steps):
                    v = kt - 4 * qc + 1
                    sp_ = spt.pop(i)
                    P = Pt[pi_[0] % 4]; pi_[0] += 1
                    if v >= 0:
                        kb.act(P[:], sp_[:, :], AF.Exp)
                        kb.tt(P[:], P[:], EB[:, h, 4 - v, :], ALU.mult)
                    else:
                        kb.act(P[:], sp_[:, :], AF.Exp, bias=col("tb31", h))
                    if i + LOOK < len(steps):
                        qk_d(i + LOOK)
                    kb.mm(OD[s][:, :], VD[:, kt, h, :], P[:], start=(kt == 0), stop=(kt == nk - 1))
                    yield
                r1, r2, o1, o2, dd = fin[0], fin[1], fin[2], fin[3], fin[4]
                kb.act(r1[0:64, :], OD[0][64:128, :], AF.Ln)
                kb.act(r1[0:64, :], r1[0:64, :], AF.Exp, scale=-1.0)
                kb.act(r2[0:64, :], OD[1][64:128, :], AF.Ln)
                kb.act(r2[0:64, :], r2[0:64, :], AF.Exp, scale=-1.0)
                kb.tt(o1[0:64, :], OD[0][0:64, :], r1[0:64, :], ALU.mult)
                kb.tt(o2[0:64, :], OD[1][0:64, :], r2[0:64, :], ALU.mult)
                kb.stt(dd[0:64, :], o2[0:64, :], neglam[0:64, 0:1], o1[0:64, :], ALU.mult, ALU.add)
                rms_T([dd[0:64, :]], [col("gsub", 0, slice(0, 64))], [yo[64 * h:64 * h + 64, :]], [ones[0:64, 0:64]], 64, 64,
                      lnbias=math.log(1.0 - lam_init))
            yout(3, slice(0, 128), qs, yo[:])

    p5c = [0]

    def p5next():
        p = PS[2 + (p5c[0] % 2)]
        p5c[0] += 1
        return p

    s2 = contextlib.ExitStack()
    if True:
        iot = kb.sb("iot", [128, CH], F32, es=s2)
        kb.op("pool", lambda: nc.gpsimd.iota(iot[:], pattern=[[1, CH]], base=0, channel_multiplier=0,
                                               allow_small_or_imprecise_dtypes=True), [], [iot[:]])
        basec = kb.sb("basec", [128, 32], F32, es=s2)
        ph0 = [kb.sb(f"ph0_{i}", [128, CH], F32, es=s2) for i in range(2)]
        NB = 2
        cosT = [kb.sb(f"cosT{i}", [128, CH], F32, es=s2) for i in range(NB)]
        sinT = [kb.sb(f"sinT{i}", [128, CH], F32, es=s2) for i in range(NB)]
        pr = [kb.sb(f"pr{i}", [128, CH], F32, es=s2) for i in range(NB)]
        pim = [kb.sb(f"pim{i}", [128, CH], F32, es=s2) for i in range(NB)]
        m = [kb.sb(f"s5m{i}", [128, CH], F32, es=s2) for i in range(4)]
        hrb = [kb.sb(f"hrb{i}", [128, CH], BF16, es=s2) for i in range(2)]
        hib = [kb.sb(f"hib{i}", [128, CH], BF16, es=s2) for i in range(2)]
        ytm = [kb.sb(f"ytm{i}", [128, CH], F32, es=s2) for i in range(2)]
        cmul = kb.sb("cmul", [128, 8], F32, es=s2)
        bx = kb.sb("bx", [128, 32], F32, es=s2)
        bki = kb.sb("bki", [128, 32], I32, es=s2)
        kb.op("pool", lambda: nc.gpsimd.iota(cmul[:], pattern=[[CH, 8]], base=0, channel_multiplier=0,
                                               allow_small_or_imprecise_dtypes=True), [], [cmul[:]])
        for st in range(4):
            kb.ts(bx[:, st * 8:st * 8 + 8], cmul[:], thp[:, st:st + 1], None, ALU.mult)
        kb.ts(bki[:], bx[:], 1.0 / (2 * PI), None, ALU.mult)
        kb.stt(basec[:], bki[:], -2 * PI, bx[:], ALU.mult, ALU.add)
        ski = [kb.sb(f"ski{i}", [128, CH], I32, es=s2) for i in range(2)]
        shh = [kb.sb(f"shh{i}", [128, CH], F32, es=s2) for i in range(2)]
        def s5_gen():
            its = [(st, c) for st in range(4) for c in range(NCH)]
            PRE, PIE, YP = PS[2], PS[3], PS[7]

            def tables(i):
                st, c = its[i]
                b = i % NB
                kb.ts(ph0[b][:], iot[:, :], thp[:, st:st + 1], basec[:, st * 8 + c:st * 8 + c + 1], ALU.mult, ALU.add)
                sincos(ph0[b][:], sinT[b][:], cosT[b][:], ski[b][:], ph0[b][:], shh[b][:], dve_cos=True)

            def bmm(i):
                st, c = its[i]
                cs = slice(c * CH, (c + 1) * CH)
                kb.mm(PRE[:, :], BL[32 * st:32 * st + 32, 0, :], uT[32 * st:32 * st + 32, cs], tile_position=(32 * st, 0))
                kb.mm(PIE[:, :], BL[32 * st:32 * st + 32, 1, :], uT[32 * st:32 * st + 32, cs], tile_position=(32 * st, 0))
            pend = []

            def epilogue():
                while pend:
                    rs_, cs_, yt_ = pend.pop(0)
                    yo_ = ynext()
                    kb.act(yo_[rs_, :], yt_[rs_, :], AF.Gelu_apprx_tanh)
                    yout(0, rs_, cs_, yo_[rs_, :])
            tables(0)
            bmm(0)
            for i, (st, c) in enumerate(its):
                cs = slice(c * CH, (c + 1) * CH)
                b = i % NB
                pb = (i - 1) % NB
                if i + 1 < len(its):
                    tables(i + 1)
                epilogue()
                yield
                kb.tt(m[0][:], PRE[:, :], cosT[b][:], ALU.mult)
                kb.tt(m[1][:], PIE[:, :], sinT[b][:], ALU.mult)
                kb.tt(pr[b][:], m[0][:], m[1][:], ALU.add)
                yield
                kb.tt(m[2][:], PIE[:, :], cosT[b][:], ALU.mult)
                kb.tt(m[3][:], PRE[:, :], sinT[b][:], ALU.mult)
                kb.tt(pim[b][:], m[2][:], m[3][:], ALU.subtract)
                if i + 1 < len(its):
                    bmm(i + 1)
                yield
                for buf in (pr, pim):
                    init = 0.0 if c == 0 else buf[pb][:, CH - 1:CH]
                    kb.scan(buf[b][:], magp[:, st:st + 1].to_broadcast([128, CH]), buf[b][:], init)
                yield
                kb.tt(m[0][:], pr[b][:], cosT[b][:], ALU.mult)
                kb.tt(m[1][:], pim[b][:], sinT[b][:], ALU.mult)
                kb.tt(hrb[c % 2][:], m[0][:], m[1][:], ALU.subtract)
                yield
                kb.tt(m[2][:], pr[b][:], sinT[b][:], ALU.mult)
                kb.tt(m[3][:], pim[b][:], cosT[b][:], ALU.mult)
                kb.stt(hib[c % 2][:], m[2][:], -1.0, m[3][:], ALU.mult, ALU.subtract)
                kb.mm(YP[0:32, :], cTb[:, 0, st, :], hrb[c % 2][:], start=True, stop=False)
                kb.mm(YP[0:32, :], cTb[:, 1, st, :], hib[c % 2][:], start=False, stop=True)
                yield
                rs = slice(32 * st, 32 * st + 32)
                yt = ytm[c % 2]
                kb.cp(yt[rs, :], YP[0:32, :])
                kb.stt(yt[rs, :], uT[rs, cs], col("s5d", 0, rs), yt[rs, :], ALU.mult, ALU.add)
                pend.append((rs, cs, yt))
                yield
            epilogue()
            yield

    warm_pe([4, 5])
    g5 = s5_gen()
    ga = attn_gen()
    n_att = sum((4 * qc + 4) * 6 for qc in range(NCH))
    per = max(1, n_att // (32 * 7))
    done5 = False
    donea = False
    while not (done5 and donea):
        if not done5:
            try:
                next(g5)
            except StopIteration:
                done5 = True
        for _ in range(per if not done5 else 10 ** 9):
            try:
                next(ga)
            except StopIteration:
                donea = True
                break
    kb.barrier()
    s2.close()
    kb.barrier()
    kb.es.close()
    kb.es = old_es
    kb.sfx = old_sfx
    if standalone:
        kb.wait_all("sp")
    return kb

import math

TO = 2048
CH = 512
NC4 = TO // CH
EPS = 1e-6
NMEM = 256

P2_LAYOUT = [("gmix", 8), ("gcross", 8), ("gmem", 8), ("gffn", 8), ("bgate", 32), ("bglu", 2), ("xgq", 1), ("xgk", 1)]
P2 = {}
_o = 0
for _n, _w in P2_LAYOUT:
    P2[_n] = (_o, _w)
    _o += _w
NP2 = _o


def build_R(moe, debug=False, kb=None, sfx="", PS=None, xin=None, yin=None, xout=None, hout=None):
    standalone = kb is None
    if standalone:
        kb = KB()
    nc = kb.nc
    old_es = kb.es
    kb.es = contextlib.ExitStack()
    old_sfx = kb.sfx
    kb.sfx = sfx
    _dram = kb.dram
    def kb_dram(name, shape, dt, kind):
        return _dram(name + sfx, shape, dt, kind)
    D = {}
    D["xT"] = kb_dram("xT", [1024, TO], F32, "ExternalInput") if xin is None else xin
    D["yall"] = kb_dram("yall", [4, 256, TO], BF16, "ExternalInput") if yin is None else yin
    D["wg8"] = kb_dram("wg8", [8, 1024, 512], F32, "ExternalInput")
    D["wbr"] = kb_dram("wbr", [4, 256, 1024], F32, "ExternalInput")
    D["wout"] = kb_dram("wout", [1024, 1024], F32, "ExternalInput")
    D["pc2"] = kb_dram("pc2", [128, NP2], F32, "ExternalInput")
    D["wglu"] = kb_dram("wglu", [256, 256], F32, "ExternalInput")
    D["xwq"] = kb_dram("xwq", [1024, 256], F32, "ExternalInput")
    D["xwk"] = kb_dram("xwk", [1024, 256], F32, "ExternalInput")
    D["xwv"] = kb_dram("xwv", [1024, 256], F32, "ExternalInput")
    D["xwo"] = kb_dram("xwo", [256, 1024], F32, "ExternalInput")
    D["memT"] = kb_dram("memT", [1024, NMEM], F32, "ExternalInput")
    if moe:
        NE, FF = 8, 3584
        D["wr"] = kb_dram("wrt", [1024, 8], F32, "ExternalInput")
    else:
        NE, FF = 1, 2816
    D["fg"] = kb_dram("fg", [NE, 1024, FF], F32, "ExternalInput")
    D["fu"] = kb_dram("fu", [NE, 1024, FF], F32, "ExternalInput")
    D["fd"] = kb_dram("fd", [NE, FF, 1024], F32, "ExternalInput")
    XO = kb_dram("xo", [1024, TO], F32, "ExternalOutput") if xout is None else xout
    NFT = FF // 128

    pc = kb.sb("pc", [128, NP2], F32)

    def col(name, i=0, rows=slice(0, 128)):
        o, w = P2[name]
        return pc[rows, o + i:o + i + 1]
    ones = kb.sb("ones", [128, 128], BF16)
    blk64 = kb.sb("blk64", [128, 128], BF16)
    xT = kb.sb("xTs", [128, 8, TO], F32)
    if PS is None:
        PS = [kb.ps(f"ps{i}", [128, 512], F32, es=old_es) for i in range(8)]
    pctr = [0]
    pbanks = [list(range(8))]

    def pnext():
        b = pbanks[0]
        p = PS[b[pctr[0] % len(b)]]
        pctr[0] += 1
        return p
    kb.dma("sp", pc[:], D["pc2"][:, :])
    kb.memset(ones[:], 1.0)
    kb.memset(blk64[:], 0.0, e="pool")
    for g in range(2):
        kb.memset(blk64[64 * g:64 * g + 64, 64 * g:64 * g + 64], 1.0, e="pool")
    xv = D["xT"].rearrange("(kt p) t -> p kt t", p=128)
    for c in range(NC4):
        kb.dma("sp", xT[:, :, c * CH:(c + 1) * CH], xv[:, :, c * CH:(c + 1) * CH], extra_r=("xsp",))

    tmpi = [0]
    sqs = [kb.sb(f"sq{i}", [128, 512], BF16) for i in range(4)]
    lnvs = [kb.sb(f"lnv{i}", [128, 512], F32) for i in range(2)]
    rstds = [kb.sb(f"rstd{i}", [128, 512], F32) for i in range(2)]

    def rms_T(srcs, gains, outs, onesT, nfeat, npo, lnbias=0.0, W=512, rstd_out=None):
        i0 = tmpi[0]
        tmpi[0] += 1
        ssp = pnext()
        n = len(srcs)
        for i, s in enumerate(srcs):
            p0, p1 = _box(s)[0], _box(s)[1]
            sq = sqs[(i0 * 2 + i) % 4]
            kb.act(sq[p0:p1, 0:W], s, AF.Square)
            kb.mm(ssp[0:npo, 0:W], onesT[i], sq[p0:p1, 0:W], start=(i == 0), stop=(i == n - 1))
        lnv = lnvs[i0 % 2]
        rstd = rstds[i0 % 2]
        kb.act(lnv[0:npo, 0:W], ssp[0:npo, 0:W], AF.Ln, bias=EPS, scale=1.0 / nfeat)
        kb.act(rstd[0:npo, 0:W], lnv[0:npo, 0:W], AF.Exp, scale=-0.5, bias=lnbias)
        if rstd_out is not None:
            kb.act(rstd_out, rstd[0:1, 0:W], AF.Copy)
        for i, s in enumerate(srcs):
            p0, p1 = _box(s)[0], _box(s)[1]
            kb.stt(outs[i], s, gains[i], rstd[p0:p1, 0:W], ALU.mult, ALU.mult)

    def norm_x(hT, gname, rstd_row=None):
        for c in range(NC4):
            cs = slice(c * CH, (c + 1) * CH)
            rms_T([xT[:, kt, cs] for kt in range(8)], [col(gname, kt) for kt in range(8)],
                  [hT[:, kt, cs] for kt in range(8)], [ones[:, :]] * 8, 1024, 128,
                  rstd_out=(None if rstd_row is None else rstd_row[32 * c:32 * c + 1, :]))

    def resid_add(ot, cs, ps_ap):
        kb.tt(xT[:, ot, cs], xT[:, ot, cs], ps_ap, ALU.add)

    with contextlib.ExitStack() as e1:
        merged = kb.sb("merged", [128, 8, TO], BF16, es=e1)
        with contextlib.ExitStack() as e2:
            hT = kb.sb("hT", [128, 8, TO], BF16, es=e2)
            yb = kb.sb("yb", [128, 4, 2, TO], BF16, es=e2)
            wbrb = kb.sb("wbrb", [128, 4, 2, 1024], BF16, es=e2)
            wglub = kb.sb("wglub", [128, 2, 256], BF16, es=e2)
            wgb = [kb.sb(f"wgb{i}", [128, 8, 512], BF16, es=e2) for i in range(1)] * 2
            sg = [kb.sb(f"sg{i}", [128, CH], F32, es=e2) for i in range(2)] + [None]
            sg[2] = sg[0]
            tmpm = [kb.sb(f"tmpm{i}", [128, CH], F32, es=e2) for i in range(1)] * 2
            accm = [kb.sb(f"accm{i}", [128, CH], F32, es=e2) for i in range(2)]
            for b in range(4):
                for k2 in range(2):
                    kb.dma("sp", yb[:, b, k2, :], D["yall"][b, 128 * k2:128 * k2 + 128, :], extra_r=("ydst",))
                    kb.dma("pool", wbrb[:, b, k2, :], D["wbr"][b, 128 * k2:128 * k2 + 128, :])
            for k2 in range(2):
                kb.dma("pool", wglub[:, k2, :], D["wglu"][128 * k2:128 * k2 + 128, :])
            norm_x(hT, "gmix")
            for c in range(NC4):
                cs = slice(c * CH, (c + 1) * CH)
                gl = []
                for j in range(2):
                    gp = pnext()
                    for k2 in range(2):
                        kb.mm(gp[:, :], wglub[:, k2, 128 * j:128 * j + 128], yb[:, 0, k2, cs], start=(k2 == 0), stop=(k2 == 1))
                    kb.act(sg[j][:], gp[:, :], AF.Sigmoid, bias=col("bglu", j))
                for j in range(2):
                    kb.tt(yb[:, 0, j, cs], yb[:, 0, j, cs], sg[j][:], ALU.mult)
            gv = D["wg8"].rearrange("d (kt p) n -> d p kt n", p=128)
            si = 0
            for dt in range(8):
                wg = wgb[dt % 2]
                kb.dma("pool", wg[:], gv[dt])
                for c in range(NC4):
                    cs = slice(c * CH, (c + 1) * CH)
                    acc = accm[(dt * NC4 + c) % 2]
                    for b in range(4):
                        up = pnext()
                        for k2 in range(2):
                            kb.mm(up[:, :], wbrb[:, b, k2, 128 * dt:128 * dt + 128], yb[:, b, k2, cs], start=(k2 == 0), stop=(k2 == 1))
                        gp = pnext()
                        for kt in range(8):
                            kb.mm(gp[:, :], wg[:, kt, 128 * b:128 * b + 128], hT[:, kt, cs], start=(kt == 0), stop=(kt == 7))
                        s = sg[si % 2]
                        si += 1
                        kb.act(s[:], gp[:, :], AF.Sigmoid, bias=col("bgate", b * 8 + dt))
                        if b == 0:
                            kb.tt(acc[:], s[:], up[:, :], ALU.mult)
                        elif b < 3:
                            t = tmpm[b % 2]
                            kb.tt(t[:], s[:], up[:, :], ALU.mult)
                            kb.tt(acc[:], acc[:], t[:], ALU.add, e="pool")
                        else:
                            t = tmpm[b % 2]
                            kb.tt(t[:], s[:], up[:, :], ALU.mult)
                            kb.tt(merged[:, dt, cs], acc[:], t[:], ALU.add, e="pool")
            kb.barrier()
        woutb = kb.sb("woutb", [128, 8, 1024], BF16, es=e1)
        kb.dma("pool", woutb[:], D["wout"].rearrange("(kt p) n -> p kt n", p=128))
        for ot in range(8):
            for c in range(NC4):
                cs = slice(c * CH, (c + 1) * CH)
                op_ = pnext()
                for kt in range(8):
                    kb.mm(op_[:, :], woutb[:, kt, 128 * ot:128 * ot + 128], merged[:, kt, cs], start=(kt == 0), stop=(kt == 7))
                resid_add(ot, cs, op_[:, :])
        kb.barrier()
    if debug:
        DBG = kb_dram("dbgxa", [1024, TO], F32, "ExternalOutput")
        kb.dma("sp", DBG.rearrange("(kt p) t -> p kt t", p=128), xT[:])

    with contextlib.ExitStack() as e1:
        hT = kb.sb("hTc", [128, 8, TO], BF16, es=e1)
        wq = kb.sb("wq", [128, 8, 256], BF16, es=e1)
        wk = kb.sb("wk", [128, 8, 256], BF16, es=e1)
        wvv = kb.sb("wvv", [128, 8, 256], BF16, es=e1)
        wo = kb.sb("wo", [128, 2, 1024], BF16, es=e1)
        mT = kb.sb("mT", [128, 8, NMEM], F32, es=e1)
        mh = kb.sb("mh", [128, 8, NMEM], BF16, es=e1)
        QcT = kb.sb("QcT", [128, 2, TO], BF16, es=e1)
        KcT = kb.sb("KcT", [128, 2, NMEM], BF16, es=e1)
        VC = kb.sb("VC", [128, 2, 4, 128], BF16, es=e1)
        OcT = kb.sb("OcT", [128, 2, TO], BF16, es=e1)
        Pt = [kb.sb(f"Pt{i}", [128, CH], BF16, es=e1) for i in range(4)]
        rdt = [kb.sb(f"rdt{i}", [128, CH], F32, es=e1) for i in range(2)]
        for nm, t in (("xwq", wq), ("xwk", wk), ("xwv", wvv)):
            kb.dma("pool", t[:], D[nm].rearrange("(kt p) n -> p kt n", p=128))
        kb.dma("pool", wo[:], D["xwo"].rearrange("(kt p) n -> p kt n", p=128))
        kb.dma("sp", mT[:], D["memT"].rearrange("(kt p) t -> p kt t", p=128))
        kb.memset(VC[:, :, :, 64:128], 1.0, e="pool")
        rms_T([mT[:, kt, :] for kt in range(8)], [col("gmem", kt) for kt in range(8)], [mh[:, kt, :] for kt in range(8)],
              [ones[:, :]] * 8, 1024, 128, W=NMEM)
        for j in range(2):
            kp = pnext()
            for kt in range(8):
                kb.mm(kp[:, 0:NMEM], wk[:, kt, 128 * j:128 * j + 128], mh[:, kt, :], start=(kt == 0), stop=(kt == 7))
            rms_T([kp[:, 0:NMEM]], [col("xgk", 0)], [KcT[:, j, :]], [blk64[:, :]], 64, 128, W=NMEM)
        for mt in range(2):
            vp = pnext()
            for kt in range(8):
                kb.mm(vp[:, 0:256], mh[:, kt, 128 * mt:128 * mt + 128], wvv[:, kt, :], start=(kt == 0), stop=(kt == 7))
            kb.act(VC[:, mt, :, 0:64], vp[:, 0:256].rearrange("p (h d) -> p h d", h=4), AF.Copy)
        norm_x(hT, "gcross")
        for c in range(NC4):
            cs = slice(c * CH, (c + 1) * CH)
            for j in range(2):
                qp = pnext()
                for kt in range(8):
                    kb.mm(qp[:, :], wq[:, kt, 128 * j:128 * j + 128], hT[:, kt, cs], start=(kt == 0), stop=(kt == 7))
                rms_T([qp[:, :]], [col("xgq", 0)], [QcT[:, j, cs]], [blk64[:, :]], 64, 128, lnbias=math.log(64 ** -0.5))
        pbanks[0] = [0, 1, 2, 3]
        pi_ = 0
        for c in range(NC4):
            cs = slice(c * CH, (c + 1) * CH)
            for h in range(4):
                r0 = 64 * (h % 2)
                j = h // 2
                O = PS[4 + h]
                for mt in range(2):
                    sp_ = pnext()
                    kb.mm(sp_[:, :], KcT[r0:r0 + 64, j, 128 * mt:128 * mt + 128], QcT[r0:r0 + 64, j, cs])
                    P = Pt[pi_ % 4]
                    pi_ += 1
                    kb.act(P[:], sp_[:, :], AF.Exp)
                    kb.mm(O[:, :], VC[:, mt, h, :], P[:], start=(mt == 0), stop=(mt == 1))
                rd = rdt[h % 2]
                kb.recip(rd[0:64, :], O[64:128, :])
                kb.tt(OcT[r0:r0 + 64, j, cs], O[0:64, :], rd[0:64, :], ALU.mult)
        pbanks[0] = list(range(8))
        for ot in range(8):
            for c in range(NC4):
                cs = slice(c * CH, (c + 1) * CH)
                op_ = pnext()
                for k2 in range(2):
                    kb.mm(op_[:, :], wo[:, k2, 128 * ot:128 * ot + 128], OcT[:, k2, cs], start=(k2 == 0), stop=(k2 == 1))
                resid_add(ot, cs, op_[:, :])
        kb.barrier()
    if debug:
        DBG2 = kb_dram("dbgxb", [1024, TO], F32, "ExternalOutput")
        kb.dma("sp", DBG2.rearrange("(kt p) t -> p kt t", p=128), xT[:])

    TB = TO
    NTB = 1
    JB = 256 if moe else 128
    WDW = 256
    NH = NFT // 2
    with contextlib.ExitStack() as e1:
        hT = kb.sb("hTf", [128, 8, TO], BF16, es=e1)
        hmid = kb.sb("hmid", [128, NH, TB], BF16, es=e1)
        wgB = [kb.sb(f"wgB{i}", [128, 8, JB], BF16, es=e1) for i in range(2)]
        wuB = [kb.sb(f"wuB{i}", [128, 8, JB], BF16, es=e1) for i in range(2)]
        wdB = [kb.sb(f"wdB{i}", [128, NH, WDW], BF16, es=e1) for i in range(2)]
        av = [kb.sb(f"av{i}", [128, CH], F32, es=e1) for i in range(2 if not moe else 1)] * 2
        tv = [kb.sb(f"tv{i}", [128, CH], F32, es=e1) for i in range(1)] * 2
        rrow = None
        if moe:
            rrow = kb.sb("rrow", [128, CH], F32, es=e1)
            identf = kb.sb("identf", [128, 128], F32, es=e1)
            wrf = kb.sb("wrf", [128, 8, 8], F32, es=e1)
            Wtok = kb.sb("Wtok", [128, TO // 128, 8], F32, es=e1)
            lg = kb.sb("lg", [128, 8], F32, es=e1)
            mx = kb.sb("mx", [128, 8], F32, es=e1)
            ex = kb.sb("ex", [128, 8], F32, es=e1)
            mk = kb.sb("mk", [128, 8], F32, es=e1)
            sm = kb.sb("sm", [128, 4], F32, es=e1)
            rc = kb.sb("rc", [128, 1], F32, es=e1)
            onef = kb.sb("onef", [128, 1], F32, es=e1)
            wbc = kb.sb("wbc", [128, TB], BF16, es=e1)
            wb128 = kb.sb("wb128", [128, 128], F32, es=e1)
            kb.memset(onef[:], 1.0)
            iop = kb.sb("iop", [128, 128], F32, es=e1)
            kb.op("pool", lambda: nc.gpsimd.iota(iop[:], pattern=[[1, 128]], base=0, channel_multiplier=-1,
                                                   allow_small_or_imprecise_dtypes=True), [], [iop[:]])
            kb.ts(identf[:], iop[:], 0.0, None, ALU.is_equal)
            kb.dma("sp", wrf[:], D["wr"].rearrange("(kt p) n -> p kt n", p=128))
            for kt in range(8):
                kb.ts(wrf[:, kt, :], wrf[:, kt, :], col("gffn", kt), None, ALU.mult)
        norm_x(hT, "gffn", rstd_row=rrow)
        if moe:
            for tt_ in range(TO // 128):
                ts_ = slice(128 * tt_, 128 * tt_ + 128)
                lp = pnext()
                for kt in range(8):
                    kb.mm(lp[:, 0:8], xT[:, kt, ts_], wrf[:, kt, :], start=(kt == 0), stop=(kt == 7))
                rp = pnext()
                cq = tt_ // 4
                kb.mm(rp[:, 0:1], rrow[32 * cq:32 * cq + 1, 128 * (tt_ % 4):128 * (tt_ % 4) + 128], onef[32 * cq:32 * cq + 1, 0:1], tile_position=(32 * cq, 0))
                kb.act(rc[:], rp[:, 0:1], AF.Copy)
                kb.ts(lg[:], lp[:, 0:8], rc[:, 0:1], None, ALU.mult)
                kb.op("dve", lambda: nc.vector.max(out=mx[:], in_=lg[:]), [lg[:]], [mx[:]])
                kb.ts(mk[:], lg[:], mx[:, 1:2], None, ALU.is_ge)
                kb.ts(sm[:, 0:1], mx[:, 0:1], -1.0, None, ALU.mult)
                kb.act(ex[:], lg[:], AF.Exp, bias=sm[:, 0:1])
                kb.tt(ex[:], ex[:], mk[:], ALU.mult)
                kb.op("dve", lambda: nc.vector.reduce_sum(out=sm[:, 1:2], in_=ex[:], axis=AX.X), [ex[:]], [sm[:, 1:2]])
                kb.recip(sm[:, 2:3], sm[:, 1:2])
                kb.ts(Wtok[:, tt_, :], ex[:], sm[:, 2:3], None, ALU.mult)
        fgv = D["fg"].rearrange("e (kt p) n -> e p kt n", p=128)
        fuv = D["fu"].rearrange("e (kt p) n -> e p kt n", p=128)
        fdv = D["fd"].rearrange("e (kt p) n -> e p kt n", p=128)
        bi = 0
        di = 0
        ai = 0
        for e in range(NE):
            if moe:
                for q in range(TB // 128):
                    kb.cp(wb128[:, :], Wtok[:, q, e:e + 1].to_broadcast([128, 128]))
                    wp = pnext()
                    kb.mm(wp[:, 0:128], wb128[:, :], identf[:, :])
                    kb.act(wbc[:, 128 * q:128 * q + 128], wp[:, 0:128], AF.Copy)
            for jh in range(2):
                nblk = (NH * 128) // JB
                for jb in range(nblk):
                    wgt = wgB[bi % 2]
                    wut = wuB[bi % 2]
                    bi += 1
                    c0 = jh * NH * 128 + JB * jb
                    kb.dma("pool", wgt[:], fgv[e][:, :, c0:c0 + JB])
                    kb.dma("pool", wut[:], fuv[e][:, :, c0:c0 + JB])
                    for jj in range(JB // 128):
                        jl = jb * (JB // 128) + jj
                        for c2 in range(TB // CH):
                            cs = slice(c2 * CH, (c2 + 1) * CH)
                            gp = pnext()
                            for kt in range(8):
                                kb.mm(gp[:, :], wgt[:, kt, 128 * jj:128 * jj + 128], hT[:, kt, cs], start=(kt == 0), stop=(kt == 7))
                            up = pnext()
                            for kt in range(8):
                                kb.mm(up[:, :], wut[:, kt, 128 * jj:128 * jj + 128], hT[:, kt, cs], start=(kt == 0), stop=(kt == 7))
                            a = av[ai % 2]
                            t = tv[ai % 2]
                            ai += 1
                            kb.act(a[:], gp[:, :], AF.Silu)
                            if moe:
                                kb.tt(t[:], a[:], up[:, :], ALU.mult)
                                kb.tt(hmid[:, jl, cs], t[:], wbc[:, cs], ALU.mult, e="pool")
                            else:
                                kb.tt(hmid[:, jl, cs], a[:], up[:, :], ALU.mult)
                for op2 in range(1024 // WDW):
                    wdt = wdB[di % 2]
                    di += 1
                    kb.dma("pool", wdt[:], fdv[e][:, jh * NH:(jh + 1) * NH, WDW * op2:WDW * (op2 + 1)])
                    for o2 in range(WDW // 128):
                        ot = (WDW // 128) * op2 + o2
                        for c2 in range(TB // CH):
                            cs = slice(c2 * CH, (c2 + 1) * CH)
                            dp = pnext()
                            for jl in range(NH):
                                kb.mm(dp[:, :], wdt[:, jl, 128 * o2:128 * o2 + 128], hmid[:, jl, cs],
                                      start=(jl == 0), stop=(jl == NH - 1))
                            resid_add(ot, cs, dp[:, :])
        kb.barrier()
    xov = XO.rearrange("(kt p) t -> p kt t", p=128)
    for c in range(NC4):
        kb.dma("sp", xov[:, :, c * CH:(c + 1) * CH], xT[:, :, c * CH:(c + 1) * CH], extra_w=("xsp",))
    if hout is not None:
        with contextlib.ExitStack() as eh:
            hTn = kb.sb("hTn", [128, 8, TO], BF16, es=eh)
            hmk = [kb.sb(f"hmk{i}", [128, 8, CH], BF16, es=eh) for i in range(2)]
            for c in range(NC4):
                cs = slice(c * CH, (c + 1) * CH)
                rms_T([xT[:, kt, cs] for kt in range(8)], [hout["g"][:, kt:kt + 1] for kt in range(8)],
                      [hTn[:, kt, cs] for kt in range(8)], [ones[:, :]] * 8, 1024, 128)
                for s in range(2):
                    t = hmk[s]
                    kb.act(t[:], hTn[:, :, cs], AF.Copy, scale=hout["meq"][:, s:s + 1])
                    for q in range(2):
                        kb.dma("sp", hout["hsrc"].ap()[q, s].rearrange("(kt p) t -> p kt t", p=128)[:, :, cs], t[:], extra_w=("hsrc",))
            kb.barrier()
    kb.barrier()
    kb.es.close()
    kb.es = old_es
    kb.sfx = old_sfx
    if standalone:
        kb.wait_all("sp")
    return kb

import math

RG = [[0, 1], [2, 3], [4, 5], [6, 7]]

def build_fused():
    kb = KB()
    nc = kb.nc
    PS = [kb.ps(f"ps{i}", [128, 512], F32) for i in range(8)]
    meq = kb.sb("meq_sb", [128, 2], F32)
    g1 = kb.sb("g1c_sb", [128, 8], F32)
    meq_d = kb.dram("meq", [128, 2], F32, "ExternalInput")
    g1_d = kb.dram("g1c", [128, 8], F32, "ExternalInput")
    kb.dma("sp", meq[:], meq_d[:, :])
    kb.dma("sp", g1[:], g1_d[:, :])
    ysrc = nc.dram_tensor("ysrc", [2, 4, 2, 128, 2048], BF16)
    ydst = nc.dram_tensor("ydst", [4, 256, 2048], BF16)
    hsrc = nc.dram_tensor("hsrc", [2, 2, 1024, 2048], BF16)
    hdst = nc.dram_tensor("hdst", [2, 1024, 2048], BF16)
    xsp = nc.dram_tensor("xsp", [1024, 2048], F32)
    fo = dict(ysrc=ysrc, meq=meq)
    build_M(0.8 - 0.6 * math.exp(-0.3 * 0), kb=kb, sfx="_m0", PS=PS, fin=None, fout=fo)
    kb.collective("ReduceScatter", ALU.add, RG, ysrc, ydst, "ysrc", "ydst")
    build_R(False, kb=kb, sfx="_r0", PS=PS, xin=None, yin=ydst.ap(), xout=xsp.ap(), hout=dict(hsrc=hsrc, meq=meq, g=g1))
    kb.collective("ReduceScatter", ALU.add, RG, hsrc, hdst, "hsrc", "hdst")
    build_M(0.8 - 0.6 * math.exp(-0.3 * 1), kb=kb, sfx="_m1", PS=PS, fin=hdst.ap(), fout=fo)
    kb.collective("ReduceScatter", ALU.add, RG, ysrc, ydst, "ysrc", "ydst")
    build_R(True, kb=kb, sfx="_r1", PS=PS, xin=xsp.ap(), yin=ydst.ap(), xout=None, hout=None)
    kb.wait_all("sp")
    return kb

import math
import numpy as np

SP = [256, 448, 576, 608, 864, 1120, 1376, 1632, 1888]
f32 = np.float32


def bucket_consts():
    dist = np.arange(NJ, dtype=np.int64) - 511
    n = np.maximum(dist, 0)
    exact = 16
    lr = np.log(np.maximum(n, exact).astype(f32) / f32(exact)) / f32(math.log(128 / exact))
    large = np.minimum(exact + (lr * f32(32 - exact)).astype(np.int32), 31)
    bk = np.where(n < exact, n, large)
    oh = np.zeros((32, NJ), f32)
    oh[bk, np.arange(NJ)] = 1.0
    mask = (dist >= 0).astype(f32)
    return oh, np.tile(mask[None], (3, 1))


def rot_consts():
    rotT = np.zeros((96, 96), f32)
    for i in range(16):
        rotT[80 + i, 64 + i] = -1.0
        rotT[64 + i, 80 + i] = 1.0
    inv = (f32(10000.0) ** (-np.arange(16, dtype=f32) / f32(16))).astype(f32)
    return rotT, inv


def colpad(v, rows=128):
    out = np.zeros((rows,), f32)
    out[:len(v)] = v
    return out


def prep_M(I, l, b, p, xT):
    w_in = I["w_in"][l]
    cols = np.concatenate([
        np.arange(128 * p, 128 * p + 128),
        np.arange(256, 448), np.arange(448, 576), np.arange(576, 608),
        np.arange(608 + 128 * p, 608 + 128 * p + 128),
        np.arange(864 + 128 * p, 864 + 128 * p + 128),
        np.arange(1120 + 128 * p, 1120 + 128 * p + 128),
        np.arange(1376 + 128 * p, 1376 + 128 * p + 128),
        np.arange(1632 + 128 * p, 1632 + 128 * p + 128)])
    m = {}
    m["xT"] = np.ascontiguousarray(xT)
    m["wmix"] = np.ascontiguousarray(w_in[:, cols])
    m["pos"] = np.ascontiguousarray(I["positions"][b:b + 1].astype(np.int32))
    hs = [2 * p, 2 * p + 1]
    wuq = I["mla_w_uq"][l]
    m["wuq"] = np.ascontiguousarray(np.concatenate([wuq[:, 96 * h:96 * h + 96] for h in hs], axis=1))
    wukv = I["mla_w_ukv"][l]
    m["wkn"] = np.ascontiguousarray(np.concatenate([wukv[:, 128 * h:128 * h + 64] for h in hs], axis=1))
    m["wvm"] = np.ascontiguousarray(np.concatenate([wukv[:, 128 * h + 64:128 * h + 128] for h in hs], axis=1))
    rotT, inv = rot_consts()
    m["rotT"] = rotT
    for nm, key in (("wr", "lru_w_r"), ("wi", "lru_w_i")):
        w = np.zeros((128, 128), f32)
        for j in range(2):
            w[64 * j:64 * j + 64, 64 * j:64 * j + 64] = I[key][l][2 * p + j]
        m[nm] = w
    gs = slice(8 * p, 8 * p + 8)
    lr = I["s5_lam_re"][l][gs].reshape(512)
    li = I["s5_lam_im"][l][gs].reshape(512)
    ls = np.repeat(I["s5_log_step"][l][gs], 64)
    m["s5rep"] = np.ascontiguousarray(np.tile(np.stack([lr, li, ls])[None], (32, 1, 1)).astype(f32))
    bT = np.zeros((32, 2, 512), f32)
    cT = np.zeros((128, 2, 4, 32), f32)
    for gi in range(8):
        g = 8 * p + gi
        st, half = gi // 2, gi % 2
        for ri, key in enumerate(("s5_b_re", "s5_b_im")):
            bT[16 * half:16 * half + 16, ri, st * 128 + 64 * half: st * 128 + 64 * half + 64] = I[key][l][g].T
        for ri, key in enumerate(("s5_c_re", "s5_c_im")):
            cT[64 * half:64 * half + 64, ri, st, 16 * half:16 * half + 16] = I[key][l][g].T
    m["s5bT"] = bT
    m["s5cT"] = cT
    oh, maskrows = bucket_consts()
    m["oh"] = oh
    m["maskrows"] = maskrows
    m["relmy"] = np.ascontiguousarray(I["rel_table"][:, hs])
    pc = np.zeros((128, NPC), f32)

    def put(name, i, v):
        o, w = PC[name]
        pc[:len(v), o + i] = v
    for kt in range(8):
        put("gmix", kt, I["g_mix"][l][128 * kt:128 * kt + 128])
    put("gcq", 0, I["mla_g_cq"][l][0:128]); put("gcq", 1, I["mla_g_cq"][l][128:192])
    put("gckv", 0, I["mla_g_ckv"][l])
    put("mgq", 0, I["mla_g_qn"][l]); put("mgk", 0, I["mla_g_kn"][l])
    iv = np.zeros(128, f32); iv[64:96] = np.tile(inv, 2); put("inv", 0, iv)
    put("dgq", 0, np.tile(I["diff_g_qn"][l], 4)); put("dgk", 0, np.tile(I["diff_g_kn"][l], 4))
    put("gsub", 0, np.tile(I["diff_g_sub"][l], 2))
    for j, h in enumerate(hs):
        put("tb31", j, np.full(128, I["rel_table"][31, h], f32))
    for st in range(4):
        put("s5lr", st, lr[128 * st:128 * st + 128]); put("s5li", st, li[128 * st:128 * st + 128])
        put("s5ls", st, ls[128 * st:128 * st + 128])
    cs = slice(128 * p, 128 * p + 128)
    put("s5d", 0, I["s5_d"][l][cs])
    for k in range(4):
        put("cw", k, I["lru_conv_w"][l][k][cs])
    put("cb", 0, I["lru_conv_b"][l][cs]); put("br", 0, I["lru_b_r"][l][cs]); put("bi", 0, I["lru_b_i"][l][cs])
    put("llam", 0, I["lru_lam"][l][cs])
    for nm, key in (("lq1", "diff_lq1"), ("lk1", "diff_lk1"), ("lq2", "diff_lq2"), ("lk2", "diff_lk2")):
        o, w = PC[nm]
        pc[:, o:o + 32] = I[key][l][None, :]
    m["pcols"] = pc
    return m

import numpy as np
f32 = np.float32

def prep_R(I, l, b, p, xT_own, yall):
    m = {}
    m["xT"] = np.ascontiguousarray(xT_own)
    m["yall"] = np.ascontiguousarray(yall) if yall is not None else None
    w_in = I["w_in"][l]
    m["wg8"] = np.ascontiguousarray(np.stack([np.concatenate([w_in[:, 1888 + 1024 * bb + 128 * dt:1888 + 1024 * bb + 128 * dt + 128] for bb in range(4)], axis=1) for dt in range(8)]))
    m["wbr"] = np.ascontiguousarray(I["w_branch"][l])
    m["wout"] = np.ascontiguousarray(I["w_out"][l])
    m["wglu"] = np.ascontiguousarray(I["s5_w_glu"][l])
    for nm, key in (("xwq", "x_wq"), ("xwk", "x_wk"), ("xwv", "x_wv"), ("xwo", "x_wo")):
        m[nm] = np.ascontiguousarray(I[key][l])
    m["memT"] = np.ascontiguousarray(I["mem"][b].T)
    pc = np.zeros((128, NP2), f32)
    def put(name, i, v):
        o, w = P2[name]
        pc[:len(v), o + i] = v
    for nm, key in (("gmix", "g_mix"), ("gcross", "g_cross"), ("gmem", "g_mem"), ("gffn", "g_ffn")):
        for kt in range(8):
            put(nm, kt, I[key][l][128 * kt:128 * kt + 128])
    for bb in range(4):
        for dt in range(8):
            put("bgate", bb * 8 + dt, I["b_gate"][l][bb][128 * dt:128 * dt + 128])
    for j in range(2):
        put("bglu", j, I["s5_b_glu"][l][128 * j:128 * j + 128])
    put("xgq", 0, np.tile(I["x_g_qn"][l], 2)); put("xgk", 0, np.tile(I["x_g_kn"][l], 2))
    m["pc2"] = pc
    if l % 2 == 0:
        m["fg"] = np.ascontiguousarray(I["ffn_w_gate"][l // 2][None]); m["fu"] = np.ascontiguousarray(I["ffn_w_up"][l // 2][None])
        m["fd"] = np.ascontiguousarray(I["ffn_w_down"][l // 2][None])
    else:
        m["fg"] = I["moe_w_gate"][l // 2]; m["fu"] = I["moe_w_up"][l // 2]; m["fd"] = I["moe_w_down"][l // 2]
        m["wrt"] = np.ascontiguousarray(I["moe_w_router"][l // 2])
    return m

import numpy as np
f32 = np.float32

def prep_fused(I, c):
    b, p = c // 2, c % 2
    m = {}
    xT = np.ascontiguousarray(I["x"][b].T)
    ts = slice(2048 * p, 2048 * p + 2048)
    for l in range(2):
        mm = prep_M(I, l, b, p, xT)
        if l == 1:
            mm.pop("xT")
        for k, v in mm.items():
            m[k + f"_m{l}"] = v
        mr = prep_R(I, l, b, p, xT[:, ts], None)
        mr.pop("yall")
        if l == 1:
            mr.pop("xT")
        for k, v in mr.items():
            m[k + f"_r{l}"] = v
    meq = np.zeros((128, 2), f32)
    meq[:, p] = 1.0
    m["meq"] = meq
    m["g1c"] = np.ascontiguousarray(I["g_mix"][1].reshape(8, 128).T)
    return m


def kernel(**inputs):
    I = {k: np.asarray(v) for k, v in inputs.items()}
    kb = build_fused()
    maps = [prep_fused(I, c) for c in range(8)]
    res = kb.run(maps, n=8)
    out = np.empty((4, 4096, 1024), np.float32)
    for c in range(8):
        b, p = c // 2, c % 2
        out[b, 2048 * p:2048 * p + 2048, :] = res.results[c]["xo_r1"].T
    return out
```
